# Optimizing a Trainium2 kernel written in Bass

```python
import math
import jax
import jax.numpy as jnp
from jax import lax
import numpy as np

D_MODEL = 1024
BATCH = 4
SEQ = 4096
DEPTH = 2

H_A = 8
HD_A = 64
W_A = H_A * HD_A
MOBA_BLOCK = 256
MOBA_TOPK = 3
Q_CHUNK = 64
NUM_BUCKETS = 32
MAX_EXACT = NUM_BUCKETS // 2
MAX_DISTANCE = 128
H_B = 4
HD_B = 128
W_B = H_B * HD_B
CONV_K = 4
DN_CHUNK = 64
N_GROUPS = 4
EXPERTS_PER_GROUP = 8
TOPK_IN_GROUP = 2
D_FF_EXPERT = 256
D_IN = 3 * W_A + 4 * W_B + 2 * H_B + 2 * D_MODEL
N_MOD = 6
EPS = 1e-6
NEG = -1e30

kernel_name = "hybrid_moba_gdn_hmoe_block"


def rms_norm(x, w):
    xf = x.astype(jnp.float32)
    y = xf * lax.rsqrt(jnp.mean(xf * xf, axis=-1, keepdims=True) + EPS)
    return (y * w.astype(jnp.float32)).astype(x.dtype)


def l2_normalize(t):
    return t * lax.rsqrt(jnp.sum(t * t, axis=-1, keepdims=True) + EPS)


def t5_bucket(dist):
    n = jnp.maximum(dist, 0)
    nf = jnp.maximum(n, 1).astype(jnp.float32)
    large = MAX_EXACT + (jnp.log(nf / MAX_EXACT) / math.log(MAX_DISTANCE / MAX_EXACT)
                         * (NUM_BUCKETS - MAX_EXACT)).astype(jnp.int32)
    large = jnp.minimum(large, NUM_BUCKETS - 1)
    return jnp.where(n < MAX_EXACT, n, large)


def moba_attention(q, k, v, rel_bias):
    B, H, S, hd = q.shape
    L = MOBA_BLOCK
    Sp = -(-S // L) * L
    pad = ((0, 0), (0, 0), (0, Sp - S), (0, 0))
    q, k, v = jnp.pad(q, pad), jnp.pad(k, pad), jnp.pad(v, pad)
    NB, NQ = Sp // L, Sp // Q_CHUNK
    topk = min(MOBA_TOPK, NB)
    scale = HD_A ** -0.5
    k_blocks = k.reshape(B, H, NB, L, hd)
    v_blocks = v.reshape(B, H, NB, L, hd)
    k_mean = jnp.mean(k_blocks.astype(jnp.float32), axis=3)
    q_blk = jnp.arange(Sp) // L
    gate = jnp.einsum('bhsd,bhnd->bhsn', q.astype(jnp.float32), k_mean)
    gate = jnp.where(jnp.arange(NB)[None, :] < q_blk[:, None], gate, NEG)
    _, sel = lax.top_k(gate, topk)
    q_c = q.reshape(B, H, NQ, Q_CHUNK, hd).transpose(2, 0, 1, 3, 4)
    sel_c = sel.reshape(B, H, NQ, Q_CHUNK, topk).transpose(2, 0, 1, 3, 4)
    bi = jnp.arange(B)[:, None, None, None]
    hi = jnp.arange(H)[None, :, None, None]
    hi5 = hi[..., None]
    offs = jnp.arange(L)

    def one_chunk(args):
        q_i, sel_i, i = args
        qb = (i * Q_CHUNK) // L
        q_pos = i * Q_CHUNK + jnp.arange(Q_CHUNK)
        k_sel = k_blocks[bi, hi, sel_i]
        v_sel = v_blocks[bi, hi, sel_i]
        dist_sel = q_pos[None, None, :, None, None] - (sel_i[..., None] * L + offs)
        bias_sel = rel_bias[t5_bucket(dist_sel), hi5].astype(jnp.float32)
        s_sel = jnp.einsum('bhqd,bhqkld->bhqkl', q_i, k_sel).astype(jnp.float32) * scale + bias_sel
        s_sel = jnp.where((sel_i < qb)[..., None], s_sel, NEG)
        k_own = lax.dynamic_index_in_dim(k_blocks, qb, axis=2, keepdims=False)
        v_own = lax.dynamic_index_in_dim(v_blocks, qb, axis=2, keepdims=False)
        dist_own = q_pos[:, None] - (qb * L + offs)[None, :]
        bias_own = rel_bias[t5_bucket(dist_own)].transpose(2, 0, 1).astype(jnp.float32)
        s_own = jnp.einsum('bhqd,bhld->bhql', q_i, k_own).astype(jnp.float32) * scale + bias_own
        s_own = jnp.where(dist_own >= 0, s_own, NEG)
        p = jax.nn.softmax(jnp.concatenate(
            [s_sel.reshape(B, H, Q_CHUNK, topk * L), s_own], axis=-1), axis=-1)
        p_sel = p[..., :topk * L].reshape(B, H, Q_CHUNK, topk, L).astype(v.dtype)
        p_own = p[..., topk * L:].astype(v.dtype)
        return (jnp.einsum('bhqkl,bhqkld->bhqd', p_sel, v_sel)
                + jnp.einsum('bhql,bhld->bhqd', p_own, v_own))

    o = lax.map(one_chunk, (q_c, sel_c, jnp.arange(NQ)))
    o = o.transpose(1, 2, 0, 3, 4).reshape(B, H, Sp, hd)
    return o[:, :, :S]


def causal_depthwise_conv(x, w):
    C = x.shape[-1]
    return lax.conv_general_dilated(
        x, w[:, None, :].astype(x.dtype), window_strides=(1,), padding=[(CONV_K - 1, 0)],
        dimension_numbers=('NWC', 'WIO', 'NWC'), feature_group_count=C)


def gated_delta_rule(q, k, v, beta, g):
    out_dtype = v.dtype
    B, S, H, DK = q.shape
    DV = v.shape[-1]
    C = DN_CHUNK
    N = S // C
    f32 = jnp.float32
    q = l2_normalize(q.astype(f32)) * (DK ** -0.5)
    k = l2_normalize(k.astype(f32))
    v = v.astype(f32)

    def chunk(t):
        return t.reshape(B, N, C, H, -1).transpose(0, 3, 1, 2, 4)

    q, k, v = chunk(q), chunk(k), chunk(v)
    beta = beta.astype(f32).reshape(B, N, C, H).transpose(0, 3, 1, 2)
    G = jnp.cumsum(g.astype(f32).reshape(B, N, C, H).transpose(0, 3, 1, 2), axis=-1)
    tril = jnp.tril(jnp.ones((C, C), bool))
    strict = jnp.tril(jnp.ones((C, C), bool), -1)
    dec = jnp.exp(jnp.where(tril, G[..., :, None] - G[..., None, :], -jnp.inf))
    kb = k * beta[..., None]
    A = jnp.eye(C, dtype=f32) + jnp.where(strict, jnp.einsum('bhnid,bhnjd->bhnij', kb, k) * dec, 0.0)
    u = lax.linalg.triangular_solve(A, v * beta[..., None], left_side=True, lower=True, unit_diagonal=True)
    w = lax.linalg.triangular_solve(A, kb * jnp.exp(G)[..., None], left_side=True, lower=True, unit_diagonal=True)
    P = jnp.einsum('bhnid,bhnjd->bhnij', q, k) * dec
    qg = q * jnp.exp(G)[..., None]
    kd = k * jnp.exp(G[..., -1:] - G)[..., None]
    gl = jnp.exp(G[..., -1])
    xs = tuple(jnp.moveaxis(t, 2, 0) for t in (u, w, qg, P, kd, gl))

    def step(state, inp):
        u_n, w_n, qg_n, P_n, kd_n, gl_n = inp
        v_new = u_n - jnp.einsum('bhck,bhkv->bhcv', w_n, state)
        o_n = jnp.einsum('bhck,bhkv->bhcv', qg_n, state) + jnp.einsum('bhcj,bhjv->bhcv', P_n, v_new)
        state = state * gl_n[..., None, None] + jnp.einsum('bhck,bhcv->bhkv', kd_n, v_new)
        return state, o_n

    _, o = lax.scan(step, jnp.zeros((B, H, DK, DV), f32), xs)
    return o.transpose(1, 0, 3, 2, 4).reshape(B, S, H, DV).astype(out_dtype)


def token_mixer(h, rel_bias, w_in, conv_w, a_log, dt_bias, onorm_w, w_up_a, w_up_b, w_out):
    B, S, _ = h.shape
    proj = h @ w_in
    sizes = (W_A, W_A, W_A, 3 * W_B, W_B, H_B, H_B, D_MODEL, D_MODEL)
    offs = np.cumsum(sizes)[:-1].tolist()
    qa, ka, va, qkv_b, z_b, b_b, a_b, gate_a, gate_b = jnp.split(proj, offs, axis=-1)
    heads_a = lambda t: t.reshape(B, S, H_A, HD_A).transpose(0, 2, 1, 3)
    y_a = moba_attention(heads_a(qa), heads_a(ka), heads_a(va), rel_bias)
    y_a = y_a.transpose(0, 2, 1, 3).reshape(B, S, W_A)
    qkv_b = jax.nn.silu(causal_depthwise_conv(qkv_b, conv_w))
    q_b, k_b, v_b = (t.reshape(B, S, H_B, HD_B) for t in jnp.split(qkv_b, 3, axis=-1))
    beta = jax.nn.sigmoid(b_b)
    g = -jnp.exp(a_log) * jax.nn.softplus(a_b + dt_bias)
    o_b = gated_delta_rule(q_b, k_b, v_b, beta, g)
    o_b = rms_norm(o_b, onorm_w) * jax.nn.silu(z_b.reshape(B, S, H_B, HD_B))
    y_b = o_b.reshape(B, S, W_B)
    m = jax.nn.sigmoid(gate_a) * (y_a @ w_up_a) + jax.nn.sigmoid(gate_b) * (y_b @ w_up_b)
    return m @ w_out


def hier_moe(h, w_rg, b_rg, w_re, b_re, w_e_gate, w_e_up, w_e_down):
    B, S, D = h.shape
    t = h.reshape(-1, D)
    T = t.shape[0]
    pg = jax.nn.softmax((t @ w_rg + b_rg).astype(jnp.float32), axis=-1)
    p_top, g_top = lax.top_k(pg, 1)
    oh_g = jax.nn.one_hot(g_top[:, 0], N_GROUPS, dtype=jnp.float32)
    le = (t @ w_re + b_re).astype(jnp.float32).reshape(T, N_GROUPS, EXPERTS_PER_GROUP)
    le_g = jnp.einsum('tge,tg->te', le, oh_g)
    v2, e2 = lax.top_k(le_g, TOPK_IN_GROUP)
    w2 = jax.nn.softmax(v2, axis=-1) * p_top
    comb_e = jnp.sum(w2[..., None] * jax.nn.one_hot(e2, EXPERTS_PER_GROUP, dtype=jnp.float32), axis=1)
    comb = (oh_g[:, :, None] * comb_e[:, None, :]).astype(t.dtype)
    y = jnp.zeros_like(t)
    for gi in range(N_GROUPS):
        a = jax.nn.silu(jnp.einsum('td,edf->tef', t, w_e_gate[gi])) * jnp.einsum('td,edf->tef', t, w_e_up[gi])
        y = y + jnp.einsum('tef,te,efd->td', a, comb[:, gi], w_e_down[gi])
    return y.reshape(B, S, D)


def setup_inputs(seed: int = 0) -> dict:
    key = jax.random.key(seed)
    ks = iter(jax.random.split(key, 32))
    L, D = DEPTH, D_MODEL
    G, E, F = N_GROUPS, EXPERTS_PER_GROUP, D_FF_EXPERT

    def nrm(shape, std):
        return std * jax.random.normal(next(ks), shape, jnp.float32)

    x = nrm((BATCH, SEQ, D), 1.0)
    c = nrm((BATCH, D), 1.0)
    rel_bias = nrm((NUM_BUCKETS, H_A), 0.5)
    final_norm_w = 1.0 + nrm((D,), 0.02)
    norm1_w = 1.0 + nrm((L, D), 0.02)
    norm2_w = 1.0 + nrm((L, D), 0.02)
    w_mod = nrm((L, D, N_MOD * D), 0.5 * D ** -0.5)
    b_mod = nrm((L, N_MOD * D), 0.02)
    w_in = nrm((L, D, D_IN), D ** -0.5)
    conv_w = nrm((L, CONV_K, 3 * W_B), CONV_K ** -0.5)
    a_log = jnp.log(jax.random.uniform(next(ks), (L, H_B), jnp.float32, 1.0, 16.0))
    dt = jnp.exp(jax.random.uniform(next(ks), (L, H_B), jnp.float32, math.log(1e-3), math.log(1e-1)))
    dt_bias = dt + jnp.log(-jnp.expm1(-dt))
    onorm_w = 1.0 + nrm((L, HD_B), 0.02)
    w_up_a = nrm((L, W_A, D), W_A ** -0.5)
    w_up_b = nrm((L, W_B, D), W_B ** -0.5)
    w_out = nrm((L, D, D), D ** -0.5)
    w_rg = nrm((L, D, G), D ** -0.5)
    b_rg = nrm((L, G), 0.01)
    w_re = nrm((L, D, G * E), D ** -0.5)
    b_re = nrm((L, G * E), 0.01)
    w_e_gate = nrm((L, G, E, D, F), D ** -0.5)
    w_e_up = nrm((L, G, E, D, F), D ** -0.5)
    w_e_down = nrm((L, G, E, F, D), F ** -0.5)
    return {"x": x, "c": c, "rel_bias": rel_bias, "final_norm_w": final_norm_w,
            "norm1_w": norm1_w, "norm2_w": norm2_w, "w_mod": w_mod, "b_mod": b_mod,
            "w_in": w_in, "conv_w": conv_w, "a_log": a_log, "dt_bias": dt_bias,
            "onorm_w": onorm_w, "w_up_a": w_up_a, "w_up_b": w_up_b, "w_out": w_out,
            "w_rg": w_rg, "b_rg": b_rg, "w_re": w_re, "b_re": b_re,
            "w_e_gate": w_e_gate, "w_e_up": w_e_up, "w_e_down": w_e_down}


def reference(x, c, rel_bias, final_norm_w, norm1_w, norm2_w, w_mod, b_mod, w_in, conv_w,
              a_log, dt_bias, onorm_w, w_up_a, w_up_b, w_out, w_rg, b_rg, w_re, b_re,
              w_e_gate, w_e_up, w_e_down):
    c_act = jax.nn.silu(c)
    for l in range(DEPTH):
        mod = (c_act @ w_mod[l] + b_mod[l])[:, None, :]
        sh1, sc1, g1, sh2, sc2, g2 = jnp.split(mod, N_MOD, axis=-1)
        h = rms_norm(x, norm1_w[l]) * (1.0 + sc1) + sh1
        x = x + g1 * token_mixer(h, rel_bias, w_in[l], conv_w[l], a_log[l], dt_bias[l],
                                 onorm_w[l], w_up_a[l], w_up_b[l], w_out[l])
        h = rms_norm(x, norm2_w[l]) * (1.0 + sc2) + sh2
        x = x + g2 * hier_moe(h, w_rg[l], b_rg[l], w_re[l], b_re[l],
                              w_e_gate[l], w_e_up[l], w_e_down[l])
    return rms_norm(x, final_norm_w)
```

```python
import math
from contextlib import ExitStack

import numpy as np
import ml_dtypes
import concourse.bass as bass
import concourse.mybir as mybir
from concourse.bass_utils import run_bass_kernel_spmd

F32 = mybir.dt.float32
BF16 = mybir.dt.bfloat16
AF = mybir.ActivationFunctionType
ALU = mybir.AluOpType
AX = mybir.AxisListType

D = 1024
T = 4096
TO = 2048
NT = 32
H_A = 8
H_B = 4
D_IN = 5640
EPS = 1e-6
BIG = 30000.0

ENGS = ("pe", "act", "dve", "pool", "sp")
NDMA = 12


class Buf:
    __slots__ = ("lw", "rs", "excl")

    def __init__(self, excl=False):
        self.lw = None
        self.rs = []
        self.excl = excl


class Op:
    __slots__ = ("eng", "fn", "deps", "dma", "needed", "sigval", "dsem", "dval", "prev_slot", "phase")

    def __init__(self, eng, fn, dma, phase):
        self.eng = eng
        self.fn = fn
        self.deps = []
        self.dma = dma
        self.needed = False
        self.sigval = None
        self.dsem = None
        self.dval = None
        self.prev_slot = None
        self.phase = phase


class Sched:
    def __init__(self):
        self.ops = {e: [] for e in ENGS}
        self.dma_slot = {e: 0 for e in ENGS}
        self.dma_last = {}
        self.dma_cnt = {}
        self.phase = 0
        self.last = {e: None for e in ENGS}

    def add(self, eng, fn, reads=(), writes=(), dma=False):
        op = Op(eng, fn, dma, self.phase)
        deps = []
        xr = [b for b in reads if b.excl]
        if xr:
            reads = [b for b in reads if not b.excl]
            writes = list(writes) + xr
        for b in reads:
            if b.lw is not None:
                deps.append(b.lw)
        for b in writes:
            if b.lw is not None:
                deps.append(b.lw)
            deps.extend(b.rs)
        seen = set()
        for d in deps:
            if id(d) in seen or d.phase < self.phase:
                continue
            seen.add(id(d))
            if d.eng == "pe" and eng == "pe" and not d.dma and not dma:
                continue
            op.deps.append(d)
            d.needed = True
        for b in reads:
            if not dma:
                b.rs = [o for o in b.rs if o.dma or o.eng != eng]
            b.rs.append(op)
        for b in writes:
            b.lw = op
            b.rs = []
        if dma:
            slot = self.dma_slot[eng]
            self.dma_slot[eng] = (slot + 1) % NDMA
            key = (eng, slot)
            op.prev_slot = self.dma_last.get(key)
            self.dma_last[key] = op
            self.dma_cnt[key] = self.dma_cnt.get(key, 0) + 16
            op.dsem = key
            op.dval = self.dma_cnt[key]
        else:
            self.last[eng] = op
        self.ops[eng].append(op)
        return op

    def barrier(self):
        deps = []
        for e in ENGS:
            if self.last[e] is not None:
                deps.append(self.last[e])
        deps.extend(self.dma_last.values())
        for e in ENGS:
            op = Op(e, None, False, self.phase)
            for d in deps:
                op.deps.append(d)
                d.needed = True
            self.ops[e].append(op)
        self.phase += 1

    def emit(self, block, esem, dsem):
        for e in ENGS:
            cnt = 0
            for op in self.ops[e]:
                if op.fn is not None and not op.dma and op.needed:
                    cnt += 1
                    op.sigval = cnt
        sched = self

        def run_engine(e, engobj):
            known = {}

            def wait(key, sem, val):
                if known.get(key, 0) >= val:
                    return
                engobj.wait_ge(sem, val)
                known[key] = val

            for op in sched.ops[e]:
                for d in op.deps:
                    if d.dma:
                        wait(d.dsem, dsem[d.dsem], d.dval)
                    else:
                        wait(d.eng, esem[d.eng], d.sigval)
                if op.fn is None:
                    continue
                if op.dma:
                    if op.prev_slot is not None:
                        p = op.prev_slot
                        wait(p.dsem, dsem[p.dsem], p.dval)
                    op.fn(engobj).then_inc(dsem[op.dsem], 16)
                else:
                    ins = op.fn(engobj)
                    if op.needed:
                        ins.then_inc(esem[e], 1)
            for (qe, slot), last in sched.dma_last.items():
                if qe == e:
                    wait(last.dsem, dsem[last.dsem], last.dval)

        block.tensor(lambda eng: run_engine("pe", eng))
        block.scalar(lambda eng: run_engine("act", eng))
        block.vector(lambda eng: run_engine("dve", eng))
        block.gpsimd(lambda eng: run_engine("pool", eng))
        block.sync(lambda eng: run_engine("sp", eng))


class Ring:
    def __init__(self, items):
        self.items = items
        self.i = 0

    def get(self):
        it = self.items[self.i % len(self.items)]
        self.i += 1
        return it


class K:
    def __init__(self, nc):
        self.nc = nc
        self.S = Sched()
        self.uid = 0

    def sb(self, st, shape, dt, name=None):
        self.uid += 1
        t = st.enter_context(self.nc.sbuf_tensor(f"{name or 't'}_{self.uid}", list(shape), dt))
        return t, Buf()

    def ps(self, st, shape, dt, name=None):
        self.uid += 1
        t = st.enter_context(self.nc.psum_tensor(f"{name or 'p'}_{self.uid}", list(shape), dt))
        return t, Buf(excl=True)

    def ring_sb(self, st, n, shape, dt, name=None):
        return Ring([self.sb(st, shape, dt, name) for _ in range(n)])

    def ring_ps(self, st, n, shape, dt, name=None):
        return Ring([self.ps(st, shape, dt, name) for _ in range(n)])

    def dma(self, out, in_, r=(), w=(), q="sp"):
        self.S.add(q, lambda e: e.dma_start(out=out, in_=in_), reads=r, writes=w, dma=True)

    def mm(self, out, lhsT, rhs, start, stop, r=(), w=()):
        self.S.add("pe", lambda e: e.matmul(out, lhsT=lhsT, rhs=rhs, start=start, stop=stop, skip_group_check=True), reads=r, writes=w)

    def tr(self, out, in_, ident, r=(), w=()):
        self.S.add("pe", lambda e: e.transpose(out=out, in_=in_, identity=ident), reads=r, writes=w)

    def act(self, out, in_, func, r=(), w=(), bias=None, scale=None, accum_out=None):
        kw = {}
        if bias is not None:
            kw["bias"] = bias
        if scale is not None:
            kw["scale"] = scale
        if accum_out is not None:
            kw["accum_out"] = accum_out
        self.S.add("act", lambda e: e.activation(out=out, in_=in_, func=func, **kw), reads=r, writes=w)

    def tt(self, eng, out, in0, in1, op, r=(), w=()):
        self.S.add(eng, lambda e: e.tensor_tensor(out=out, in0=in0, in1=in1, op=op), reads=r, writes=w)

    def ts(self, eng, out, in0, s1, s2, op0, op1=None, r=(), w=()):
        if op1 is None:
            self.S.add(eng, lambda e: e.tensor_scalar(out=out, in0=in0, scalar1=s1, scalar2=None, op0=op0),
                       reads=r, writes=w)
        else:
            self.S.add(eng, lambda e: e.tensor_scalar(out=out, in0=in0, scalar1=s1, scalar2=s2, op0=op0, op1=op1),
                       reads=r, writes=w)

    def stt(self, eng, out, in0, scalar, in1, op0, op1, r=(), w=()):
        self.S.add(eng, lambda e: e.scalar_tensor_tensor(out=out, in0=in0, scalar=scalar, in1=in1, op0=op0, op1=op1),
                   reads=r, writes=w)

    def cp(self, eng, out, in_, r=(), w=()):
        if eng == "act":
            self.S.add("act", lambda e: e.activation(out=out, in_=in_, func=AF.Copy), reads=r, writes=w)
        else:
            self.S.add(eng, lambda e: e.tensor_copy(out=out, in_=in_), reads=r, writes=w)

    def memset(self, eng, out, val, r=(), w=()):
        self.S.add(eng, lambda e: e.memset(out, val), reads=r, writes=w)

    def red(self, eng, out, in_, op, r=(), w=()):
        self.S.add(eng, lambda e: e.tensor_reduce(out=out, in_=in_, axis=AX.X, op=op), reads=r, writes=w)

    def recip(self, out, in_, r=(), w=()):
        self.S.add("dve", lambda e: e.reciprocal(out=out, in_=in_), reads=r, writes=w)

    def max8(self, out, in_, r=(), w=()):
        self.S.add("dve", lambda e: e.max(out=out, in_=in_), reads=r, writes=w)


def t5_bucket_np(dist):
    n = np.maximum(dist, 0)
    nf = np.maximum(n, 1).astype(np.float32)
    large = 16 + (np.log(nf / np.float32(16)) / np.float32(math.log(8.0)) * np.float32(16)).astype(np.int32)
    large = np.minimum(large, 31)
    return np.where(n < 16, n, large)


def build_program(final, dbg=False, stop_after=None, nl=1):
    nc = bass.Bass("TRN2", target_bir_lowering=False)
    kb = K(nc)
    S = kb.S

    def din(name, shape, dt=F32):
        return nc.dram_tensor(name, list(shape), dt, kind="ExternalInput").ap()

    def dscr(name, shape, dt):
        return nc.dram_tensor(name, list(shape), dt, kind=("ExternalOutput" if dbg else "Internal")).ap()

    xf = din("xf", [T, D])
    xo = din("xo", [TO, D])
    cT = din("cT", [128, 8])
    sel = din("sel", [128, 2])
    WL = []
    for li_ in range(nl):
        sfx = "" if nl == 1 else f"_{li_}"
        Wd_ = {}
        Wd_["w_mod"] = din("w_mod" + sfx, [D, 6 * D])
        Wd_["b_mod"] = din("b_mod" + sfx, [1, 6 * D])
        Wd_["n1w"] = din("n1w" + sfx, [128, 8])
        Wd_["n2w"] = din("n2w" + sfx, [128, 8])
        Wd_["w_in"] = din("w_in" + sfx, [D, D_IN])
        Wd_["convT"] = din("convT" + sfx, [128, 12, 4])
        Wd_["alog"] = din("alog" + sfx, [128, 4])
        Wd_["dtb"] = din("dtb" + sfx, [128, 4])
        Wd_["onw"] = din("onw" + sfx, [128, 512])
        Wd_["w_up_a"] = din("w_up_a" + sfx, [512, D])
        Wd_["w_up_b"] = din("w_up_b" + sfx, [512, D])
        Wd_["w_out"] = din("w_out" + sfx, [D, D])
        Wd_["w_r"] = din("w_r" + sfx, [D, 36])
        Wd_["b_r"] = din("b_r" + sfx, [128, 36])
        Wd_["w_eg"] = din("w_eg" + sfx, [32, D, 256])
        Wd_["w_eu"] = din("w_eu" + sfx, [32, D, 256])
        Wd_["w_ed"] = din("w_ed" + sfx, [32, 256, D])
        WL.append(Wd_)
    fnw = din("fnw", [128, D])
    identb_d = din("identb", [128, 128], BF16)
    identf_d = din("identf", [128, 128])
    onehotK = din("onehotK", [16, T], BF16)
    padneg_d = din("padneg", [128, 17, 128])
    braw = din("braw", [8, 128, 1024])
    negm = din("negm", [128, 1024])
    b31 = din("b31", [128, 8])
    mneg_d = din("mneg", [128, 128])
    strict_d = din("strict", [128, 128])
    ut_d = din("ut", [128, 128])
    bmask_d = din("bmask", [128, 5, 128], BF16)

    out = nc.dram_tensor("out", [TO, D], F32, kind="ExternalOutput").ap()

    qT_s = dscr("qT_s", [4, 128, T], BF16)
    kT_s = dscr("kT_s", [4, 128, T], BF16)
    MT_s = dscr("MT_s", [128, T], BF16)
    v_s = dscr("v_s", [NT, 128, 520], BF16)
    qTb_s = dscr("qTb_s", [4, 128, T], BF16)
    kTb_s = dscr("kTb_s", [4, 128, T], BF16)
    kb_s = dscr("kb_s", [NT, 128, 512], BF16)
    vb_s = dscr("vb_s", [NT, 128, 512], BF16)
    z_s = dscr("z_s", [NT, 128, 512], F32)
    ya_s = dscr("ya_s", [T, 512], BF16)
    xown_s = nc.dram_tensor("xown_s", [TO, D], F32, kind="Internal").ap()
    xfull_s = nc.dram_tensor("xfull_s", [T, D], F32, kind="Internal").ap()
    b_xown_s, b_xfull_s = Buf(), Buf()
    yb_s = dscr("yb_s", [T, 512], BF16)
    b_qT_s, b_kT_s, b_MT_s, b_v_s = Buf(), Buf(), Buf(), Buf()
    b_qTb_s, b_kTb_s, b_kb_s, b_vb_s, b_z_s, b_ya_s, b_yb_s = Buf(), Buf(), Buf(), Buf(), Buf(), Buf(), Buf()
    if dbg:
        dbg_mod = nc.dram_tensor("dbg_mod", [128, 48], F32, kind="ExternalOutput").ap()
        dbg_bg = nc.dram_tensor("dbg_bg", [128, NT, 8], F32, kind="ExternalOutput").ap()
        dbg_x1 = nc.dram_tensor("dbg_x1", [TO, D], F32, kind="ExternalOutput").ap()
    b_out = Buf()

    with ExitStack() as top:
        identb, b_identb = kb.sb(top, [128, 128], BF16, "identb")
        identf, b_identf = kb.sb(top, [128, 128], F32, "identf")
        onesb, b_onesb = kb.sb(top, [128, 128], BF16, "onesb")
        onesf, b_onesf = kb.sb(top, [128, 128], F32, "onesf")
        epsc, b_epsc = kb.sb(top, [128, 1], F32, "epsc")
        modT, b_modT = kb.sb(top, [128, 48], F32, "modT")
        AB, b_AB = kb.sb(top, [128, 4, 8], F32, "AB")
        G1bc, b_G1bc = kb.sb(top, [128, D], F32, "G1bc")
        G2bc, b_G2bc = kb.sb(top, [128, D], F32, "G2bc")
        BG, b_BG = kb.sb(top, [128, NT, 8], F32, "BG")
        selt, b_selt = kb.sb(top, [128, 2], F32, "selt")
        consts_r = [b_identb, b_identf, b_onesb, b_onesf, b_epsc]

        kb.dma(identb[:], identb_d[:, :], w=[b_identb])
        kb.dma(identf[:], identf_d[:, :], w=[b_identf])
        kb.dma(selt[:], sel[:, :], w=[b_selt])
        kb.memset("pool", onesb[:], 1.0, w=[b_onesb])
        kb.memset("pool", onesf[:], 1.0, w=[b_onesf])
        kb.memset("pool", epsc[:], EPS, w=[b_epsc])

        for li in range(nl):
            Wl = WL[li]
            w_mod = Wl["w_mod"]
            b_mod = Wl["b_mod"]
            n1w = Wl["n1w"]
            n2w = Wl["n2w"]
            w_in = Wl["w_in"]
            convT = Wl["convT"]
            alog = Wl["alog"]
            dtb = Wl["dtb"]
            onw = Wl["onw"]
            w_up_a = Wl["w_up_a"]
            w_up_b = Wl["w_up_b"]
            w_out = Wl["w_out"]
            w_r = Wl["w_r"]
            b_r = Wl["b_r"]
            w_eg = Wl["w_eg"]
            w_eu = Wl["w_eu"]
            w_ed = Wl["w_ed"]
            xsrc_full = xf if li == 0 else xfull_s
            xsrc_own = xo if li == 0 else xown_s
            last = (li == nl - 1)
            with ExitStack() as ph:
                ct_t, b_ct = kb.sb(ph, [128, 8], F32)
                cact, b_cact = kb.sb(ph, [128, 8], F32)
                CB, b_CB = kb.sb(ph, [128, 8, 128], F32)
                bm, b_bm = kb.sb(ph, [1, 6 * D], F32)
                modbc, b_modbc = kb.sb(ph, [128, 6 * D], F32)
                n1t, b_n1t = kb.sb(ph, [128, 8], F32)
                n2t, b_n2t = kb.sb(ph, [128, 8], F32)
                stg = kb.ring_sb(ph, 2, [128, 8, 512], F32)
                pmr = kb.ring_ps(ph, 2, [128, 512], F32)
                kb.dma(ct_t[:], cT[:, :], w=[b_ct])
                kb.dma(bm[:], b_mod[:, :], w=[b_bm])
                kb.dma(n1t[:], n1w[:, :], w=[b_n1t])
                kb.dma(n2t[:], n2w[:, :], w=[b_n2t])
                kb.act(cact[:], ct_t[:], AF.Silu, r=[b_ct], w=[b_cact])
                kb.cp("dve", CB[:], cact[:, :].unsqueeze(2).to_broadcast([128, 8, 128]), r=[b_cact], w=[b_CB])
                wmv = w_mod.rearrange("(k p) n -> p k n", p=128)
                for jg in range(12):
                    st_t, b_st = stg.get()
                    kb.dma(st_t[:], wmv[:, :, jg * 512:(jg + 1) * 512], w=[b_st])
                    pm, b_pm = pmr.get()
                    for k in range(8):
                        kb.mm(pm[:], CB[:, k, :], st_t[:, k, :], k == 0, False, r=[b_CB, b_st], w=[b_pm])
                    kb.mm(pm[:], onesf[0:1, :], bm[0:1, jg * 512:(jg + 1) * 512], False, True, r=[b_onesf, b_bm], w=[b_pm])
                    kb.cp("act" if jg % 2 else "dve", modbc[:, jg * 512:(jg + 1) * 512], pm[:], r=[b_pm], w=[b_modbc])
                with ExitStack() as ph2:
                    prod, b_prod = kb.sb(ph2, [128, 48, 128], F32)
                    kb.tt("dve", prod[:], modbc[:, :].rearrange("p (a b) -> p a b", b=128),
                          identf[:, :].unsqueeze(1).to_broadcast([128, 48, 128]), ALU.mult,
                          r=[b_modbc, b_identf], w=[b_prod])
                    kb.red("dve", modT[:], prod[:], ALU.add, r=[b_prod], w=[b_modT])
                kb.stt("dve", AB[:, 0, :], modT[:, 8:16], 1.0, n1t[:], ALU.add, ALU.mult, r=[b_modT, b_n1t], w=[b_AB])
                kb.cp("dve", AB[:, 1, :], modT[:, 0:8], r=[b_modT], w=[b_AB])
                kb.stt("dve", AB[:, 2, :], modT[:, 32:40], 1.0, n2t[:], ALU.add, ALU.mult, r=[b_modT, b_n2t], w=[b_AB])
                kb.cp("dve", AB[:, 3, :], modT[:, 24:32], r=[b_modT], w=[b_AB])
                kb.cp("act", G1bc[:], modbc[:, 2 * D:3 * D], r=[b_modbc], w=[b_G1bc])
                kb.cp("act", G2bc[:], modbc[:, 5 * D:6 * D], r=[b_modbc], w=[b_G2bc])
                if dbg and li == 0:
                    kb.dma(dbg_mod[:, :], modT[:], r=[b_modT], w=[Buf()])
                S.barrier()

            if li == 0 and stop_after == "M":
                return finish(nc, kb, top, out, b_out)

            with ExitStack() as ph:
                Wm, b_Wm = kb.sb(ph, [128, 8, 3600], BF16, "Wm")
                wiv = w_in.rearrange("(k p) n -> p k n", p=128)
                with ExitStack() as ph2:
                    stg = kb.ring_sb(ph2, 2, [128, 8, 512], F32)
                    for g in range(8):
                        c0 = g * 512
                        cw = min(512, 3600 - c0)
                        st_t, b_st = stg.get()
                        kb.dma(st_t[:, :, 0:cw], wiv[:, :, c0:c0 + cw], w=[b_st])
                        kb.cp("pool" if g % 2 else "dve", Wm[:, :, c0:c0 + cw], st_t[:, :, 0:cw], r=[b_st], w=[b_Wm])
                    S.barrier()
                padneg, b_padneg = kb.sb(ph, [128, 17, 128], F32, "padneg")
                kb.dma(padneg[:], padneg_d[:, :, :], w=[b_padneg])
                cvw, b_cvw = kb.sb(ph, [128, 12, 4], F32, "cvw")
                kb.dma(cvw[:], convT[:, :, :], w=[b_cvw])
                negA, b_negA = kb.sb(ph, [128, 4], F32, "negA")
                dtb_t, b_dtb = kb.sb(ph, [128, 4], F32, "dtb")
                kb.dma(negA[:], alog[:, :], w=[b_negA])
                kb.dma(dtb_t[:], dtb[:, :], w=[b_dtb])
                kb.act(negA[:], negA[:], AF.Exp, r=[b_negA], w=[b_negA])
                kb.ts("dve", negA[:], negA[:], -1.0, None, ALU.mult, r=[b_negA], w=[b_negA])
                KMbd, b_KMbd = kb.sb(ph, [128, 4, 32], BF16, "KMbd")
                kb.memset("pool", KMbd[:], 0.0, w=[b_KMbd])
                raw, b_raw = kb.sb(ph, [128, 12, 515], BF16, "raw")
                Dg, b_Dg = kb.sb(ph, [128, 12, 4, 128], BF16, "Dg")
                for ct_ in range(12):
                    for jj_ in range(4):
                        kb.ts("dve", Dg[:, ct_, jj_, :], identf[:], cvw[:, ct_, jj_:jj_ + 1], None, ALU.mult,
                              r=[b_identf, b_cvw], w=[b_Dg])
                b_rawc = [Buf() for _ in range(12)]
                kb.memset("pool", raw[:, :, 0:3], 0.0, w=b_rawc)

                xt_r = kb.ring_sb(ph, 3, [128, D], F32, "xt")
                junk_r = kb.ring_sb(ph, 2, [128, D], BF16, "junk")
                xn_r = kb.ring_sb(ph, 2, [128, D], BF16, "xn")
                st_r = kb.ring_sb(ph, 4, [128, 2], F32, "st")
                hT_r = kb.ring_sb(ph, 2, [128, 8, 512], BF16, "hT")
                pT_r = kb.ring_ps(ph, 2, [128, 8, 128], BF16, "pT")
                pm_r = kb.ring_ps(ph, 4, [128, 512], F32, "pm")
                pg_r = kb.ring_ps(ph, 1, [128, 512], F32, "pg")
                pX_r = kb.ring_ps(ph, 1, [128, 8, 128], BF16, "pX")
                qsb_r = kb.ring_sb(ph, 8, [128, 512], BF16, "qsb")
                ksb_r = kb.ring_sb(ph, 3, [128, 512], BF16, "ksb")
                km2_r = kb.ring_sb(ph, 2, [128, 2], F32, "km2")
                vsb_r = kb.ring_sb(ph, 3, [128, 8, 65], BF16, "vsb")
                for (vt, bvt) in vsb_r.items:
                    kb.memset("pool", vt[:, :, 64:65], 1.0, w=[bvt])
                zsb_r = kb.ring_sb(ph, 2, [128, 512], F32, "zsb")
                Gs_r = kb.ring_sb(ph, 2, [128, 128], F32, "Gs")
                m8_r = kb.ring_sb(ph, 2, [128, 8, 8], F32, "m8")
                Mbf_r = kb.ring_sb(ph, 5, [128, 128], BF16, "Mbf")
                MTsb_r = kb.ring_sb(ph, 2, [128, 512], BF16, "MTsb")
                t4_r = kb.ring_sb(ph, 4, [128, 8], F32, "t4")
                sc_r = kb.ring_sb(ph, 6, [128, 512], F32, "sc")
                sq_r = kb.ring_sb(ph, 4, [128, 512], BF16, "sq")
                rr_r = kb.ring_sb(ph, 3, [128, 512], F32, "rr")
                nrm_r = kb.ring_sb(ph, 5, [128, 512], BF16, "nrm")
                tok_r = kb.ring_sb(ph, 3, [128, 4, 128], BF16, "tok")

                def do_norm(c):
                    hT, b_hT = hT_r.get()
                    for j in range(4):
                        ti = 4 * c + j
                        xt, b_xt = xt_r.get()
                        kb.dma(xt[:], xsrc_full[ti * 128:(ti + 1) * 128, :], r=[b_xfull_s], w=[b_xt])
                        junk, b_junk = junk_r.get()
                        st, b_st = st_r.get()
                        kb.act(junk[:], xt[:], AF.Square, r=[b_xt], w=[b_junk, b_st], accum_out=st[:, 0:1])
                        kb.ts("dve", st[:, 1:2], st[:, 0:1], 1.0 / D, EPS, ALU.mult, ALU.add, r=[b_st], w=[b_st])
                        kb.act(st[:, 1:2], st[:, 1:2], AF.Sqrt, r=[b_st], w=[b_st])
                        kb.recip(st[:, 1:2], st[:, 1:2], r=[b_st], w=[b_st])
                        xn, b_xn = xn_r.get()
                        kb.act(xn[:], xt[:], AF.Copy, r=[b_xt, b_st], w=[b_xn], scale=st[:, 1:2])
                        pT, b_pT = pT_r.get()
                        for k in range(8):
                            kb.tr(pT[:, k, :], xn[:, k * 128:(k + 1) * 128], identb[:], r=[b_xn, b_identb], w=[b_pT])
                        hv = hT[:, :, j * 128:(j + 1) * 128]
                        kb.tt("dve", hv, pT[:], AB[:, 0, :].unsqueeze(2).to_broadcast([128, 8, 128]), ALU.mult,
                              r=[b_pT, b_AB], w=[b_hT])
                        kb.tt("pool", hv, hv, AB[:, 1, :].unsqueeze(2).to_broadcast([128, 8, 128]), ALU.add,
                              r=[b_hT, b_AB], w=[b_hT])
                    return hT, b_hT

                nxt_h = do_norm(0)
                for c in range(8):
                    hT, b_hT = nxt_h
                    if c + 1 < 8:
                        nxt_h = do_norm(c + 1)
                    cs = slice(c * 512, (c + 1) * 512)
                    import os
                    PARTS = os.environ.get("KPARTS", "kqsvg")
                    for p in range(4 if "k" in PARTS else 0):
                        pm, b_pm = pm_r.get()
                        for k in range(8):
                            kb.mm(pm[:], Wm[:, k, 512 + p * 128:512 + (p + 1) * 128], hT[:, k, :], k == 0, k == 7,
                                  r=[b_Wm, b_hT], w=[b_pm])
                        ksb, b_ksb = ksb_r.get()
                        kb.cp("act", ksb[:], pm[:], r=[b_pm], w=[b_ksb])
                        kb.dma(kT_s[p, :, cs], ksb[:], r=[b_ksb], w=[b_kT_s])
                        km2, b_km2 = km2_r.get()
                        kb.red("dve", km2[:], pm[:, :].rearrange("p (a b) -> p a b", b=256), ALU.add, r=[b_pm], w=[b_km2])
                        kb.cp("dve", KMbd[0:64, p, 2 * c:2 * c + 2], km2[0:64, :], r=[b_km2], w=[b_KMbd])
                        kb.cp("dve", KMbd[64:128, p, 16 + 2 * c:16 + 2 * c + 2], km2[64:128, :], r=[b_km2], w=[b_KMbd])
                    qs = []
                    for p in range(4 if "q" in PARTS else 0):
                        pm, b_pm = pm_r.get()
                        for k in range(8):
                            kb.mm(pm[:], Wm[:, k, p * 128:(p + 1) * 128], hT[:, k, :], k == 0, k == 7,
                                  r=[b_Wm, b_hT], w=[b_pm])
                        qsb, b_qsb = qsb_r.get()
                        kb.cp("act", qsb[:], pm[:], r=[b_pm], w=[b_qsb])
                        kb.dma(qT_s[p, :, cs], qsb[:], r=[b_qsb], w=[b_qT_s])
                        qs.append((qsb, b_qsb))
                    Mbfs = []
                    for j in range(4 if "s" in PARTS else 0):
                        ti = 4 * c + j
                        qb = ti // 2
                        Mbf, b_Mbf = Mbf_r.get()
                        if qb < 4:
                            kb.ts("dve", Mbf[:], padneg[:, qb + 1, :], -BIG, None, ALU.max, r=[b_padneg], w=[b_Mbf])
                        else:
                            pg, b_pg = pg_r.get()
                            for p in range(4):
                                kb.mm(pg[:, p * 32:(p + 1) * 32], qs[p][0][:, j * 128:(j + 1) * 128], KMbd[:, p, :], True, True,
                                      r=[qs[p][1], b_KMbd], w=[b_pg])
                            Gs, b_Gs = Gs_r.get()
                            kb.tt("dve", Gs[:], pg[:, 0:128], padneg[:, qb, :], ALU.add, r=[b_pg, b_padneg], w=[b_Gs])
                            m8, b_m8 = m8_r.get()
                            for h in range(8):
                                kb.max8(m8[:, h, :], Gs[:, h * 16:(h + 1) * 16], r=[b_Gs], w=[b_m8])
                            G3 = Gs[:, :].rearrange("p (h n) -> p h n", n=16)
                            kb.tt("dve", G3, G3, m8[:, :, 2:3].to_broadcast([128, 8, 16]), ALU.is_ge, r=[b_Gs, b_m8], w=[b_Gs])
                            M3 = Mbf[:, :].rearrange("p (h n) -> p h n", n=16)
                            kb.ts("dve", M3, G3, -1.0, BIG, ALU.add, ALU.mult, r=[b_Gs], w=[b_Mbf])
                            kb.memset("dve", M3[:, :, qb:qb + 1], 0.0, r=[], w=[b_Mbf])
                        Mbfs.append((Mbf, b_Mbf))
                    nj = 4 if "v" in PARTS else 0
                    for j in range(nj):
                        ti = 4 * c + j
                        hs = slice(j * 128, (j + 1) * 128)
                        pm, b_pm = pm_r.get()
                        for k in range(8):
                            kb.mm(pm[:], hT[:, k, hs], Wm[:, k, 1024:1536], k == 0, k == 7, r=[b_Wm, b_hT], w=[b_pm])
                        vsb, b_vsb = vsb_r.get()
                        kb.cp("dve", vsb[:, :, 0:64], pm[:, :].rearrange("p (h d) -> p h d", d=64), r=[b_pm], w=[b_vsb])
                        kb.dma(v_s[ti, :, :], vsb[:, :, :].rearrange("p h d -> p (h d)"), r=[b_vsb], w=[b_v_s])
                    t4s = []
                    for j in range(nj):
                        ti = 4 * c + j
                        hs = slice(j * 128, (j + 1) * 128)
                        pm, b_pm = pm_r.get()
                        for k in range(8):
                            kb.mm(pm[:, 0:8], hT[:, k, hs], Wm[:, k, 3584:3592], k == 0, k == 7, r=[b_Wm, b_hT], w=[b_pm])
                        t4, b_t4 = t4_r.get()
                        kb.cp("dve", t4[:, 0:4], pm[:, 0:4], r=[b_pm], w=[b_t4])
                        kb.tt("dve", t4[:, 4:8], pm[:, 4:8], dtb_t[:], ALU.add, r=[b_pm, b_dtb], w=[b_t4])
                        t4s.append((t4, b_t4))
                    for j in range(nj):
                        t4, b_t4 = t4s[j]
                        kb.act(BG[:, 4 * c + j, 0:4], t4[:, 0:4], AF.Sigmoid, r=[b_t4], w=[b_BG])
                    for j in range(nj):
                        ti = 4 * c + j
                        hs = slice(j * 128, (j + 1) * 128)
                        pm, b_pm = pm_r.get()
                        for k in range(8):
                            kb.mm(pm[:], hT[:, k, hs], Wm[:, k, 3072:3584], k == 0, k == 7, r=[b_Wm, b_hT], w=[b_pm])
                        zsb, b_zsb = zsb_r.get()
                        kb.act(zsb[:], pm[:], AF.Silu, r=[b_pm], w=[b_zsb])
                        kb.dma(z_s[ti, :, :], zsb[:], r=[b_zsb], w=[b_z_s])
                    for j in range(nj):
                        t4, b_t4 = t4s[j]
                        kb.act(t4[:, 4:8], t4[:, 4:8], AF.Exp, r=[b_t4], w=[b_t4])
                    for j in range(nj):
                        t4, b_t4 = t4s[j]
                        kb.act(t4[:, 4:8], t4[:, 4:8], AF.Ln, r=[b_t4], w=[b_t4], bias=1.0)
                        kb.tt("dve", BG[:, 4 * c + j, 4:8], t4[:, 4:8], negA[:], ALU.mult, r=[b_t4, b_negA], w=[b_BG])
                    if "s" in PARTS:
                        pX, b_pX = pX_r.get()
                        for j in range(4):
                            kb.tr(pX[:, j, :], Mbfs[j][0][:], identb[:], r=[Mbfs[j][1], b_identb], w=[b_pX])
                        MTsb, b_MTsb = MTsb_r.get()
                        kb.cp("act", MTsb[:, :].rearrange("p (a b) -> p a b", b=128), pX[:, 0:4, :], r=[b_pX], w=[b_MTsb])
                        kb.dma(MT_s[:, cs], MTsb[:], r=[b_MTsb], w=[b_MT_s])
                    for g3 in range(3 if "g" in PARTS else 0):
                        cts = list(range(4 * g3, 4 * g3 + 4))
                        scs = {}

                        def gproj(ct):
                            pm, b_pm = pm_r.get()
                            for k in range(8):
                                kb.mm(pm[:], Wm[:, k, 1536 + ct * 128:1536 + (ct + 1) * 128], hT[:, k, :], k == 0, k == 7,
                                      r=[b_Wm, b_hT], w=[b_pm])
                            kb.cp("act", raw[:, ct, 3:515], pm[:], r=[b_pm], w=[b_rawc[ct]])

                        gproj(cts[0])
                        for ii, ct in enumerate(cts):
                            if ii + 1 < 4:
                                gproj(cts[ii + 1])
                            brc = b_rawc[ct]
                            acc, b_acc = pm_r.get()
                            for jj in range(4):
                                kb.mm(acc[:], Dg[:, ct, jj, :], raw[:, ct, jj:jj + 512], jj == 0, jj == 3, r=[brc, b_Dg], w=[b_acc])
                            kb.cp("dve", raw[:, ct, 0:3], raw[:, ct, 512:515], r=[brc], w=[brc])
                            sc, b_sc = sc_r.get()
                            kb.act(sc[:], acc[:], AF.Silu, r=[b_acc], w=[b_sc])
                            scs[ct] = (sc, b_sc)
                        nrms = {}
                        if g3 < 2:
                            sqs, pns = {}, {}
                            for ct in cts:
                                sq, b_sq = sq_r.get()
                                kb.act(sq[:], scs[ct][0][:], AF.Square, r=[scs[ct][1]], w=[b_sq])
                                sqs[ct] = (sq, b_sq)
                            for ct in cts:
                                pn, b_pn = pm_r.get()
                                kb.mm(pn[:], onesb[:], sqs[ct][0][:], True, True, r=[b_onesb, sqs[ct][1]], w=[b_pn])
                                pns[ct] = (pn, b_pn)
                            for ct in cts:
                                sc, b_sc = scs[ct]
                                pn, b_pn = pns[ct]
                                rr, b_rr = rr_r.get()
                                kb.act(rr[:], pn[:], AF.Sqrt, r=[b_pn, b_epsc], w=[b_rr], bias=epsc[:, 0:1])
                                kb.recip(rr[:], rr[:], r=[b_rr], w=[b_rr])
                                nrm, b_nrm = nrm_r.get()
                                if ct < 4:
                                    kb.stt("dve", nrm[:], sc[:], 128.0 ** -0.5, rr[:], ALU.mult, ALU.mult, r=[b_sc, b_rr], w=[b_nrm])
                                    kb.dma(qTb_s[ct % 4, :, cs], nrm[:], r=[b_nrm], w=[b_qTb_s])
                                else:
                                    kb.tt("dve", nrm[:], sc[:], rr[:], ALU.mult, r=[b_sc, b_rr], w=[b_nrm])
                                    kb.dma(kTb_s[ct % 4, :, cs], nrm[:], r=[b_nrm], w=[b_kTb_s])
                                nrms[ct] = (nrm, b_nrm)
                        else:
                            for ct in cts:
                                nrm, b_nrm = nrm_r.get()
                                kb.cp("dve", nrm[:], scs[ct][0][:], r=[scs[ct][1]], w=[b_nrm])
                                nrms[ct] = (nrm, b_nrm)
                        if g3 >= 1:
                            for ct in cts:
                                nrm, b_nrm = nrms[ct]
                                head = ct % 4
                                pX, b_pX = pX_r.get()
                                for j in range(4):
                                    kb.tr(pX[:, j, :], nrm[:, j * 128:(j + 1) * 128], identb[:], r=[b_nrm, b_identb], w=[b_pX])
                                tok, b_tok = tok_r.get()
                                kb.cp("act", tok[:], pX[:, 0:4, :], r=[b_pX], w=[b_tok])
                                dst = kb_s if ct < 8 else vb_s
                                bdst = b_kb_s if ct < 8 else b_vb_s
                                kb.dma(dst[4 * c:4 * c + 4, :, head * 128:(head + 1) * 128].rearrange("j t d -> t j d"), tok[:],
                                       r=[b_tok], w=[bdst])
                if dbg and li == 0:
                    kb.dma(dbg_bg[:, :, :], BG[:], r=[b_BG], w=[Buf()])
                S.barrier()

            if li == 0 and stop_after == "A":
                return finish(nc, kb, top, out, b_out)

            with ExitStack() as ph:
                EB, b_EB = kb.sb(ph, [128, 8, 1024], BF16, "EB")
                nb31, b_nb31 = kb.sb(ph, [128, 8], F32, "nb31")
                negm_t, b_negm = kb.sb(ph, [128, 1024], F32, "negm")
                kb.dma(nb31[:], b31[:, :], w=[b_nb31])
                kb.ts("dve", nb31[:], nb31[:], -1.0, None, ALU.mult, r=[b_nb31], w=[b_nb31])
                kb.dma(negm_t[:], negm[:, :], w=[b_negm])
                with ExitStack() as ph2:
                    br_r = kb.ring_sb(ph2, 2, [128, 1024], F32, "braw")
                    for h in range(8):
                        brt, b_brt = br_r.get()
                        kb.dma(brt[:], braw[h, :, :], w=[b_brt])
                        kb.tt("dve", brt[:], brt[:], negm_t[:], ALU.add, r=[b_brt, b_negm], w=[b_brt])
                        kb.act(EB[:, h, :], brt[:], AF.Exp, r=[b_brt, b_nb31], w=[b_EB], bias=nb31[:, h:h + 1])
                    S.barrier()
                qa_r = kb.ring_sb(ph, 2, [128, T], BF16, "qaug")
                ka_r = kb.ring_sb(ph, 2, [128, T], BF16, "kaug")
                for (t_, b_) in qa_r.items + ka_r.items:
                    kb.memset("pool", t_[64:128, :], 0.0, w=[b_])
                vh_r = kb.ring_sb(ph, 2, [128, NT, 65], BF16, "vh")
                for (kt_, bk_) in ka_r.items:
                    kb.dma(kt_[64:80, :], onehotK[:, :], w=[bk_])
                pS_r = kb.ring_ps(ph, 4, [128, 512], F32, "pS")
                pO_r = kb.ring_ps(ph, 2, [128, 4, 128], F32, "pO")
                PT_r = kb.ring_sb(ph, 4, [128, 512], BF16, "PT")
                rec_r = kb.ring_sb(ph, 2, [128, 4, 1], F32, "rec")
                ya_r = kb.ring_sb(ph, 2, [128, 4, 64], BF16, "yat")
                for h in range(8):
                    p, hh = h // 2, h % 2
                    qa_t, b_qa = qa_r.get()
                    ka_t, b_ka = ka_r.get()
                    vh, b_vh = vh_r.get()
                    kb.dma(qa_t[0:64, :], qT_s[p, hh * 64:(hh + 1) * 64, :], r=[b_qT_s], w=[b_qa])
                    kb.dma(qa_t[64:80, :], MT_s[h * 16:(h + 1) * 16, :], r=[b_MT_s], w=[b_qa])
                    kb.dma(ka_t[0:64, :], kT_s[p, hh * 64:(hh + 1) * 64, :], r=[b_kT_s], w=[b_ka])
                    kb.dma(vh[:], v_s[:, :, h * 65:(h + 1) * 65].rearrange("n t d -> t n d"), r=[b_v_s], w=[b_vh])
                    for c in range(8):
                        cs = slice(c * 512, (c + 1) * 512)
                        pO, b_pO = pO_r.get()
                        nk = 4 * c + 4
                        def qk(kt_):
                            pS_, b_pS_ = pS_r.get()
                            kb.mm(pS_[:], ka_t[:, kt_ * 128:(kt_ + 1) * 128], qa_t[:, cs], True, True, r=[b_ka, b_qa], w=[b_pS_])
                            return pS_, b_pS_
                        nxt_qk = [qk(0), qk(1)]
                        for kt in range(nk):
                            pS, b_pS = nxt_qk.pop(0)
                            if kt + 2 < nk:
                                nxt_qk.append(qk(kt + 2))
                            PT, b_PT = PT_r.get()
                            kb.act(PT[:], pS[:], AF.Exp, r=[b_pS], w=[b_PT], scale=0.125)
                            if kt >= 4 * c - 1:
                                off = 512 * c - 128 * kt + 384
                                kb.tt("dve", PT[:], PT[:], EB[:, h, off:off + 512], ALU.mult, r=[b_PT, b_EB], w=[b_PT])
                            for j in range(4):
                                kb.mm(pO[:, j, 0:65], PT[:, j * 128:(j + 1) * 128], vh[:, kt, :],
                                      (kt == 0 and j == 0), (kt == nk - 1), r=[b_PT, b_vh], w=[b_pO])
                        rec, b_rec = rec_r.get()
                        kb.recip(rec[:], pO[:, :, 64:65], r=[b_pO], w=[b_rec])
                        yat, b_yat = ya_r.get()
                        kb.tt("dve", yat[:], pO[:, :, 0:64], rec[:, :, :].to_broadcast([128, 4, 64]), ALU.mult,
                              r=[b_pO, b_rec], w=[b_yat])
                        kb.dma(ya_s[cs, h * 64:(h + 1) * 64].rearrange("(j t) d -> t j d", t=128), yat[:],
                               r=[b_yat], w=[b_ya_s])
                S.barrier()

            if li == 0 and stop_after == "C":
                return finish(nc, kb, top, out, b_out)

            with ExitStack() as ph:
                mneg, b_mneg = kb.sb(ph, [128, 128], F32, "mneg")
                strict, b_strict = kb.sb(ph, [128, 128], F32, "strict")
                ut, b_ut = kb.sb(ph, [128, 128], F32, "ut")
                onw_t, b_onw = kb.sb(ph, [128, 512], F32, "onw")
                kb.dma(mneg[:], mneg_d[:, :], w=[b_mneg])
                kb.dma(strict[:], strict_d[:, :], w=[b_strict])
                kb.dma(ut[:], ut_d[:, :], w=[b_ut])
                kb.dma(onw_t[:], onw[:, :], w=[b_onw])
                Sf, b_Sf = kb.sb(ph, [128, 4, 128], F32, "Sf")
                Sb, b_Sb = kb.sb(ph, [128, 4, 128], BF16, "Sb")
                kb.memset("pool", Sf[:], 0.0, w=[b_Sf])
                kb.memset("pool", Sb[:], 0.0, w=[b_Sb])
                pF_r = kb.ring_ps(ph, 6, [128, 4, 128], F32, "pF")
                pB_r = kb.ring_ps(ph, 2, [128, 8, 128], BF16, "pB")
                R2 = lambda shape, dt, nm, n=2: kb.ring_sb(ph, n, shape, dt, nm)
                kT_r = R2([128, 4, 128], BF16, "gkT", 3)
                qT_r = R2([128, 4, 128], BF16, "gqT", 6)
                ktok_r = R2([128, 4, 128], BF16, "gktok", 3)
                vtok_r = R2([128, 4, 128], BF16, "gvtok", 3)
                z_r = R2([128, 512], F32, "gz", 6)
                gB_r = R2([128, 4, 128], F32, "gB", 3)
                gBn_r = R2([128, 4, 128], F32, "gBn", 3)
                Gcl_r = R2([128, 8], F32, "Gcl", 3)
                eGl_r = R2([128, 8], F32, "eGl", 6)
                f12_r = R2([128, 8], F32, "f12", 3)
                dec_r = R2([128, 4, 128], F32, "dec", 3)
                decS_r = R2([128, 4, 128], F32, "decS", 3)
                tmpL_r = R2([128, 4, 128], F32, "tmpL", 3)
                L_r = R2([128, 4, 128], BF16, "L", 3)
                P_r = R2([128, 4, 128], BF16, "P", 3)
                LP_r = R2([128, 8, 128], BF16, "LP", 6)
                gt_r = R2([128, 4, 128], BF16, "gt", 56)
                bmask, b_bmask = kb.sb(ph, [128, 5, 128], BF16, "bmask")
                kb.dma(bmask[:], bmask_d[:, :, :], w=[b_bmask])
                vb_r = R2([128, 4, 128], BF16, "vb", 3)
                kbg_r = R2([128, 4, 128], BF16, "kbg", 3)
                kd_r = R2([128, 4, 128], BF16, "kd", 6)
                u_r = R2([128, 4, 128], F32, "u", 6)
                wT_r = R2([128, 4, 128], BF16, "wT", 6)
                vn_r = R2([128, 4, 128], BF16, "vn")
                o1_r = R2([128, 4, 128], F32, "o1")
                o_r = R2([128, 4, 128], F32, "o")
                sq_r = R2([128, 4, 128], F32, "osq")
                ss_r = R2([128, 4], F32, "oss")
                yb_r = R2([128, 512], BF16, "ybt")
                yf_r = R2([128, 4, 128], F32, "yf")

                def bc4(ap):
                    return ap.unsqueeze(2).to_broadcast([128, 4, 128])

                def bcm(ap):
                    return ap.unsqueeze(1).to_broadcast([128, 4, 128])

                def act4(dst, src, col, r, w):
                    for h in range(4):
                        kb.act(dst[:, h, :], src[:, h, :], AF.Copy, r=r, w=w, scale=col[:, h:h + 1])

                def mm4(pt, b_pt, lhs, b_lhs, rhs, b_rhs, first=True):
                    for h in range(4):
                        kb.mm(pt[:, h, :], lhs[:, h, :], rhs[:, h, :], first and h == 0, True, r=[b_lhs, b_rhs], w=[b_pt])

                def prep(n):
                    ts_ = slice(n * 128, (n + 1) * 128)
                    kT, b_kT = kT_r.get()
                    qT, b_qT = qT_r.get()
                    ktok, b_ktok = ktok_r.get()
                    vtok, b_vtok = vtok_r.get()
                    z, b_z = z_r.get()
                    kb.dma(kT[:], kTb_s[:, :, ts_].rearrange("h d t -> d h t"), r=[b_kTb_s], w=[b_kT])
                    kb.dma(qT[:], qTb_s[:, :, ts_].rearrange("h d t -> d h t"), r=[b_qTb_s], w=[b_qT])
                    kb.dma(ktok[:, :, :].rearrange("p h d -> p (h d)"), kb_s[n, :, :], r=[b_kb_s], w=[b_ktok])
                    kb.dma(vtok[:, :, :].rearrange("p h d -> p (h d)"), vb_s[n, :, :], r=[b_vb_s], w=[b_vtok])
                    kb.dma(z[:], z_s[n, :, :], r=[b_z_s], w=[b_z])
                    beta = BG[:, n, 0:4]
                    g = BG[:, n, 4:8]
                    gB, b_gB = gB_r.get()
                    gBn, b_gBn = gBn_r.get()
                    kb.tt("pool", gB[:], bcm(onesf[:, :]), bc4(g), ALU.mult, r=[b_onesf, b_BG], w=[b_gB])
                    kb.ts("pool", gBn[:], gB[:], -1.0, None, ALU.mult, r=[b_gB], w=[b_gBn])
                    pG, b_pG = pF_r.get()
                    for h in range(4):
                        kb.mm(pG[:, h, :], ut[:], gB[:, h, :], h == 0, False, r=[b_ut, b_gB], w=[b_pG])
                        kb.mm(pG[:, h, :], gBn[:, h, :], ut[:], False, True, r=[b_ut, b_gBn], w=[b_pG])
                    pC, b_pC = pF_r.get()
                    pCv = pC[:, :, :].rearrange("p a b -> p (a b)")
                    kb.mm(pCv[:, 0:4], ut[:], g, True, True, r=[b_ut, b_BG], w=[b_pC])
                    kb.mm(pCv[:, 4:8], onesf[:], g, False, True, r=[b_onesf, b_BG], w=[b_pC])
                    Gcl, b_Gcl = Gcl_r.get()
                    kb.cp("dve", Gcl[:], pCv[:, 0:8], r=[b_pC], w=[b_Gcl])
                    eGl, b_eGl = eGl_r.get()
                    kb.act(eGl[:], Gcl[:], AF.Exp, r=[b_Gcl], w=[b_eGl])
                    f12, b_f12 = f12_r.get()
                    kb.tt("dve", f12[:, 0:4], beta, eGl[:, 0:4], ALU.mult, r=[b_BG, b_eGl], w=[b_f12])
                    kb.tt("dve", f12[:, 4:8], Gcl[:, 4:8], Gcl[:, 0:4], ALU.subtract, r=[b_Gcl], w=[b_f12])
                    kb.act(f12[:, 4:8], f12[:, 4:8], AF.Exp, r=[b_f12], w=[b_f12])
                    dec, b_dec = dec_r.get()
                    kb.tt("dve", dec[:], pG[:], bcm(mneg[:, :]), ALU.add, r=[b_pG, b_mneg], w=[b_dec])
                    kb.act(dec[:], dec[:], AF.Exp, r=[b_dec], w=[b_dec])
                    yield
                    decS, b_decS = decS_r.get()
                    kb.tt("dve", decS[:], dec[:], bcm(strict[:, :]), ALU.mult, r=[b_dec, b_strict], w=[b_decS])
                    pKK, b_pKK = pF_r.get()
                    mm4(pKK, b_pKK, kT, b_kT, kT, b_kT)
                    tmpL, b_tmpL = tmpL_r.get()
                    kb.tt("dve", tmpL[:], pKK[:], decS[:], ALU.mult, r=[b_pKK, b_decS], w=[b_tmpL])
                    L, b_L = L_r.get()
                    act4(L, tmpL, beta, [b_tmpL, b_BG], [b_L])
                    yield
                    pQK, b_pQK = pF_r.get()
                    mm4(pQK, b_pQK, qT, b_qT, kT, b_kT)
                    P, b_P = P_r.get()
                    kb.tt("dve", P[:], pQK[:], dec[:], ALU.mult, r=[b_pQK, b_dec], w=[b_P])
                    yield
                    pX, b_pX = pB_r.get()
                    for h in range(4):
                        kb.tr(pX[:, h, :], L[:, h, :], identb[:], r=[b_L, b_identb], w=[b_pX])
                    for h in range(4):
                        kb.tr(pX[:, 4 + h, :], P[:, h, :], identb[:], r=[b_P, b_identb], w=[b_pX])
                    LP, b_LP = LP_r.get()
                    kb.cp("act", LP[:], pX[:], r=[b_pX], w=[b_LP])
                    yield
                    LT = LP[:, 0:4, :]
                    PT = LP[:, 4:8, :]
                    cnt = [0]

                    def evac(dst, src_ps, b_src, b_dst, add=None, b_add=None, sub=False):
                        cnt[0] += 1
                        if add is None:
                            kb.cp("act" if cnt[0] % 2 else "dve", dst, src_ps, r=[b_src], w=[b_dst])
                        else:
                            kb.tt("dve", dst, add, src_ps, ALU.subtract if sub else ALU.add, r=[b_src, b_add], w=[b_dst])

                    def newt():
                        return gt_r.get()

                    L8, b_L8 = newt()
                    L8T, b_L8T = newt()
                    kb.tt("pool", L8[:], L[:], bcm(bmask[:, 0, :]), ALU.mult, r=[b_L, b_bmask], w=[b_L8])
                    kb.tt("pool", L8T[:], LT, bcm(bmask[:, 0, :]), ALU.mult, r=[b_LP, b_bmask], w=[b_L8T])
                    T0, b_T0 = newt()
                    T0T, b_T0T = newt()
                    kb.tt("pool", T0[:], bcm(identb[:, :]), L8[:], ALU.subtract, r=[b_identb, b_L8], w=[b_T0])
                    kb.tt("pool", T0T[:], bcm(identb[:, :]), L8T[:], ALU.subtract, r=[b_identb, b_L8T], w=[b_T0T])
                    def mk_E(lv):
                        E, b_E = newt()
                        ET, b_ET = newt()
                        kb.tt("pool", E[:], L[:], bcm(bmask[:, 1 + lv, :]), ALU.mult, r=[b_L, b_bmask], w=[b_E])
                        kb.tt("pool", ET[:], LT, bcm(bmask[:, 1 + lv, :]), ALU.mult, r=[b_LP, b_bmask], w=[b_ET])
                        return (E, b_E, ET, b_ET)
                    pM, b_pM = pF_r.get()
                    mm4(pM, b_pM, L8T, b_L8T, L8, b_L8)
                    M1, b_M1 = newt()
                    evac(M1[:], pM[:], b_pM, b_M1)
                    yield
                    pM, b_pM = pF_r.get()
                    mm4(pM, b_pM, L8, b_L8, L8T, b_L8T)
                    M1T, b_M1T = newt()
                    evac(M1T[:], pM[:], b_pM, b_M1T)
                    yield
                    pM, b_pM = pF_r.get()
                    mm4(pM, b_pM, T0T, b_T0T, M1, b_M1)
                    T1, b_T1 = newt()
                    evac(T1[:], pM[:], b_pM, b_T1, add=T0[:], b_add=b_T0)
                    yield
                    pM, b_pM = pF_r.get()
                    mm4(pM, b_pM, M1, b_M1, T0T, b_T0T)
                    T1T, b_T1T = newt()
                    evac(T1T[:], pM[:], b_pM, b_T1T, add=T0T[:], b_add=b_T0T)
                    yield
                    pM, b_pM = pF_r.get()
                    mm4(pM, b_pM, M1T, b_M1T, M1, b_M1)
                    M2, b_M2 = newt()
                    evac(M2[:], pM[:], b_pM, b_M2)
                    yield
                    pM, b_pM = pF_r.get()
                    mm4(pM, b_pM, T1T, b_T1T, M2, b_M2)
                    Tb, b_Tb = newt()
                    evac(Tb[:], pM[:], b_pM, b_Tb, add=T1[:], b_add=b_T1)
                    yield
                    pM, b_pM = pF_r.get()
                    mm4(pM, b_pM, M2, b_M2, T1T, b_T1T)
                    TbT, b_TbT = newt()
                    evac(TbT[:], pM[:], b_pM, b_TbT, add=T1T[:], b_add=b_T1T)
                    yield
                    nxtE = mk_E(0)
                    for lv in range(4):
                        E, b_E, ET, b_ET = nxtE
                        if lv < 3:
                            nxtE = mk_E(lv + 1)
                        pM, b_pM = pF_r.get()
                        mm4(pM, b_pM, E, b_E, TbT, b_TbT)
                        W1, b_W1 = newt()
                        evac(W1[:], pM[:], b_pM, b_W1)
                        yield
                        if lv < 3:
                            pV, b_pV = pF_r.get()
                            mm4(pV, b_pV, ET, b_ET, Tb, b_Tb)
                            V1, b_V1 = newt()
                            evac(V1[:], pV[:], b_pV, b_V1)
                            yield
                        pM, b_pM = pF_r.get()
                        mm4(pM, b_pM, Tb, b_Tb, W1, b_W1)
                        TnT, b_TnT = newt()
                        evac(TnT[:], pM[:], b_pM, b_TnT, add=TbT[:], b_add=b_TbT, sub=True)
                        yield
                        if lv < 3:
                            pV, b_pV = pF_r.get()
                            mm4(pV, b_pV, TbT, b_TbT, V1, b_V1)
                            Tn, b_Tn = newt()
                            evac(Tn[:], pV[:], b_pV, b_Tn, add=Tb[:], b_add=b_Tb, sub=True)
                            yield
                            Tb, b_Tb = Tn, b_Tn
                        TbT, b_TbT = TnT, b_TnT
                    Tt, b_Tt = TbT, b_TbT
                    vb, b_vb = vb_r.get()
                    kbg, b_kbg = kbg_r.get()
                    kd, b_kd = kd_r.get()
                    act4(vb, vtok, beta, [b_vtok, b_BG], [b_vb])
                    act4(kbg, ktok, f12[:, 0:4], [b_ktok, b_f12], [b_kbg])
                    act4(kd, ktok, f12[:, 4:8], [b_ktok, b_f12], [b_kd])
                    pu, b_pu = pF_r.get()
                    mm4(pu, b_pu, Tt, b_Tt, vb, b_vb)
                    u, b_u = u_r.get()
                    kb.cp("act", u[:], pu[:], r=[b_pu], w=[b_u])
                    pw, b_pw = pF_r.get()
                    mm4(pw, b_pw, kbg, b_kbg, Tt, b_Tt)
                    wT, b_wT = wT_r.get()
                    kb.cp("dve", wT[:], pw[:], r=[b_pw], w=[b_wT])
                    return dict(n=n, qT=(qT, b_qT), PT=(PT, b_LP), u=(u, b_u), wT=(wT, b_wT), kd=(kd, b_kd),
                                eGl=(eGl, b_eGl), z=(z, b_z))

                def scan(st_):
                    n = st_["n"]
                    qT, b_qT = st_["qT"]
                    PT, b_PT = st_["PT"]
                    u, b_u = st_["u"]
                    wT, b_wT = st_["wT"]
                    kd, b_kd = st_["kd"]
                    eGl, b_eGl = st_["eGl"]
                    z, b_z = st_["z"]
                    pwS, b_pwS = pF_r.get()
                    mm4(pwS, b_pwS, wT, b_wT, Sb, b_Sb)
                    vn, b_vn = vn_r.get()
                    kb.tt("dve", vn[:], u[:], pwS[:], ALU.subtract, r=[b_u, b_pwS], w=[b_vn])
                    yield
                    pA1, b_pA1 = pF_r.get()
                    mm4(pA1, b_pA1, qT, b_qT, Sb, b_Sb)
                    o1, b_o1 = o1_r.get()
                    kb.tt("dve", o1[:], pA1[:], bc4(eGl[:, 0:4]), ALU.mult, r=[b_pA1, b_eGl], w=[b_o1])
                    pA2, b_pA2 = pF_r.get()
                    mm4(pA2, b_pA2, PT, b_PT, vn, b_vn)
                    o, b_o = o_r.get()
                    kb.tt("dve", o[:], pA2[:], o1[:], ALU.add, r=[b_pA2, b_o1], w=[b_o])
                    yield
                    pSn, b_pSn = pF_r.get()
                    mm4(pSn, b_pSn, kd, b_kd, vn, b_vn)
                    act4(Sf, Sf, eGl[:, 4:8], [b_Sf, b_eGl, b_Sb], [b_Sf])
                    kb.tt("dve", Sf[:], pSn[:], Sf[:], ALU.add, r=[b_pSn, b_Sf], w=[b_Sf])
                    kb.cp("act", Sb[:], Sf[:], r=[b_Sf], w=[b_Sb])
                    yield
                    sq, b_sq = sq_r.get()
                    kb.act(sq[:], o[:], AF.Square, r=[b_o], w=[b_sq])
                    ss, b_ss = ss_r.get()
                    kb.red("dve", ss[:], sq[:], ALU.add, r=[b_sq], w=[b_ss])
                    kb.ts("dve", ss[:], ss[:], 1.0 / 128, EPS, ALU.mult, ALU.add, r=[b_ss], w=[b_ss])
                    kb.act(ss[:], ss[:], AF.Sqrt, r=[b_ss], w=[b_ss])
                    kb.recip(ss[:], ss[:], r=[b_ss], w=[b_ss])
                    yield
                    yf, b_yf = yf_r.get()
                    kb.tt("dve", yf[:], o[:], bc4(ss[:, :]), ALU.mult, r=[b_o, b_ss], w=[b_yf])
                    yfv = yf[:, :, :].rearrange("p h d -> p (h d)")
                    kb.tt("dve", yfv, yfv, onw_t[:], ALU.mult, r=[b_yf, b_onw], w=[b_yf])
                    ybt, b_ybt = yb_r.get()
                    kb.tt("dve", ybt[:], yfv, z[:], ALU.mult, r=[b_yf, b_z], w=[b_ybt])
                    kb.dma(yb_s[n * 128:(n + 1) * 128, :], ybt[:], r=[b_ybt], w=[b_yb_s])

                def run_rr(gens):
                    results = [None] * len(gens)
                    active = list(range(len(gens)))
                    while active:
                        for gi in list(active):
                            try:
                                next(gens[gi])
                            except StopIteration as ex:
                                results[gi] = ex.value
                                active.remove(gi)
                    return results

                def scan_pair(sa, sb2):
                    yield from scan(sa)
                    yield from scan(sb2)

                WPAR, MAXAHEAD = 3, 5
                next_chunk, next_scan = 0, 0
                preps, done_st, scan_gen = [], {}, None
                while next_scan < NT:
                    while len(preps) < WPAR and next_chunk < NT and (next_chunk - next_scan) < MAXAHEAD:
                        preps.append((next_chunk, prep(next_chunk)))
                        next_chunk += 1
                    for (n_, g_) in list(preps):
                        try:
                            next(g_)
                        except StopIteration as ex:
                            done_st[n_] = ex.value
                            preps.remove((n_, g_))
                    if scan_gen is None and next_scan in done_st:
                        scan_gen = scan(done_st.pop(next_scan))
                    if scan_gen is not None:
                        try:
                            next(scan_gen)
                        except StopIteration:
                            scan_gen = None
                            next_scan += 1
                S.barrier()

            if li == 0 and stop_after == "D":
                return finish(nc, kb, top, out, b_out)

            def make_norm(ph):
                junk_r = kb.ring_sb(ph, 1, [128, D], BF16, "njunk")
                xn_r = kb.ring_sb(ph, 2, [128, D], BF16, "nxn")
                st_r = kb.ring_sb(ph, 4, [128, 2], F32, "nst")
                pT_r = kb.ring_ps(ph, 2, [128, 8, 128], BF16, "npT")

                def norm_tile(xt_ap, b_xt, ai, hv, b_hT):
                    junk, b_junk = junk_r.get()
                    st, b_st = st_r.get()
                    kb.act(junk[:], xt_ap, AF.Square, r=[b_xt], w=[b_junk, b_st], accum_out=st[:, 0:1])
                    kb.ts("dve", st[:, 1:2], st[:, 0:1], 1.0 / D, EPS, ALU.mult, ALU.add, r=[b_st], w=[b_st])
                    kb.act(st[:, 1:2], st[:, 1:2], AF.Sqrt, r=[b_st], w=[b_st])
                    kb.recip(st[:, 1:2], st[:, 1:2], r=[b_st], w=[b_st])
                    xn, b_xn = xn_r.get()
                    kb.act(xn[:], xt_ap, AF.Copy, r=[b_xt, b_st], w=[b_xn], scale=st[:, 1:2])
                    pT, b_pT = pT_r.get()
                    for k in range(8):
                        kb.tr(pT[:, k, :], xn[:, k * 128:(k + 1) * 128], identb[:], r=[b_xn, b_identb], w=[b_pT])
                    kb.tt("dve", hv, pT[:], AB[:, ai, :].unsqueeze(2).to_broadcast([128, 8, 128]), ALU.mult,
                          r=[b_pT, b_AB], w=[b_hT])
                    kb.tt("pool", hv, hv, AB[:, ai + 1, :].unsqueeze(2).to_broadcast([128, 8, 128]), ALU.add,
                          r=[b_hT, b_AB], w=[b_hT])
                return norm_tile

            for hf in ([None] if last else [0, 1]):
                with ExitStack() as phEF:
                    x1, _ = kb.sb(phEF, [128, 16, D], F32, "x1")
                    b_x1 = [Buf() for _ in range(16)]

                    with ExitStack() as ph:
                        Wua, b_Wua = kb.sb(ph, [128, 4, D], BF16, "Wua")
                        Wub, b_Wub = kb.sb(ph, [128, 4, D], BF16, "Wub")
                        Wg, b_Wg = kb.sb(ph, [128, 8, 2 * D], BF16, "Wg")
                        Wo, b_Wo = kb.sb(ph, [128, 8, D], BF16, "Wo")
                        with ExitStack() as ph2:
                            stg = kb.ring_sb(ph2, 2, [128, 8, 512], F32)
                            wuav = w_up_a.rearrange("(k p) n -> p k n", p=128)
                            wubv = w_up_b.rearrange("(k p) n -> p k n", p=128)
                            wov = w_out.rearrange("(k p) n -> p k n", p=128)
                            i = 0
                            for (dst, bd, src, nk, ncol, c0s) in ((Wua, b_Wua, wuav, 4, 2, 0), (Wub, b_Wub, wubv, 4, 2, 0),
                                                                  (Wg, b_Wg, wiv, 8, 4, 3592), (Wo, b_Wo, wov, 8, 2, 0)):
                                for g in range(ncol):
                                    st_t, b_st = stg.get()
                                    kb.dma(st_t[:, 0:nk, :], src[:, :, c0s + g * 512:c0s + (g + 1) * 512], w=[b_st])
                                    kb.cp("pool" if i % 2 else "dve", dst[:, :, g * 512:(g + 1) * 512], st_t[:, 0:nk, :],
                                          r=[b_st], w=[bd])
                                    i += 1
                            S.barrier()
                        norm_tile = make_norm(ph)
                        hoT_r = kb.ring_sb(ph, 1, [128, 8, 512], BF16, "hoT")
                        yaT_r = kb.ring_sb(ph, 1, [128, 4, 512], BF16, "yaT")
                        ybT_r = kb.ring_sb(ph, 1, [128, 4, 512], BF16, "ybT")
                        yl_r = kb.ring_sb(ph, 4, [128, 512], BF16, "yl")
                        yo_r = kb.ring_sb(ph, 2, [128, 512], BF16, "yo")
                        xl_r = kb.ring_sb(ph, 1, [128, D], F32, "xl")
                        pX_r = kb.ring_ps(ph, 1, [128, 8, 128], BF16, "epX")
                        p5_r = kb.ring_ps(ph, 5, [128, 512], F32, "ep5")
                        sg_r = kb.ring_sb(ph, 2, [128, 512], F32, "esg")
                        m12_r = kb.ring_sb(ph, 2, [128, 512], F32, "em12")
                        mT_r = kb.ring_sb(ph, 1, [128, 8, 512], BF16, "mT")
                        tmo_r = kb.ring_sb(ph, 2, [128, 512], F32, "tmo")
                        for c in range(4):
                            hoT, b_hoT = hoT_r.get()
                            yaT, b_yaT = yaT_r.get()
                            ybT, b_ybT = ybT_r.get()
                            for j in range(4):
                                ti = 4 * c + j
                                if hf is not None:
                                    r0 = hf * TO + ti * 128
                                    kb.dma(x1[:, ti, :], xsrc_full[r0:r0 + 128, :], r=[b_xfull_s], w=[b_x1[ti]])
                                elif li == 0:
                                    kb.dma(x1[:, ti, :], xo[ti * 128:(ti + 1) * 128, :], w=[b_x1[ti]])
                                else:
                                    xl, b_xl = xl_r.get()
                                    kb.dma(x1[:, ti, :], xfull_s[ti * 128:(ti + 1) * 128, :], r=[b_xfull_s], w=[b_x1[ti]])
                                    kb.dma(xl[:], xfull_s[TO + ti * 128:TO + (ti + 1) * 128, :], r=[b_xfull_s], w=[b_xl])
                                    kb.ts("dve", x1[:, ti, :], x1[:, ti, :], selt[:, 0:1], None, ALU.mult, r=[b_x1[ti], b_selt], w=[b_x1[ti]])
                                    kb.stt("dve", x1[:, ti, :], xl[:], selt[:, 1:2], x1[:, ti, :], ALU.mult, ALU.add,
                                           r=[b_xl, b_selt, b_x1[ti]], w=[b_x1[ti]])
                                norm_tile(x1[:, ti, :], b_x1[ti], 0, hoT[:, :, j * 128:(j + 1) * 128], b_hoT)
                                for (src, bsrc, dstT, bdst) in ((ya_s, b_ya_s, yaT, b_yaT), (yb_s, b_yb_s, ybT, b_ybT)):
                                    if hf is not None:
                                        yo, b_yo = yl_r.get()
                                        r0 = hf * TO + ti * 128
                                        kb.dma(yo[:], src[r0:r0 + 128, :], r=[bsrc], w=[b_yo])
                                    else:
                                        la, b_la = yl_r.get()
                                        lb, b_lb = yl_r.get()
                                        kb.dma(la[:], src[ti * 128:(ti + 1) * 128, :], r=[bsrc], w=[b_la])
                                        kb.dma(lb[:], src[TO + ti * 128:TO + (ti + 1) * 128, :], r=[bsrc], w=[b_lb])
                                        yo, b_yo = yo_r.get()
                                        kb.ts("dve", yo[:], la[:], selt[:, 0:1], None, ALU.mult, r=[b_la, b_selt], w=[b_yo])
                                        kb.stt("dve", yo[:], lb[:], selt[:, 1:2], yo[:], ALU.mult, ALU.add, r=[b_lb, b_selt, b_yo], w=[b_yo])
                                    pX, b_pX = pX_r.get()
                                    for kc in range(4):
                                        kb.tr(pX[:, kc, :], yo[:, kc * 128:(kc + 1) * 128], identb[:], r=[b_yo, b_identb], w=[b_pX])
                                    kb.cp("act", dstT[:, :, j * 128:(j + 1) * 128], pX[:, 0:4, :], r=[b_pX], w=[bdst])
                            mT, b_mT = mT_r.get()
                            for f in range(8):
                                fs = slice(f * 128, (f + 1) * 128)
                                pUa, b_pUa = p5_r.get()
                                for kc in range(4):
                                    kb.mm(pUa[:], Wua[:, kc, fs], yaT[:, kc, :], kc == 0, kc == 3, r=[b_Wua, b_yaT], w=[b_pUa])
                                pga, b_pga = p5_r.get()
                                for k in range(8):
                                    kb.mm(pga[:], Wg[:, k, fs], hoT[:, k, :], k == 0, k == 7, r=[b_Wg, b_hoT], w=[b_pga])
                                sa, b_sa = sg_r.get()
                                kb.act(sa[:], pga[:], AF.Sigmoid, r=[b_pga], w=[b_sa])
                                m1, b_m1 = m12_r.get()
                                kb.tt("dve", m1[:], pUa[:], sa[:], ALU.mult, r=[b_pUa, b_sa], w=[b_m1])
                                pUb, b_pUb = p5_r.get()
                                for kc in range(4):
                                    kb.mm(pUb[:], Wub[:, kc, fs], ybT[:, kc, :], kc == 0, kc == 3, r=[b_Wub, b_ybT], w=[b_pUb])
                                pgb, b_pgb = p5_r.get()
                                for k in range(8):
                                    kb.mm(pgb[:], Wg[:, k, D + f * 128:D + (f + 1) * 128], hoT[:, k, :], k == 0, k == 7,
                                          r=[b_Wg, b_hoT], w=[b_pgb])
                                sb_, b_sb_ = sg_r.get()
                                kb.act(sb_[:], pgb[:], AF.Sigmoid, r=[b_pgb], w=[b_sb_])
                                m2, b_m2 = m12_r.get()
                                kb.tt("dve", m2[:], pUb[:], sb_[:], ALU.mult, r=[b_pUb, b_sb_], w=[b_m2])
                                kb.tt("pool", mT[:, f, :], m1[:], m2[:], ALU.add, r=[b_m1, b_m2], w=[b_mT])
                            for j in range(4):
                                ti = 4 * c + j
                                for half in range(2):
                                    hs = slice(half * 512, (half + 1) * 512)
                                    pmo, b_pmo = p5_r.get()
                                    for f in range(8):
                                        kb.mm(pmo[:], mT[:, f, j * 128:(j + 1) * 128], Wo[:, f, hs], f == 0, f == 7,
                                              r=[b_mT, b_Wo], w=[b_pmo])
                                    tmo, b_tmo = tmo_r.get()
                                    kb.tt("dve", tmo[:], pmo[:], G1bc[:, hs], ALU.mult, r=[b_pmo, b_G1bc], w=[b_tmo])
                                    kb.tt("pool", x1[:, ti, hs], x1[:, ti, hs], tmo[:], ALU.add, r=[b_tmo, b_x1[ti]], w=[b_x1[ti]])
                        if dbg and li == 0:
                            for ti in range(16):
                                kb.dma(dbg_x1[ti * 128:(ti + 1) * 128, :], x1[:, ti, :], r=[b_x1[ti]], w=[Buf()])
                        S.barrier()

                    if li == 0 and stop_after == "E":
                        return finish(nc, kb, top, out, b_out)

                    with ExitStack() as ph:
                        h2T, _ = kb.sb(ph, [128, 8, TO], BF16, "h2T")
                        b_h2T = [Buf() for _ in range(4)]
                        comb, b_comb = kb.sb(ph, [128, 16, 32], F32, "comb")
                        Wr, b_Wr = kb.sb(ph, [128, 8, 36], BF16, "Wr")
                        Wrf, b_Wrf = kb.sb(ph, [128, 8, 36], F32, "Wrf")
                        brt, b_brt = kb.sb(ph, [128, 36], F32, "brt")
                        kb.dma(Wrf[:], w_r.rearrange("(k p) n -> p k n", p=128), w=[b_Wrf])
                        kb.cp("dve", Wr[:], Wrf[:], r=[b_Wrf], w=[b_Wr])
                        kb.dma(brt[:], b_r[:, :], w=[b_brt])
                        norm_tile = make_norm(ph)
                        p6_r = kb.ring_ps(ph, 6, [128, 512], F32, "fp6")
                        lg_r = kb.ring_sb(ph, 2, [128, 36], F32, "lg")
                        sm_r = kb.ring_sb(ph, 2, [128, 16], F32, "sm")
                        e4_r = kb.ring_sb(ph, 2, [128, 4], F32, "e4")
                        lem_r = kb.ring_sb(ph, 2, [128, 32], F32, "lem")
                        m8_r = kb.ring_sb(ph, 2, [128, 8], F32, "fm8")
                        c12_r = kb.ring_sb(ph, 4, [128, 32], F32, "c12")
                        for ti in range(16):
                            c = ti // 4
                            tsl = slice(ti * 128, (ti + 1) * 128)
                            norm_tile(x1[:, ti, :], b_x1[ti], 2, h2T[:, :, tsl], b_h2T[c])
                            pl, b_pl = p6_r.get()
                            for k in range(8):
                                kb.mm(pl[:, 0:36], h2T[:, k, tsl], Wr[:, k, :], k == 0, k == 7, r=[b_h2T[c], b_Wr], w=[b_pl])
                            lg, b_lg = lg_r.get()
                            kb.tt("dve", lg[:], pl[:, 0:36], brt[:], ALU.add, r=[b_pl, b_brt], w=[b_lg])
                            sm, b_sm = sm_r.get()
                            kb.red("dve", sm[:, 0:1], lg[:, 0:4], ALU.max, r=[b_lg], w=[b_sm])
                            kb.ts("dve", sm[:, 1:2], sm[:, 0:1], -1.0, None, ALU.mult, r=[b_sm], w=[b_sm])
                            e4, b_e4 = e4_r.get()
                            kb.act(e4[:], lg[:, 0:4], AF.Exp, r=[b_lg, b_sm], w=[b_e4, b_sm], bias=sm[:, 1:2], accum_out=sm[:, 2:3])
                            kb.recip(sm[:, 3:4], sm[:, 2:3], r=[b_sm], w=[b_sm])
                            kb.ts("dve", sm[:, 8:12], lg[:, 0:4], sm[:, 0:1], None, ALU.is_ge, r=[b_lg, b_sm], w=[b_sm])
                            kb.ts("dve", sm[:, 8:12], sm[:, 8:12], -1.0, 1e9, ALU.add, ALU.mult, r=[b_sm], w=[b_sm])
                            lem, b_lem = lem_r.get()
                            kb.tt("dve", lem[:, :].rearrange("p (g e) -> p g e", e=8), lg[:, 4:36].rearrange("p (g e) -> p g e", e=8),
                                  sm[:, 8:12].unsqueeze(2).to_broadcast([128, 4, 8]), ALU.add, r=[b_lg, b_sm], w=[b_lem])
                            m8, b_m8 = m8_r.get()
                            kb.max8(m8[:], lem[:], r=[b_lem], w=[b_m8])
                            kb.tt("dve", sm[:, 4:5], m8[:, 0:1], m8[:, 1:2], ALU.subtract, r=[b_m8], w=[b_sm])
                            kb.act(sm[:, 5:6], sm[:, 4:5], AF.Sigmoid, r=[b_sm], w=[b_sm])
                            kb.tt("dve", sm[:, 6:7], sm[:, 5:6], sm[:, 3:4], ALU.mult, r=[b_sm], w=[b_sm])
                            kb.tt("dve", sm[:, 7:8], sm[:, 3:4], sm[:, 6:7], ALU.subtract, r=[b_sm], w=[b_sm])
                            c1, b_c1 = c12_r.get()
                            c2, b_c2 = c12_r.get()
                            kb.ts("dve", c1[:], lem[:], m8[:, 0:1], sm[:, 6:7], ALU.is_equal, ALU.mult, r=[b_lem, b_m8, b_sm], w=[b_c1])
                            kb.ts("dve", c2[:], lem[:], m8[:, 1:2], sm[:, 7:8], ALU.is_equal, ALU.mult, r=[b_lem, b_m8, b_sm], w=[b_c2])
                            kb.tt("dve", comb[:, ti, :], c1[:], c2[:], ALU.add, r=[b_c1, b_c2], w=[b_comb])
                        stg_r = kb.ring_sb(ph, 3, [128, 2048], F32, "xstg")
                        Wge_r = kb.ring_sb(ph, 2, [128, 8, 256], BF16, "Wge")
                        Wue_r = kb.ring_sb(ph, 2, [128, 8, 256], BF16, "Wue")
                        Wde_r = kb.ring_sb(ph, 2, [128, 2, D], BF16, "Wde")
                        sg_r = kb.ring_sb(ph, 3, [128, 512], F32, "fsg")
                        aT_r = kb.ring_sb(ph, 6, [128, 512], BF16, "aT")
                        tmo_r = kb.ring_sb(ph, 4, [128, 512], F32, "ftmo")
                        for e in range(32):
                            Wge, b_Wge = Wge_r.get()
                            Wue, b_Wue = Wue_r.get()
                            Wde, b_Wde = Wde_r.get()
                            s1, b_s1 = stg_r.get()
                            kb.dma(s1[:, :].rearrange("p (k f) -> p k f", f=256), w_eg[e].rearrange("(k p) f -> p k f", p=128), w=[b_s1])
                            kb.cp("pool", Wge[:, :, :].rearrange("p k f -> p (k f)"), s1[:], r=[b_s1], w=[b_Wge])
                            s2, b_s2 = stg_r.get()
                            kb.dma(s2[:, :].rearrange("p (k f) -> p k f", f=256), w_eu[e].rearrange("(k p) f -> p k f", p=128), w=[b_s2])
                            kb.cp("act", Wue[:, :, :].rearrange("p k f -> p (k f)"), s2[:], r=[b_s2], w=[b_Wue])
                            s3, b_s3 = stg_r.get()
                            kb.dma(s3[:, :].rearrange("p (k n) -> p k n", n=D), w_ed[e].rearrange("(k p) n -> p k n", p=128), w=[b_s3])
                            kb.tt("dve", Wde[:, :, :], s3[:, :].rearrange("p (k n) -> p k n", n=D),
                                  G2bc[:, :].unsqueeze(1).to_broadcast([128, 2, D]), ALU.mult, r=[b_s3, b_G2bc], w=[b_Wde])
                            def gate_up(c):
                                cs = slice(c * 512, (c + 1) * 512)
                                aTs = []
                                for ft in range(2):
                                    fs = slice(ft * 128, (ft + 1) * 128)
                                    pGt, b_pGt = p6_r.get()
                                    for k in range(8):
                                        kb.mm(pGt[:], Wge[:, k, fs], h2T[:, k, cs], k == 0, k == 7, r=[b_Wge, b_h2T[c]], w=[b_pGt])
                                    pUt, b_pUt = p6_r.get()
                                    for k in range(8):
                                        kb.mm(pUt[:], Wue[:, k, fs], h2T[:, k, cs], k == 0, k == 7, r=[b_Wue, b_h2T[c]], w=[b_pUt])
                                    sg, b_sg = sg_r.get()
                                    kb.act(sg[:], pGt[:], AF.Silu, r=[b_pGt], w=[b_sg])
                                    aT, b_aT = aT_r.get()
                                    kb.tt("dve", aT[:], pUt[:], sg[:], ALU.mult, r=[b_pUt, b_sg], w=[b_aT])
                                    aTs.append((aT, b_aT))
                                return aTs

                            def down(c, aTs):
                                for j in range(4):
                                    ti = 4 * c + j
                                    for half in range(2):
                                        hs = slice(half * 512, (half + 1) * 512)
                                        pd, b_pd = p6_r.get()
                                        for ft in range(2):
                                            kb.mm(pd[:], aTs[ft][0][:, j * 128:(j + 1) * 128], Wde[:, ft, hs], ft == 0, ft == 1,
                                                  r=[aTs[ft][1], b_Wde], w=[b_pd])
                                        kb.stt("dve", x1[:, ti, hs], pd[:], comb[:, ti, e:e + 1], x1[:, ti, hs], ALU.mult, ALU.add,
                                               r=[b_pd, b_comb, b_x1[ti]], w=[b_x1[ti]])

                            cur_a = gate_up(0)
                            for c in range(4):
                                nxt_a = gate_up(c + 1) if c + 1 < 4 else None
                                down(c, cur_a)
                                cur_a = nxt_a
                        if final and last:
                            fnw_t, b_fnw = kb.sb(ph, [128, D], F32, "fnw")
                            kb.dma(fnw_t[:], fnw[:, :], w=[b_fnw])
                            fst_r = kb.ring_sb(ph, 2, [128, 2], F32, "fst")
                            fj_r = kb.ring_sb(ph, 2, [128, D], F32, "fj")
                            for ti in range(16):
                                st, b_st = fst_r.get()
                                fj, b_fj = fj_r.get()
                                kb.act(fj[:], x1[:, ti, :], AF.Square, r=[b_x1[ti]], w=[b_fj, b_st], accum_out=st[:, 0:1])
                                kb.ts("dve", st[:, 1:2], st[:, 0:1], 1.0 / D, EPS, ALU.mult, ALU.add, r=[b_st], w=[b_st])
                                kb.act(st[:, 1:2], st[:, 1:2], AF.Sqrt, r=[b_st], w=[b_st])
                                kb.recip(st[:, 1:2], st[:, 1:2], r=[b_st], w=[b_st])
                                kb.act(fj[:], x1[:, ti, :], AF.Copy, r=[b_x1[ti], b_st], w=[b_fj], scale=st[:, 1:2])
                                kb.tt("dve", fj[:], fj[:], fnw_t[:], ALU.mult, r=[b_fj, b_fnw], w=[b_fj])
                                kb.dma(out[ti * 128:(ti + 1) * 128, :], fj[:], r=[b_fj], w=[b_out])
                        elif last:
                            for ti in range(16):
                                kb.dma(out[ti * 128:(ti + 1) * 128, :], x1[:, ti, :], r=[b_x1[ti]], w=[b_out])
                        else:
                            for ti in range(16):
                                r0 = hf * TO + ti * 128
                                kb.dma(xfull_s[r0:r0 + 128, :], x1[:, ti, :], r=[b_x1[ti]], w=[b_xfull_s])
                        S.barrier()
        return finish(nc, kb, top, out, b_out)


def finish(nc, kb, top, out, b_out):
    esem = {e: top.enter_context(nc.semaphore("es_" + e)) for e in ENGS}
    dsem = {}
    for e in ("sp", "pool"):
        for s in range(NDMA):
            dsem[(e, s)] = top.enter_context(nc.semaphore(f"ds_{e}_{s}"))
    with nc.Block() as block:
        kb.S.emit(block, esem, dsem)
    return nc


def make_consts(rel_bias):
    bf = ml_dtypes.bfloat16
    cst = {}
    cst["identb"] = np.eye(128, dtype=np.float32).astype(bf)
    cst["identf"] = np.eye(128, dtype=np.float32)
    oh = np.zeros((16, T), np.float32)
    for n in range(16):
        oh[n, n * 256:(n + 1) * 256] = 1.0
    cst["onehotK"] = oh.astype(bf)
    pn = np.zeros((128, 17, 8, 16), np.float32)
    for qb in range(17):
        pn[:, qb, :, qb:] = -1e30
    cst["padneg"] = pn.reshape(128, 17, 128)
    ki = np.arange(128)[:, None]
    col = np.arange(1024)[None, :]
    dist = col - 384 - ki
    bucket = t5_bucket_np(dist)
    rb = np.asarray(rel_bias, np.float32)
    cst["braw"] = np.ascontiguousarray(np.transpose(rb[bucket], (2, 0, 1)))
    cst["negm"] = np.where(dist >= 0, 0.0, -1e4).astype(np.float32)
    cst["b31"] = np.ascontiguousarray(np.broadcast_to(rb[31][None, :], (128, 8)))
    i = np.arange(128)[:, None]
    j = np.arange(128)[None, :]
    cst["mneg"] = np.where(j <= i, 0.0, -1e5).astype(np.float32)
    cst["strict"] = (j < i).astype(np.float32)
    cst["ut"] = (i <= j).astype(np.float32)
    bm = np.zeros((128, 5, 128), np.float32)
    bm[:, 0, :] = (i // 8 == j // 8)
    for lv, b in enumerate((8, 16, 32, 64)):
        bm[:, 1 + lv, :] = (i // (2 * b) == j // (2 * b)) & (i // b != j // b)
    cst["bmask"] = bm.astype(bf)
    return cst


def fm(v):
    return np.ascontiguousarray(np.asarray(v, np.float32).reshape(8, 128).T)


def layer_inputs(inp, l, cst):
    m = dict(cst)
    m["w_mod"] = np.ascontiguousarray(inp["w_mod"][l])
    m["b_mod"] = np.ascontiguousarray(inp["b_mod"][l][None, :])
    m["n1w"] = fm(inp["norm1_w"][l])
    m["n2w"] = fm(inp["norm2_w"][l])
    m["w_in"] = np.ascontiguousarray(inp["w_in"][l])
    cw = np.asarray(inp["conv_w"][l], np.float32)
    m["convT"] = np.ascontiguousarray(cw.reshape(4, 12, 128).transpose(2, 1, 0))
    m["alog"] = np.ascontiguousarray(np.broadcast_to(inp["a_log"][l][None, :], (128, 4)))
    m["dtb"] = np.ascontiguousarray(np.broadcast_to(inp["dt_bias"][l][None, :], (128, 4)))
    m["onw"] = np.ascontiguousarray(np.broadcast_to(np.tile(inp["onorm_w"][l], 4)[None, :], (128, 512)))
    m["w_up_a"] = np.ascontiguousarray(inp["w_up_a"][l])
    m["w_up_b"] = np.ascontiguousarray(inp["w_up_b"][l])
    m["w_out"] = np.ascontiguousarray(inp["w_out"][l])
    m["w_r"] = np.ascontiguousarray(np.concatenate([inp["w_rg"][l], inp["w_re"][l]], axis=1))
    br = np.concatenate([inp["b_rg"][l], inp["b_re"][l]])
    m["b_r"] = np.ascontiguousarray(np.broadcast_to(br[None, :], (128, 36)))
    m["w_eg"] = np.ascontiguousarray(inp["w_e_gate"][l].reshape(32, D, 256))
    m["w_eu"] = np.ascontiguousarray(inp["w_e_up"][l].reshape(32, D, 256))
    m["w_ed"] = np.ascontiguousarray(inp["w_e_down"][l].reshape(32, 256, D))
    m["fnw"] = np.ascontiguousarray(np.broadcast_to(inp["final_norm_w"][None, :], (128, D)))
    return m


def core_inputs(base, x_full, c, core):
    b, half = core // 2, core % 2
    m = dict(base)
    m["xf"] = np.ascontiguousarray(x_full[b])
    m["xo"] = np.ascontiguousarray(x_full[b, half * TO:(half + 1) * TO])
    m["cT"] = fm(c[b])
    s = np.zeros((128, 2), np.float32)
    s[:, half] = 1.0
    m["sel"] = s
    return m


_PROGS = {}


def _prog():
    if "p" not in _PROGS:
        _PROGS["p"] = build_program(final=True, nl=2)
    return _PROGS["p"]


def kernel(**inputs):
    inp = {k: np.asarray(v) for k, v in inputs.items()}
    cst = make_consts(inp["rel_bias"])
    x = np.asarray(inp["x"], np.float32)
    c = np.asarray(inp["c"], np.float32)
    base = dict(cst)
    shared = set(cst.keys()) | {"fnw"}
    for l in range(2):
        li = layer_inputs(inp, l, cst)
        for k, v in li.items():
            if k in shared:
                base[k] = v
            else:
                base[f"{k}_{l}"] = v
    in_maps = [core_inputs(base, x, c, core) for core in range(8)]
    res = run_bass_kernel_spmd(_prog(), in_maps, core_ids=list(range(8)))
    return np.stack([np.concatenate([res.results[2 * b]["out"], res.results[2 * b + 1]["out"]], axis=0)
                     for b in range(4)]).astype(np.float32)
```

```python
import math
from contextlib import ExitStack

import numpy as np
import ml_dtypes
import concourse.bass as bass
import concourse.mybir as mybir
from concourse.bass_utils import run_bass_kernel_spmd

F32 = mybir.dt.float32
BF16 = mybir.dt.bfloat16
AF = mybir.ActivationFunctionType
ALU = mybir.AluOpType
AX = mybir.AxisListType

D = 1024
T = 4096
TO = 2048
NT = 32
H_A = 8
H_B = 4
D_IN = 5640
EPS = 1e-6
BIG = 30000.0

ENGS = ("pe", "act", "dve", "pool", "sp")
NDMA = 12


class Buf:
    __slots__ = ("lw", "rs", "excl")

    def __init__(self, excl=False):
        self.lw = None
        self.rs = []
        self.excl = excl


class Op:
    __slots__ = ("eng", "fn", "deps", "dma", "needed", "sigval", "dsem", "dval", "prev_slot", "phase")

    def __init__(self, eng, fn, dma, phase):
        self.eng = eng
        self.fn = fn
        self.deps = []
        self.dma = dma
        self.needed = False
        self.sigval = None
        self.dsem = None
        self.dval = None
        self.prev_slot = None
        self.phase = phase


class Sched:
    def __init__(self):
        self.ops = {e: [] for e in ENGS}
        self.dma_slot = {e: 0 for e in ENGS}
        self.dma_last = {}
        self.dma_cnt = {}
        self.phase = 0
        self.last = {e: None for e in ENGS}

    def add(self, eng, fn, reads=(), writes=(), dma=False):
        op = Op(eng, fn, dma, self.phase)
        deps = []
        xr = [b for b in reads if b.excl]
        if xr:
            reads = [b for b in reads if not b.excl]
            writes = list(writes) + xr
        for b in reads:
            if b.lw is not None:
                deps.append(b.lw)
        for b in writes:
            if b.lw is not None:
                deps.append(b.lw)
            deps.extend(b.rs)
        seen = set()
        for d in deps:
            if id(d) in seen or d.phase < self.phase:
                continue
            seen.add(id(d))
            if d.eng == "pe" and eng == "pe" and not d.dma and not dma:
                continue
            op.deps.append(d)
            d.needed = True
        for b in reads:
            if not dma:
                b.rs = [o for o in b.rs if o.dma or o.eng != eng]
            b.rs.append(op)
        for b in writes:
            b.lw = op
            b.rs = []
        if dma:
            slot = self.dma_slot[eng]
            self.dma_slot[eng] = (slot + 1) % NDMA
            key = (eng, slot)
            op.prev_slot = self.dma_last.get(key)
            self.dma_last[key] = op
            self.dma_cnt[key] = self.dma_cnt.get(key, 0) + 16
            op.dsem = key
            op.dval = self.dma_cnt[key]
        else:
            self.last[eng] = op
        self.ops[eng].append(op)
        return op

    def barrier(self):
        deps = []
        for e in ENGS:
            if self.last[e] is not None:
                deps.append(self.last[e])
        deps.extend(self.dma_last.values())
        for e in ENGS:
            op = Op(e, None, False, self.phase)
            for d in deps:
                op.deps.append(d)
                d.needed = True
            self.ops[e].append(op)
        self.phase += 1

    def emit(self, block, esem, dsem):
        for e in ENGS:
            cnt = 0
            for op in self.ops[e]:
                if op.fn is not None and not op.dma and op.needed:
                    cnt += 1
                    op.sigval = cnt
        sched = self

        def run_engine(e, engobj):
            known = {}

            def wait(key, sem, val):
                if known.get(key, 0) >= val:
                    return
                engobj.wait_ge(sem, val)
                known[key] = val

            for op in sched.ops[e]:
                for d in op.deps:
                    if d.dma:
                        wait(d.dsem, dsem[d.dsem], d.dval)
                    else:
                        wait(d.eng, esem[d.eng], d.sigval)
                if op.fn is None:
                    continue
                if op.dma:
                    if op.prev_slot is not None:
                        p = op.prev_slot
                        wait(p.dsem, dsem[p.dsem], p.dval)
                    op.fn(engobj).then_inc(dsem[op.dsem], 16)
                else:
                    ins = op.fn(engobj)
                    if op.needed:
                        ins.then_inc(esem[e], 1)
            for (qe, slot), last in sched.dma_last.items():
                if qe == e:
                    wait(last.dsem, dsem[last.dsem], last.dval)

        block.tensor(lambda eng: run_engine("pe", eng))
        block.scalar(lambda eng: run_engine("act", eng))
        block.vector(lambda eng: run_engine("dve", eng))
        block.gpsimd(lambda eng: run_engine("pool", eng))
        block.sync(lambda eng: run_engine("sp", eng))


class Ring:
    def __init__(self, items):
        self.items = items
        self.i = 0

    def get(self):
        it = self.items[self.i % len(self.items)]
        self.i += 1
        return it


class K:
    def __init__(self, nc):
        self.nc = nc
        self.S = Sched()
        self.uid = 0

    def sb(self, st, shape, dt, name=None):
        self.uid += 1
        t = st.enter_context(self.nc.sbuf_tensor(f"{name or 't'}_{self.uid}", list(shape), dt))
        return t, Buf()

    def ps(self, st, shape, dt, name=None):
        self.uid += 1
        t = st.enter_context(self.nc.psum_tensor(f"{name or 'p'}_{self.uid}", list(shape), dt))
        return t, Buf(excl=True)

    def ring_sb(self, st, n, shape, dt, name=None):
        return Ring([self.sb(st, shape, dt, name) for _ in range(n)])

    def ring_ps(self, st, n, shape, dt, name=None):
        return Ring([self.ps(st, shape, dt, name) for _ in range(n)])

    def dma(self, out, in_, r=(), w=(), q="sp"):
        self.S.add(q, lambda e: e.dma_start(out=out, in_=in_), reads=r, writes=w, dma=True)

    def mm(self, out, lhsT, rhs, start, stop, r=(), w=()):
        self.S.add("pe", lambda e: e.matmul(out, lhsT=lhsT, rhs=rhs, start=start, stop=stop, skip_group_check=True), reads=r, writes=w)

    def tr(self, out, in_, ident, r=(), w=()):
        self.S.add("pe", lambda e: e.transpose(out=out, in_=in_, identity=ident), reads=r, writes=w)

    def act(self, out, in_, func, r=(), w=(), bias=None, scale=None, accum_out=None):
        kw = {}
        if bias is not None:
            kw["bias"] = bias
        if scale is not None:
            kw["scale"] = scale
        if accum_out is not None:
            kw["accum_out"] = accum_out
        self.S.add("act", lambda e: e.activation(out=out, in_=in_, func=func, **kw), reads=r, writes=w)

    def tt(self, eng, out, in0, in1, op, r=(), w=()):
        self.S.add(eng, lambda e: e.tensor_tensor(out=out, in0=in0, in1=in1, op=op), reads=r, writes=w)

    def ts(self, eng, out, in0, s1, s2, op0, op1=None, r=(), w=()):
        if op1 is None:
            self.S.add(eng, lambda e: e.tensor_scalar(out=out, in0=in0, scalar1=s1, scalar2=None, op0=op0),
                       reads=r, writes=w)
        else:
            self.S.add(eng, lambda e: e.tensor_scalar(out=out, in0=in0, scalar1=s1, scalar2=s2, op0=op0, op1=op1),
                       reads=r, writes=w)

    def stt(self, eng, out, in0, scalar, in1, op0, op1, r=(), w=()):
        self.S.add(eng, lambda e: e.scalar_tensor_tensor(out=out, in0=in0, scalar=scalar, in1=in1, op0=op0, op1=op1),
                   reads=r, writes=w)

    def cp(self, eng, out, in_, r=(), w=()):
        if eng == "act":
            self.S.add("act", lambda e: e.activation(out=out, in_=in_, func=AF.Copy), reads=r, writes=w)
        else:
            self.S.add(eng, lambda e: e.tensor_copy(out=out, in_=in_), reads=r, writes=w)

    def memset(self, eng, out, val, r=(), w=()):
        self.S.add(eng, lambda e: e.memset(out, val), reads=r, writes=w)

    def red(self, eng, out, in_, op, r=(), w=()):
        self.S.add(eng, lambda e: e.tensor_reduce(out=out, in_=in_, axis=AX.X, op=op), reads=r, writes=w)

    def recip(self, out, in_, r=(), w=()):
        self.S.add("dve", lambda e: e.reciprocal(out=out, in_=in_), reads=r, writes=w)

    def max8(self, out, in_, r=(), w=()):
        self.S.add("dve", lambda e: e.max(out=out, in_=in_), reads=r, writes=w)


def t5_bucket_np(dist):
    n = np.maximum(dist, 0)
    nf = np.maximum(n, 1).astype(np.float32)
    large = 16 + (np.log(nf / np.float32(16)) / np.float32(math.log(8.0)) * np.float32(16)).astype(np.int32)
    large = np.minimum(large, 31)
    return np.where(n < 16, n, large)


def build_program(final, dbg=False, stop_after=None, nl=1):
    nc = bass.Bass("TRN2", target_bir_lowering=False)
    kb = K(nc)
    S = kb.S

    def din(name, shape, dt=F32):
        return nc.dram_tensor(name, list(shape), dt, kind="ExternalInput").ap()

    def dscr(name, shape, dt):
        return nc.dram_tensor(name, list(shape), dt, kind=("ExternalOutput" if dbg else "Internal")).ap()

    xf = din("xf", [T, D])
    xo = din("xo", [TO, D])
    cT = din("cT", [128, 8])
    sel = din("sel", [128, 2])
    WL = []
    for li_ in range(nl):
        sfx = "" if nl == 1 else f"_{li_}"
        Wd_ = {}
        Wd_["w_mod"] = din("w_mod" + sfx, [D, 6 * D])
        Wd_["b_mod"] = din("b_mod" + sfx, [1, 6 * D])
        Wd_["n1w"] = din("n1w" + sfx, [128, 8])
        Wd_["n2w"] = din("n2w" + sfx, [128, 8])
        Wd_["w_in"] = din("w_in" + sfx, [D, D_IN])
        Wd_["convT"] = din("convT" + sfx, [128, 12, 4])
        Wd_["alog"] = din("alog" + sfx, [128, 4])
        Wd_["dtb"] = din("dtb" + sfx, [128, 4])
        Wd_["onw"] = din("onw" + sfx, [128, 512])
        Wd_["w_up_a"] = din("w_up_a" + sfx, [512, D])
        Wd_["w_up_b"] = din("w_up_b" + sfx, [512, D])
        Wd_["w_out"] = din("w_out" + sfx, [D, D])
        Wd_["w_r"] = din("w_r" + sfx, [D, 36])
        Wd_["b_r"] = din("b_r" + sfx, [128, 36])
        Wd_["w_eg"] = din("w_eg" + sfx, [32, D, 256])
        Wd_["w_eu"] = din("w_eu" + sfx, [32, D, 256])
        Wd_["w_ed"] = din("w_ed" + sfx, [32, 256, D])
        WL.append(Wd_)
    fnw = din("fnw", [128, D])
    identb_d = din("identb", [128, 128], BF16)
    identf_d = din("identf", [128, 128])
    onehotK = din("onehotK", [16, T], BF16)
    padneg_d = din("padneg", [128, 17, 128])
    braw = din("braw", [8, 128, 1024])
    negm = din("negm", [128, 1024])
    b31 = din("b31", [128, 8])
    mneg_d = din("mneg", [128, 128])
    strict_d = din("strict", [128, 128])
    ut_d = din("ut", [128, 128])
    bmask_d = din("bmask", [128, 5, 128], BF16)

    out = nc.dram_tensor("out", [TO, D], F32, kind="ExternalOutput").ap()

    qT_s = dscr("qT_s", [4, 128, T], BF16)
    kT_s = dscr("kT_s", [4, 128, T], BF16)
    MT_s = dscr("MT_s", [128, T], BF16)
    v_s = dscr("v_s", [NT, 128, 520], BF16)
    qTb_s = dscr("qTb_s", [4, 128, T], BF16)
    kTb_s = dscr("kTb_s", [4, 128, T], BF16)
    kb_s = dscr("kb_s", [NT, 128, 512], BF16)
    vb_s = dscr("vb_s", [NT, 128, 512], BF16)
    z_s = dscr("z_s", [NT, 128, 512], F32)
    ya_s = dscr("ya_s", [T, 512], BF16)
    xown_s = nc.dram_tensor("xown_s", [TO, D], F32, kind="Internal").ap()
    xfull_s = nc.dram_tensor("xfull_s", [T, D], F32, kind="Internal").ap()
    b_xown_s, b_xfull_s = Buf(), Buf()
    yb_s = dscr("yb_s", [T, 512], BF16)
    b_qT_s, b_kT_s, b_MT_s, b_v_s = Buf(), Buf(), Buf(), Buf()
    b_qTb_s, b_kTb_s, b_kb_s, b_vb_s, b_z_s, b_ya_s, b_yb_s = Buf(), Buf(), Buf(), Buf(), Buf(), Buf(), Buf()
    if dbg:
        dbg_mod = nc.dram_tensor("dbg_mod", [128, 48], F32, kind="ExternalOutput").ap()
        dbg_bg = nc.dram_tensor("dbg_bg", [128, NT, 8], F32, kind="ExternalOutput").ap()
        dbg_x1 = nc.dram_tensor("dbg_x1", [TO, D], F32, kind="ExternalOutput").ap()
    b_out = Buf()

    with ExitStack() as top:
        identb, b_identb = kb.sb(top, [128, 128], BF16, "identb")
        identf, b_identf = kb.sb(top, [128, 128], F32, "identf")
        onesb, b_onesb = kb.sb(top, [128, 128], BF16, "onesb")
        onesf, b_onesf = kb.sb(top, [128, 128], F32, "onesf")
        epsc, b_epsc = kb.sb(top, [128, 1], F32, "epsc")
        modT, b_modT = kb.sb(top, [128, 48], F32, "modT")
        AB, b_AB = kb.sb(top, [128, 4, 8], F32, "AB")
        G1bc, b_G1bc = kb.sb(top, [128, D], F32, "G1bc")
        G2bc, b_G2bc = kb.sb(top, [128, D], F32, "G2bc")
        BG, b_BG = kb.sb(top, [128, NT, 8], F32, "BG")
        selt, b_selt = kb.sb(top, [128, 2], F32, "selt")
        consts_r = [b_identb, b_identf, b_onesb, b_onesf, b_epsc]

        kb.dma(identb[:], identb_d[:, :], w=[b_identb])
        kb.dma(identf[:], identf_d[:, :], w=[b_identf])
        kb.dma(selt[:], sel[:, :], w=[b_selt])
        kb.memset("pool", onesb[:], 1.0, w=[b_onesb])
        kb.memset("pool", onesf[:], 1.0, w=[b_onesf])
        kb.memset("pool", epsc[:], EPS, w=[b_epsc])

        for li in range(nl):
            Wl = WL[li]
            w_mod = Wl["w_mod"]
            b_mod = Wl["b_mod"]
            n1w = Wl["n1w"]
            n2w = Wl["n2w"]
            w_in = Wl["w_in"]
            convT = Wl["convT"]
            alog = Wl["alog"]
            dtb = Wl["dtb"]
            onw = Wl["onw"]
            w_up_a = Wl["w_up_a"]
            w_up_b = Wl["w_up_b"]
            w_out = Wl["w_out"]
            w_r = Wl["w_r"]
            b_r = Wl["b_r"]
            w_eg = Wl["w_eg"]
            w_eu = Wl["w_eu"]
            w_ed = Wl["w_ed"]
            xsrc_full = xf if li == 0 else xfull_s
            xsrc_own = xo if li == 0 else xown_s
            last = (li == nl - 1)
            with ExitStack() as ph:
                ct_t, b_ct = kb.sb(ph, [128, 8], F32)
                cact, b_cact = kb.sb(ph, [128, 8], F32)
                CB, b_CB = kb.sb(ph, [128, 8, 128], F32)
                bm, b_bm = kb.sb(ph, [1, 6 * D], F32)
                modbc, b_modbc = kb.sb(ph, [128, 6 * D], F32)
                n1t, b_n1t = kb.sb(ph, [128, 8], F32)
                n2t, b_n2t = kb.sb(ph, [128, 8], F32)
                stg = kb.ring_sb(ph, 2, [128, 8, 512], F32)
                pmr = kb.ring_ps(ph, 2, [128, 512], F32)
                kb.dma(ct_t[:], cT[:, :], w=[b_ct])
                kb.dma(bm[:], b_mod[:, :], w=[b_bm])
                kb.dma(n1t[:], n1w[:, :], w=[b_n1t])
                kb.dma(n2t[:], n2w[:, :], w=[b_n2t])
                kb.act(cact[:], ct_t[:], AF.Silu, r=[b_ct], w=[b_cact])
                kb.cp("dve", CB[:], cact[:, :].unsqueeze(2).to_broadcast([128, 8, 128]), r=[b_cact], w=[b_CB])
                wmv = w_mod.rearrange("(k p) n -> p k n", p=128)
                for jg in range(12):
                    st_t, b_st = stg.get()
                    kb.dma(st_t[:], wmv[:, :, jg * 512:(jg + 1) * 512], w=[b_st])
                    pm, b_pm = pmr.get()
                    for k in range(8):
                        kb.mm(pm[:], CB[:, k, :], st_t[:, k, :], k == 0, False, r=[b_CB, b_st], w=[b_pm])
                    kb.mm(pm[:], onesf[0:1, :], bm[0:1, jg * 512:(jg + 1) * 512], False, True, r=[b_onesf, b_bm], w=[b_pm])
                    kb.cp("act" if jg % 2 else "dve", modbc[:, jg * 512:(jg + 1) * 512], pm[:], r=[b_pm], w=[b_modbc])
                with ExitStack() as ph2:
                    prod, b_prod = kb.sb(ph2, [128, 48, 128], F32)
                    kb.tt("dve", prod[:], modbc[:, :].rearrange("p (a b) -> p a b", b=128),
                          identf[:, :].unsqueeze(1).to_broadcast([128, 48, 128]), ALU.mult,
                          r=[b_modbc, b_identf], w=[b_prod])
                    kb.red("dve", modT[:], prod[:], ALU.add, r=[b_prod], w=[b_modT])
                kb.stt("dve", AB[:, 0, :], modT[:, 8:16], 1.0, n1t[:], ALU.add, ALU.mult, r=[b_modT, b_n1t], w=[b_AB])
                kb.cp("dve", AB[:, 1, :], modT[:, 0:8], r=[b_modT], w=[b_AB])
                kb.stt("dve", AB[:, 2, :], modT[:, 32:40], 1.0, n2t[:], ALU.add, ALU.mult, r=[b_modT, b_n2t], w=[b_AB])
                kb.cp("dve", AB[:, 3, :], modT[:, 24:32], r=[b_modT], w=[b_AB])
                kb.cp("act", G1bc[:], modbc[:, 2 * D:3 * D], r=[b_modbc], w=[b_G1bc])
                kb.cp("act", G2bc[:], modbc[:, 5 * D:6 * D], r=[b_modbc], w=[b_G2bc])
                if dbg and li == 0:
                    kb.dma(dbg_mod[:, :], modT[:], r=[b_modT], w=[Buf()])
                S.barrier()

            if li == 0 and stop_after == "M":
                return finish(nc, kb, top, out, b_out)

            with ExitStack() as ph:
                Wm, b_Wm = kb.sb(ph, [128, 8, 3600], BF16, "Wm")
                wiv = w_in.rearrange("(k p) n -> p k n", p=128)
                with ExitStack() as ph2:
                    stg = kb.ring_sb(ph2, 2, [128, 8, 512], F32)
                    for g in range(8):
                        c0 = g * 512
                        cw = min(512, 3600 - c0)
                        st_t, b_st = stg.get()
                        kb.dma(st_t[:, :, 0:cw], wiv[:, :, c0:c0 + cw], w=[b_st])
                        kb.cp("pool" if g % 2 else "dve", Wm[:, :, c0:c0 + cw], st_t[:, :, 0:cw], r=[b_st], w=[b_Wm])
                    S.barrier()
                padneg, b_padneg = kb.sb(ph, [128, 17, 128], F32, "padneg")
                kb.dma(padneg[:], padneg_d[:, :, :], w=[b_padneg])
                cvw, b_cvw = kb.sb(ph, [128, 12, 4], F32, "cvw")
                kb.dma(cvw[:], convT[:, :, :], w=[b_cvw])
                negA, b_negA = kb.sb(ph, [128, 4], F32, "negA")
                dtb_t, b_dtb = kb.sb(ph, [128, 4], F32, "dtb")
                kb.dma(negA[:], alog[:, :], w=[b_negA])
                kb.dma(dtb_t[:], dtb[:, :], w=[b_dtb])
                kb.act(negA[:], negA[:], AF.Exp, r=[b_negA], w=[b_negA])
                kb.ts("dve", negA[:], negA[:], -1.0, None, ALU.mult, r=[b_negA], w=[b_negA])
                KMbd, b_KMbd = kb.sb(ph, [128, 4, 32], BF16, "KMbd")
                kb.memset("pool", KMbd[:], 0.0, w=[b_KMbd])
                raw, b_raw = kb.sb(ph, [128, 12, 515], BF16, "raw")
                Dg, b_Dg = kb.sb(ph, [128, 12, 4, 128], BF16, "Dg")
                for ct_ in range(12):
                    for jj_ in range(4):
                        kb.ts("dve", Dg[:, ct_, jj_, :], identf[:], cvw[:, ct_, jj_:jj_ + 1], None, ALU.mult,
                              r=[b_identf, b_cvw], w=[b_Dg])
                b_rawc = [Buf() for _ in range(12)]
                kb.memset("pool", raw[:, :, 0:3], 0.0, w=b_rawc)

                xt_r = kb.ring_sb(ph, 3, [128, D], F32, "xt")
                junk_r = kb.ring_sb(ph, 2, [128, D], BF16, "junk")
                xn_r = kb.ring_sb(ph, 2, [128, D], BF16, "xn")
                st_r = kb.ring_sb(ph, 4, [128, 2], F32, "st")
                hT_r = kb.ring_sb(ph, 2, [128, 8, 512], BF16, "hT")
                pT_r = kb.ring_ps(ph, 2, [128, 8, 128], BF16, "pT")
                pm_r = kb.ring_ps(ph, 4, [128, 512], F32, "pm")
                pg_r = kb.ring_ps(ph, 1, [128, 512], F32, "pg")
                pX_r = kb.ring_ps(ph, 1, [128, 8, 128], BF16, "pX")
                qsb_r = kb.ring_sb(ph, 8, [128, 512], BF16, "qsb")
                ksb_r = kb.ring_sb(ph, 3, [128, 512], BF16, "ksb")
                km2_r = kb.ring_sb(ph, 2, [128, 2], F32, "km2")
                vsb_r = kb.ring_sb(ph, 3, [128, 8, 65], BF16, "vsb")
                for (vt, bvt) in vsb_r.items:
                    kb.memset("pool", vt[:, :, 64:65], 1.0, w=[bvt])
                zsb_r = kb.ring_sb(ph, 2, [128, 512], F32, "zsb")
                Gs_r = kb.ring_sb(ph, 2, [128, 128], F32, "Gs")
                m8_r = kb.ring_sb(ph, 2, [128, 8, 8], F32, "m8")
                Mbf_r = kb.ring_sb(ph, 5, [128, 128], BF16, "Mbf")
                MTsb_r = kb.ring_sb(ph, 2, [128, 512], BF16, "MTsb")
                t4_r = kb.ring_sb(ph, 4, [128, 8], F32, "t4")
                sc_r = kb.ring_sb(ph, 6, [128, 512], F32, "sc")
                sq_r = kb.ring_sb(ph, 4, [128, 512], BF16, "sq")
                rr_r = kb.ring_sb(ph, 3, [128, 512], F32, "rr")
                nrm_r = kb.ring_sb(ph, 5, [128, 512], BF16, "nrm")
                tok_r = kb.ring_sb(ph, 3, [128, 4, 128], BF16, "tok")

                def do_norm(c):
                    hT, b_hT = hT_r.get()
                    for j in range(4):
                        ti = 4 * c + j
                        xt, b_xt = xt_r.get()
                        kb.dma(xt[:], xsrc_full[ti * 128:(ti + 1) * 128, :], r=[b_xfull_s], w=[b_xt])
                        junk, b_junk = junk_r.get()
                        st, b_st = st_r.get()
                        kb.act(junk[:], xt[:], AF.Square, r=[b_xt], w=[b_junk, b_st], accum_out=st[:, 0:1])
                        kb.ts("dve", st[:, 1:2], st[:, 0:1], 1.0 / D, EPS, ALU.mult, ALU.add, r=[b_st], w=[b_st])
                        kb.act(st[:, 1:2], st[:, 1:2], AF.Sqrt, r=[b_st], w=[b_st])
                        kb.recip(st[:, 1:2], st[:, 1:2], r=[b_st], w=[b_st])
                        xn, b_xn = xn_r.get()
                        kb.act(xn[:], xt[:], AF.Copy, r=[b_xt, b_st], w=[b_xn], scale=st[:, 1:2])
                        pT, b_pT = pT_r.get()
                        for k in range(8):
                            kb.tr(pT[:, k, :], xn[:, k * 128:(k + 1) * 128], identb[:], r=[b_xn, b_identb], w=[b_pT])
                        hv = hT[:, :, j * 128:(j + 1) * 128]
                        kb.tt("dve", hv, pT[:], AB[:, 0, :].unsqueeze(2).to_broadcast([128, 8, 128]), ALU.mult,
                              r=[b_pT, b_AB], w=[b_hT])
                        kb.tt("pool", hv, hv, AB[:, 1, :].unsqueeze(2).to_broadcast([128, 8, 128]), ALU.add,
                              r=[b_hT, b_AB], w=[b_hT])
                    return hT, b_hT

                nxt_h = do_norm(0)
                for c in range(8):
                    hT, b_hT = nxt_h
                    if c + 1 < 8:
                        nxt_h = do_norm(c + 1)
                    cs = slice(c * 512, (c + 1) * 512)
                    import os
                    PARTS = os.environ.get("KPARTS", "kqsvg")
                    for p in range(4 if "k" in PARTS else 0):
                        pm, b_pm = pm_r.get()
                        for k in range(8):
                            kb.mm(pm[:], Wm[:, k, 512 + p * 128:512 + (p + 1) * 128], hT[:, k, :], k == 0, k == 7,
                                  r=[b_Wm, b_hT], w=[b_pm])
                        ksb, b_ksb = ksb_r.get()
                        kb.cp("act", ksb[:], pm[:], r=[b_pm], w=[b_ksb])
                        kb.dma(kT_s[p, :, cs], ksb[:], r=[b_ksb], w=[b_kT_s])
                        km2, b_km2 = km2_r.get()
                        kb.red("dve", km2[:], pm[:, :].rearrange("p (a b) -> p a b", b=256), ALU.add, r=[b_pm], w=[b_km2])
                        kb.cp("dve", KMbd[0:64, p, 2 * c:2 * c + 2], km2[0:64, :], r=[b_km2], w=[b_KMbd])
                        kb.cp("dve", KMbd[64:128, p, 16 + 2 * c:16 + 2 * c + 2], km2[64:128, :], r=[b_km2], w=[b_KMbd])
                    qs = []
                    for p in range(4 if "q" in PARTS else 0):
                        pm, b_pm = pm_r.get()
                        for k in range(8):
                            kb.mm(pm[:], Wm[:, k, p * 128:(p + 1) * 128], hT[:, k, :], k == 0, k == 7,
                                  r=[b_Wm, b_hT], w=[b_pm])
                        qsb, b_qsb = qsb_r.get()
                        kb.cp("act", qsb[:], pm[:], r=[b_pm], w=[b_qsb])
                        kb.dma(qT_s[p, :, cs], qsb[:], r=[b_qsb], w=[b_qT_s])
                        qs.append((qsb, b_qsb))
                    Mbfs = []
                    for j in range(4 if "s" in PARTS else 0):
                        ti = 4 * c + j
                        qb = ti // 2
                        Mbf, b_Mbf = Mbf_r.get()
                        if qb < 4:
                            kb.ts("dve", Mbf[:], padneg[:, qb + 1, :], -BIG, None, ALU.max, r=[b_padneg], w=[b_Mbf])
                        else:
                            pg, b_pg = pg_r.get()
                            for p in range(4):
                                kb.mm(pg[:, p * 32:(p + 1) * 32], qs[p][0][:, j * 128:(j + 1) * 128], KMbd[:, p, :], True, True,
                                      r=[qs[p][1], b_KMbd], w=[b_pg])
                            Gs, b_Gs = Gs_r.get()
                            kb.tt("dve", Gs[:], pg[:, 0:128], padneg[:, qb, :], ALU.add, r=[b_pg, b_padneg], w=[b_Gs])
                            m8, b_m8 = m8_r.get()
                            for h in range(8):
                                kb.max8(m8[:, h, :], Gs[:, h * 16:(h + 1) * 16], r=[b_Gs], w=[b_m8])
                            G3 = Gs[:, :].rearrange("p (h n) -> p h n", n=16)
                            kb.tt("dve", G3, G3, m8[:, :, 2:3].to_broadcast([128, 8, 16]), ALU.is_ge, r=[b_Gs, b_m8], w=[b_Gs])
                            M3 = Mbf[:, :].rearrange("p (h n) -> p h n", n=16)
                            kb.ts("dve", M3, G3, -1.0, BIG, ALU.add, ALU.mult, r=[b_Gs], w=[b_Mbf])
                            kb.memset("dve", M3[:, :, qb:qb + 1], 0.0, r=[], w=[b_Mbf])
                        Mbfs.append((Mbf, b_Mbf))
                    nj = 4 if "v" in PARTS else 0
                    for j in range(nj):
                        ti = 4 * c + j
                        hs = slice(j * 128, (j + 1) * 128)
                        pm, b_pm = pm_r.get()
                        for k in range(8):
                            kb.mm(pm[:], hT[:, k, hs], Wm[:, k, 1024:1536], k == 0, k == 7, r=[b_Wm, b_hT], w=[b_pm])
                        vsb, b_vsb = vsb_r.get()
                        kb.cp("dve", vsb[:, :, 0:64], pm[:, :].rearrange("p (h d) -> p h d", d=64), r=[b_pm], w=[b_vsb])
                        kb.dma(v_s[ti, :, :], vsb[:, :, :].rearrange("p h d -> p (h d)"), r=[b_vsb], w=[b_v_s])
                    t4s = []
                    for j in range(nj):
                        ti = 4 * c + j
                        hs = slice(j * 128, (j + 1) * 128)
                        pm, b_pm = pm_r.get()
                        for k in range(8):
                            kb.mm(pm[:, 0:8], hT[:, k, hs], Wm[:, k, 3584:3592], k == 0, k == 7, r=[b_Wm, b_hT], w=[b_pm])
                        t4, b_t4 = t4_r.get()
                        kb.cp("dve", t4[:, 0:4], pm[:, 0:4], r=[b_pm], w=[b_t4])
                        kb.tt("dve", t4[:, 4:8], pm[:, 4:8], dtb_t[:], ALU.add, r=[b_pm, b_dtb], w=[b_t4])
                        t4s.append((t4, b_t4))
                    for j in range(nj):
                        t4, b_t4 = t4s[j]
                        kb.act(BG[:, 4 * c + j, 0:4], t4[:, 0:4], AF.Sigmoid, r=[b_t4], w=[b_BG])
                    for j in range(nj):
                        ti = 4 * c + j
                        hs = slice(j * 128, (j + 1) * 128)
                        pm, b_pm = pm_r.get()
                        for k in range(8):
                            kb.mm(pm[:], hT[:, k, hs], Wm[:, k, 3072:3584], k == 0, k == 7, r=[b_Wm, b_hT], w=[b_pm])
                        zsb, b_zsb = zsb_r.get()
                        kb.act(zsb[:], pm[:], AF.Silu, r=[b_pm], w=[b_zsb])
                        kb.dma(z_s[ti, :, :], zsb[:], r=[b_zsb], w=[b_z_s])
                    for j in range(nj):
                        t4, b_t4 = t4s[j]
                        kb.act(t4[:, 4:8], t4[:, 4:8], AF.Exp, r=[b_t4], w=[b_t4])
                    for j in range(nj):
                        t4, b_t4 = t4s[j]
                        kb.act(t4[:, 4:8], t4[:, 4:8], AF.Ln, r=[b_t4], w=[b_t4], bias=1.0)
                        kb.tt("dve", BG[:, 4 * c + j, 4:8], t4[:, 4:8], negA[:], ALU.mult, r=[b_t4, b_negA], w=[b_BG])
                    if "s" in PARTS:
                        pX, b_pX = pX_r.get()
                        for j in range(4):
                            kb.tr(pX[:, j, :], Mbfs[j][0][:], identb[:], r=[Mbfs[j][1], b_identb], w=[b_pX])
                        MTsb, b_MTsb = MTsb_r.get()
                        kb.cp("act", MTsb[:, :].rearrange("p (a b) -> p a b", b=128), pX[:, 0:4, :], r=[b_pX], w=[b_MTsb])
                        kb.dma(MT_s[:, cs], MTsb[:], r=[b_MTsb], w=[b_MT_s])
                    for g3 in range(3 if "g" in PARTS else 0):
                        cts = list(range(4 * g3, 4 * g3 + 4))
                        scs = {}

                        def gproj(ct):
                            pm, b_pm = pm_r.get()
                            for k in range(8):
                                kb.mm(pm[:], Wm[:, k, 1536 + ct * 128:1536 + (ct + 1) * 128], hT[:, k, :], k == 0, k == 7,
                                      r=[b_Wm, b_hT], w=[b_pm])
                            kb.cp("act", raw[:, ct, 3:515], pm[:], r=[b_pm], w=[b_rawc[ct]])

                        gproj(cts[0])
                        for ii, ct in enumerate(cts):
                            if ii + 1 < 4:
                                gproj(cts[ii + 1])
                            brc = b_rawc[ct]
                            acc, b_acc = pm_r.get()
                            for jj in range(4):
                                kb.mm(acc[:], Dg[:, ct, jj, :], raw[:, ct, jj:jj + 512], jj == 0, jj == 3, r=[brc, b_Dg], w=[b_acc])
                            kb.cp("dve", raw[:, ct, 0:3], raw[:, ct, 512:515], r=[brc], w=[brc])
                            sc, b_sc = sc_r.get()
                            kb.act(sc[:], acc[:], AF.Silu, r=[b_acc], w=[b_sc])
                            scs[ct] = (sc, b_sc)
                        nrms = {}
                        if g3 < 2:
                            sqs, pns = {}, {}
                            for ct in cts:
                                sq, b_sq = sq_r.get()
                                kb.act(sq[:], scs[ct][0][:], AF.Square, r=[scs[ct][1]], w=[b_sq])
                                sqs[ct] = (sq, b_sq)
                            for ct in cts:
                                pn, b_pn = pm_r.get()
                                kb.mm(pn[:], onesb[:], sqs[ct][0][:], True, True, r=[b_onesb, sqs[ct][1]], w=[b_pn])
                                pns[ct] = (pn, b_pn)
                            for ct in cts:
                                sc, b_sc = scs[ct]
                                pn, b_pn = pns[ct]
                                rr, b_rr = rr_r.get()
                                kb.act(rr[:], pn[:], AF.Sqrt, r=[b_pn, b_epsc], w=[b_rr], bias=epsc[:, 0:1])
                                kb.recip(rr[:], rr[:], r=[b_rr], w=[b_rr])
                                nrm, b_nrm = nrm_r.get()
                                if ct < 4:
                                    kb.stt("dve", nrm[:], sc[:], 128.0 ** -0.5, rr[:], ALU.mult, ALU.mult, r=[b_sc, b_rr], w=[b_nrm])
                                    kb.dma(qTb_s[ct % 4, :, cs], nrm[:], r=[b_nrm], w=[b_qTb_s])
                                else:
                                    kb.tt("dve", nrm[:], sc[:], rr[:], ALU.mult, r=[b_sc, b_rr], w=[b_nrm])
                                    kb.dma(kTb_s[ct % 4, :, cs], nrm[:], r=[b_nrm], w=[b_kTb_s])
                                nrms[ct] = (nrm, b_nrm)
                        else:
                            for ct in cts:
                                nrm, b_nrm = nrm_r.get()
                                kb.cp("dve", nrm[:], scs[ct][0][:], r=[scs[ct][1]], w=[b_nrm])
                                nrms[ct] = (nrm, b_nrm)
                        if g3 >= 1:
                            for ct in cts:
                                nrm, b_nrm = nrms[ct]
                                head = ct % 4
                                pX, b_pX = pX_r.get()
                                for j in range(4):
                                    kb.tr(pX[:, j, :], nrm[:, j * 128:(j + 1) * 128], identb[:], r=[b_nrm, b_identb], w=[b_pX])
                                tok, b_tok = tok_r.get()
                                kb.cp("act", tok[:], pX[:, 0:4, :], r=[b_pX], w=[b_tok])
                                dst = kb_s if ct < 8 else vb_s
                                bdst = b_kb_s if ct < 8 else b_vb_s
                                kb.dma(dst[4 * c:4 * c + 4, :, head * 128:(head + 1) * 128].rearrange("j t d -> t j d"), tok[:],
                                       r=[b_tok], w=[bdst])
                if dbg and li == 0:
                    kb.dma(dbg_bg[:, :, :], BG[:], r=[b_BG], w=[Buf()])
                S.barrier()

            if li == 0 and stop_after == "A":
                return finish(nc, kb, top, out, b_out)

            with ExitStack() as ph:
                EB, b_EB = kb.sb(ph, [128, 8, 1024], BF16, "EB")
                nb31, b_nb31 = kb.sb(ph, [128, 8], F32, "nb31")
                negm_t, b_negm = kb.sb(ph, [128, 1024], F32, "negm")
                kb.dma(nb31[:], b31[:, :], w=[b_nb31])
                kb.ts("dve", nb31[:], nb31[:], -1.0, None, ALU.mult, r=[b_nb31], w=[b_nb31])
                kb.dma(negm_t[:], negm[:, :], w=[b_negm])
                with ExitStack() as ph2:
                    br_r = kb.ring_sb(ph2, 2, [128, 1024], F32, "braw")
                    for h in range(8):
                        brt, b_brt = br_r.get()
                        kb.dma(brt[:], braw[h, :, :], w=[b_brt])
                        kb.tt("dve", brt[:], brt[:], negm_t[:], ALU.add, r=[b_brt, b_negm], w=[b_brt])
                        kb.act(EB[:, h, :], brt[:], AF.Exp, r=[b_brt, b_nb31], w=[b_EB], bias=nb31[:, h:h + 1])
                    S.barrier()
                qa_r = kb.ring_sb(ph, 2, [128, T], BF16, "qaug")
                ka_r = kb.ring_sb(ph, 2, [128, T], BF16, "kaug")
                for (t_, b_) in qa_r.items + ka_r.items:
                    kb.memset("pool", t_[64:128, :], 0.0, w=[b_])
                vh_r = kb.ring_sb(ph, 2, [128, NT, 65], BF16, "vh")
                for (kt_, bk_) in ka_r.items:
                    kb.dma(kt_[64:80, :], onehotK[:, :], w=[bk_])
                pS_r = kb.ring_ps(ph, 4, [128, 512], F32, "pS")
                pO_r = kb.ring_ps(ph, 2, [128, 4, 128], F32, "pO")
                PT_r = kb.ring_sb(ph, 4, [128, 512], BF16, "PT")
                rec_r = kb.ring_sb(ph, 2, [128, 4, 1], F32, "rec")
                ya_r = kb.ring_sb(ph, 2, [128, 4, 64], BF16, "yat")
                for h in range(8):
                    p, hh = h // 2, h % 2
                    qa_t, b_qa = qa_r.get()
                    ka_t, b_ka = ka_r.get()
                    vh, b_vh = vh_r.get()
                    kb.dma(qa_t[0:64, :], qT_s[p, hh * 64:(hh + 1) * 64, :], r=[b_qT_s], w=[b_qa])
                    kb.dma(qa_t[64:80, :], MT_s[h * 16:(h + 1) * 16, :], r=[b_MT_s], w=[b_qa])
                    kb.dma(ka_t[0:64, :], kT_s[p, hh * 64:(hh + 1) * 64, :], r=[b_kT_s], w=[b_ka])
                    kb.dma(vh[:], v_s[:, :, h * 65:(h + 1) * 65].rearrange("n t d -> t n d"), r=[b_v_s], w=[b_vh])
                    for c in range(8):
                        cs = slice(c * 512, (c + 1) * 512)
                        pO, b_pO = pO_r.get()
                        nk = 4 * c + 4
                        def qk(kt_):
                            pS_, b_pS_ = pS_r.get()
                            kb.mm(pS_[:], ka_t[:, kt_ * 128:(kt_ + 1) * 128], qa_t[:, cs], True, True, r=[b_ka, b_qa], w=[b_pS_])
                            return pS_, b_pS_
                        nxt_qk = [qk(0), qk(1)]
                        for kt in range(nk):
                            pS, b_pS = nxt_qk.pop(0)
                            if kt + 2 < nk:
                                nxt_qk.append(qk(kt + 2))
                            PT, b_PT = PT_r.get()
                            kb.act(PT[:], pS[:], AF.Exp, r=[b_pS], w=[b_PT], scale=0.125)
                            if kt >= 4 * c - 1:
                                off = 512 * c - 128 * kt + 384
                                kb.tt("dve", PT[:], PT[:], EB[:, h, off:off + 512], ALU.mult, r=[b_PT, b_EB], w=[b_PT])
                            for j in range(4):
                                kb.mm(pO[:, j, 0:65], PT[:, j * 128:(j + 1) * 128], vh[:, kt, :],
                                      (kt == 0 and j == 0), (kt == nk - 1), r=[b_PT, b_vh], w=[b_pO])
                        rec, b_rec = rec_r.get()
                        kb.recip(rec[:], pO[:, :, 64:65], r=[b_pO], w=[b_rec])
                        yat, b_yat = ya_r.get()
                        kb.tt("dve", yat[:], pO[:, :, 0:64], rec[:, :, :].to_broadcast([128, 4, 64]), ALU.mult,
                              r=[b_pO, b_rec], w=[b_yat])
                        kb.dma(ya_s[cs, h * 64:(h + 1) * 64].rearrange("(j t) d -> t j d", t=128), yat[:],
                               r=[b_yat], w=[b_ya_s])
                S.barrier()

            if li == 0 and stop_after == "C":
                return finish(nc, kb, top, out, b_out)

            with ExitStack() as ph:
                mneg, b_mneg = kb.sb(ph, [128, 128], F32, "mneg")
                strict, b_strict = kb.sb(ph, [128, 128], F32, "strict")
                ut, b_ut = kb.sb(ph, [128, 128], F32, "ut")
                onw_t, b_onw = kb.sb(ph, [128, 512], F32, "onw")
                kb.dma(mneg[:], mneg_d[:, :], w=[b_mneg])
                kb.dma(strict[:], strict_d[:, :], w=[b_strict])
                kb.dma(ut[:], ut_d[:, :], w=[b_ut])
                kb.dma(onw_t[:], onw[:, :], w=[b_onw])
                Sf, b_Sf = kb.sb(ph, [128, 4, 128], F32, "Sf")
                Sb, b_Sb = kb.sb(ph, [128, 4, 128], BF16, "Sb")
                kb.memset("pool", Sf[:], 0.0, w=[b_Sf])
                kb.memset("pool", Sb[:], 0.0, w=[b_Sb])
                pF_r = kb.ring_ps(ph, 6, [128, 4, 128], F32, "pF")
                pB_r = kb.ring_ps(ph, 2, [128, 8, 128], BF16, "pB")
                R2 = lambda shape, dt, nm, n=2: kb.ring_sb(ph, n, shape, dt, nm)
                kT_r = R2([128, 4, 128], BF16, "gkT", 3)
                qT_r = R2([128, 4, 128], BF16, "gqT", 6)
                ktok_r = R2([128, 4, 128], BF16, "gktok", 3)
                vtok_r = R2([128, 4, 128], BF16, "gvtok", 3)
                z_r = R2([128, 512], F32, "gz", 6)
                gB_r = R2([128, 4, 128], F32, "gB", 3)
                gBn_r = R2([128, 4, 128], F32, "gBn", 3)
                Gcl_r = R2([128, 8], F32, "Gcl", 3)
                eGl_r = R2([128, 8], F32, "eGl", 6)
                f12_r = R2([128, 8], F32, "f12", 3)
                dec_r = R2([128, 4, 128], F32, "dec", 3)
                decS_r = R2([128, 4, 128], F32, "decS", 3)
                tmpL_r = R2([128, 4, 128], F32, "tmpL", 3)
                L_r = R2([128, 4, 128], BF16, "L", 3)
                P_r = R2([128, 4, 128], BF16, "P", 3)
                LP_r = R2([128, 8, 128], BF16, "LP", 6)
                gt_r = R2([128, 4, 128], BF16, "gt", 56)
                bmask, b_bmask = kb.sb(ph, [128, 5, 128], BF16, "bmask")
                kb.dma(bmask[:], bmask_d[:, :, :], w=[b_bmask])
                vb_r = R2([128, 4, 128], BF16, "vb", 3)
                kbg_r = R2([128, 4, 128], BF16, "kbg", 3)
                kd_r = R2([128, 4, 128], BF16, "kd", 6)
                u_r = R2([128, 4, 128], F32, "u", 6)
                wT_r = R2([128, 4, 128], BF16, "wT", 6)
                vn_r = R2([128, 4, 128], BF16, "vn")
                o1_r = R2([128, 4, 128], F32, "o1")
                o_r = R2([128, 4, 128], F32, "o")
                sq_r = R2([128, 4, 128], F32, "osq")
                ss_r = R2([128, 4], F32, "oss")
                yb_r = R2([128, 512], BF16, "ybt")
                yf_r = R2([128, 4, 128], F32, "yf")

                def bc4(ap):
                    return ap.unsqueeze(2).to_broadcast([128, 4, 128])

                def bcm(ap):
                    return ap.unsqueeze(1).to_broadcast([128, 4, 128])

                def act4(dst, src, col, r, w):
                    for h in range(4):
                        kb.act(dst[:, h, :], src[:, h, :], AF.Copy, r=r, w=w, scale=col[:, h:h + 1])

                def mm4(pt, b_pt, lhs, b_lhs, rhs, b_rhs, first=True):
                    for h in range(4):
                        kb.mm(pt[:, h, :], lhs[:, h, :], rhs[:, h, :], first and h == 0, True, r=[b_lhs, b_rhs], w=[b_pt])

                def prep(n):
                    ts_ = slice(n * 128, (n + 1) * 128)
                    kT, b_kT = kT_r.get()
                    qT, b_qT = qT_r.get()
                    ktok, b_ktok = ktok_r.get()
                    vtok, b_vtok = vtok_r.get()
                    z, b_z = z_r.get()
                    kb.dma(kT[:], kTb_s[:, :, ts_].rearrange("h d t -> d h t"), r=[b_kTb_s], w=[b_kT])
                    kb.dma(qT[:], qTb_s[:, :, ts_].rearrange("h d t -> d h t"), r=[b_qTb_s], w=[b_qT])
                    kb.dma(ktok[:, :, :].rearrange("p h d -> p (h d)"), kb_s[n, :, :], r=[b_kb_s], w=[b_ktok])
                    kb.dma(vtok[:, :, :].rearrange("p h d -> p (h d)"), vb_s[n, :, :], r=[b_vb_s], w=[b_vtok])
                    kb.dma(z[:], z_s[n, :, :], r=[b_z_s], w=[b_z])
                    beta = BG[:, n, 0:4]
                    g = BG[:, n, 4:8]
                    gB, b_gB = gB_r.get()
                    gBn, b_gBn = gBn_r.get()
                    kb.tt("pool", gB[:], bcm(onesf[:, :]), bc4(g), ALU.mult, r=[b_onesf, b_BG], w=[b_gB])
                    kb.ts("pool", gBn[:], gB[:], -1.0, None, ALU.mult, r=[b_gB], w=[b_gBn])
                    pG, b_pG = pF_r.get()
                    for h in range(4):
                        kb.mm(pG[:, h, :], ut[:], gB[:, h, :], h == 0, False, r=[b_ut, b_gB], w=[b_pG])
                        kb.mm(pG[:, h, :], gBn[:, h, :], ut[:], False, True, r=[b_ut, b_gBn], w=[b_pG])
                    pC, b_pC = pF_r.get()
                    pCv = pC[:, :, :].rearrange("p a b -> p (a b)")
                    kb.mm(pCv[:, 0:4], ut[:], g, True, True, r=[b_ut, b_BG], w=[b_pC])
                    kb.mm(pCv[:, 4:8], onesf[:], g, False, True, r=[b_onesf, b_BG], w=[b_pC])
                    Gcl, b_Gcl = Gcl_r.get()
                    kb.cp("dve", Gcl[:], pCv[:, 0:8], r=[b_pC], w=[b_Gcl])
                    eGl, b_eGl = eGl_r.get()
                    kb.act(eGl[:], Gcl[:], AF.Exp, r=[b_Gcl], w=[b_eGl])
                    f12, b_f12 = f12_r.get()
                    kb.tt("dve", f12[:, 0:4], beta, eGl[:, 0:4], ALU.mult, r=[b_BG, b_eGl], w=[b_f12])
                    kb.tt("dve", f12[:, 4:8], Gcl[:, 4:8], Gcl[:, 0:4], ALU.subtract, r=[b_Gcl], w=[b_f12])
                    kb.act(f12[:, 4:8], f12[:, 4:8], AF.Exp, r=[b_f12], w=[b_f12])
                    dec, b_dec = dec_r.get()
                    kb.tt("dve", dec[:], pG[:], bcm(mneg[:, :]), ALU.add, r=[b_pG, b_mneg], w=[b_dec])
                    kb.act(dec[:], dec[:], AF.Exp, r=[b_dec], w=[b_dec])
                    yield
                    decS, b_decS = decS_r.get()
                    kb.tt("dve", decS[:], dec[:], bcm(strict[:, :]), ALU.mult, r=[b_dec, b_strict], w=[b_decS])
                    pKK, b_pKK = pF_r.get()
                    mm4(pKK, b_pKK, kT, b_kT, kT, b_kT)
                    tmpL, b_tmpL = tmpL_r.get()
                    kb.tt("dve", tmpL[:], pKK[:], decS[:], ALU.mult, r=[b_pKK, b_decS], w=[b_tmpL])
                    L, b_L = L_r.get()
                    act4(L, tmpL, beta, [b_tmpL, b_BG], [b_L])
                    yield
                    pQK, b_pQK = pF_r.get()
                    mm4(pQK, b_pQK, qT, b_qT, kT, b_kT)
                    P, b_P = P_r.get()
                    kb.tt("dve", P[:], pQK[:], dec[:], ALU.mult, r=[b_pQK, b_dec], w=[b_P])
                    yield
                    pX, b_pX = pB_r.get()
                    for h in range(4):
                        kb.tr(pX[:, h, :], L[:, h, :], identb[:], r=[b_L, b_identb], w=[b_pX])
                    for h in range(4):
                        kb.tr(pX[:, 4 + h, :], P[:, h, :], identb[:], r=[b_P, b_identb], w=[b_pX])
                    LP, b_LP = LP_r.get()
                    kb.cp("act", LP[:], pX[:], r=[b_pX], w=[b_LP])
                    yield
                    LT = LP[:, 0:4, :]
                    PT = LP[:, 4:8, :]
                    cnt = [0]

                    def evac(dst, src_ps, b_src, b_dst, add=None, b_add=None, sub=False):
                        cnt[0] += 1
                        if add is None:
                            kb.cp("act" if cnt[0] % 2 else "dve", dst, src_ps, r=[b_src], w=[b_dst])
                        else:
                            kb.tt("dve", dst, add, src_ps, ALU.subtract if sub else ALU.add, r=[b_src, b_add], w=[b_dst])

                    def newt():
                        return gt_r.get()

                    L8, b_L8 = newt()
                    L8T, b_L8T = newt()
                    kb.tt("pool", L8[:], L[:], bcm(bmask[:, 0, :]), ALU.mult, r=[b_L, b_bmask], w=[b_L8])
                    kb.tt("pool", L8T[:], LT, bcm(bmask[:, 0, :]), ALU.mult, r=[b_LP, b_bmask], w=[b_L8T])
                    T0, b_T0 = newt()
                    T0T, b_T0T = newt()
                    kb.tt("pool", T0[:], bcm(identb[:, :]), L8[:], ALU.subtract, r=[b_identb, b_L8], w=[b_T0])
                    kb.tt("pool", T0T[:], bcm(identb[:, :]), L8T[:], ALU.subtract, r=[b_identb, b_L8T], w=[b_T0T])
                    def mk_E(lv):
                        E, b_E = newt()
                        ET, b_ET = newt()
                        kb.tt("pool", E[:], L[:], bcm(bmask[:, 1 + lv, :]), ALU.mult, r=[b_L, b_bmask], w=[b_E])
                        kb.tt("pool", ET[:], LT, bcm(bmask[:, 1 + lv, :]), ALU.mult, r=[b_LP, b_bmask], w=[b_ET])
                        return (E, b_E, ET, b_ET)
                    pM, b_pM = pF_r.get()
                    mm4(pM, b_pM, L8T, b_L8T, L8, b_L8)
                    M1, b_M1 = newt()
                    evac(M1[:], pM[:], b_pM, b_M1)
                    yield
                    pM, b_pM = pF_r.get()
                    mm4(pM, b_pM, L8, b_L8, L8T, b_L8T)
                    M1T, b_M1T = newt()
                    evac(M1T[:], pM[:], b_pM, b_M1T)
                    yield
                    pM, b_pM = pF_r.get()
                    mm4(pM, b_pM, T0T, b_T0T, M1, b_M1)
                    T1, b_T1 = newt()
                    evac(T1[:], pM[:], b_pM, b_T1, add=T0[:], b_add=b_T0)
                    yield
                    pM, b_pM = pF_r.get()
                    mm4(pM, b_pM, M1, b_M1, T0T, b_T0T)
                    T1T, b_T1T = newt()
                    evac(T1T[:], pM[:], b_pM, b_T1T, add=T0T[:], b_add=b_T0T)
                    yield
                    pM, b_pM = pF_r.get()
                    mm4(pM, b_pM, M1T, b_M1T, M1, b_M1)
                    M2, b_M2 = newt()
                    evac(M2[:], pM[:], b_pM, b_M2)
                    yield
                    pM, b_pM = pF_r.get()
                    mm4(pM, b_pM, T1T, b_T1T, M2, b_M2)
                    Tb, b_Tb = newt()
                    evac(Tb[:], pM[:], b_pM, b_Tb, add=T1[:], b_add=b_T1)
                    yield
                    pM, b_pM = pF_r.get()
                    mm4(pM, b_pM, M2, b_M2, T1T, b_T1T)
                    TbT, b_TbT = newt()
                    evac(TbT[:], pM[:], b_pM, b_TbT, add=T1T[:], b_add=b_T1T)
                    yield
                    nxtE = mk_E(0)
                    for lv in range(4):
                        E, b_E, ET, b_ET = nxtE
                        if lv < 3:
                            nxtE = mk_E(lv + 1)
                        pM, b_pM = pF_r.get()
                        mm4(pM, b_pM, E, b_E, TbT, b_TbT)
                        W1, b_W1 = newt()
                        evac(W1[:], pM[:], b_pM, b_W1)
                        yield
                        if lv < 3:
                            pV, b_pV = pF_r.get()
                            mm4(pV, b_pV, ET, b_ET, Tb, b_Tb)
                            V1, b_V1 = newt()
                            evac(V1[:], pV[:], b_pV, b_V1)
                            yield
                        pM, b_pM = pF_r.get()
                        mm4(pM, b_pM, Tb, b_Tb, W1, b_W1)
                        TnT, b_TnT = newt()
                        evac(TnT[:], pM[:], b_pM, b_TnT, add=TbT[:], b_add=b_TbT, sub=True)
                        yield
                        if lv < 3:
                            pV, b_pV = pF_r.get()
                            mm4(pV, b_pV, TbT, b_TbT, V1, b_V1)
                            Tn, b_Tn = newt()
                            evac(Tn[:], pV[:], b_pV, b_Tn, add=Tb[:], b_add=b_Tb, sub=True)
                            yield
                            Tb, b_Tb = Tn, b_Tn
                        TbT, b_TbT = TnT, b_TnT
                    Tt, b_Tt = TbT, b_TbT
                    vb, b_vb = vb_r.get()
                    kbg, b_kbg = kbg_r.get()
                    kd, b_kd = kd_r.get()
                    act4(vb, vtok, beta, [b_vtok, b_BG], [b_vb])
                    act4(kbg, ktok, f12[:, 0:4], [b_ktok, b_f12], [b_kbg])
                    act4(kd, ktok, f12[:, 4:8], [b_ktok, b_f12], [b_kd])
                    pu, b_pu = pF_r.get()
                    mm4(pu, b_pu, Tt, b_Tt, vb, b_vb)
                    u, b_u = u_r.get()
                    kb.cp("act", u[:], pu[:], r=[b_pu], w=[b_u])
                    pw, b_pw = pF_r.get()
                    mm4(pw, b_pw, kbg, b_kbg, Tt, b_Tt)
                    wT, b_wT = wT_r.get()
                    kb.cp("dve", wT[:], pw[:], r=[b_pw], w=[b_wT])
                    return dict(n=n, qT=(qT, b_qT), PT=(PT, b_LP), u=(u, b_u), wT=(wT, b_wT), kd=(kd, b_kd),
                                eGl=(eGl, b_eGl), z=(z, b_z))

                def scan(st_):
                    n = st_["n"]
                    qT, b_qT = st_["qT"]
                    PT, b_PT = st_["PT"]
                    u, b_u = st_["u"]
                    wT, b_wT = st_["wT"]
                    kd, b_kd = st_["kd"]
                    eGl, b_eGl = st_["eGl"]
                    z, b_z = st_["z"]
                    pwS, b_pwS = pF_r.get()
                    mm4(pwS, b_pwS, wT, b_wT, Sb, b_Sb)
                    vn, b_vn = vn_r.get()
                    kb.tt("dve", vn[:], u[:], pwS[:], ALU.subtract, r=[b_u, b_pwS], w=[b_vn])
                    yield
                    pA1, b_pA1 = pF_r.get()
                    mm4(pA1, b_pA1, qT, b_qT, Sb, b_Sb)
                    o1, b_o1 = o1_r.get()
                    kb.tt("dve", o1[:], pA1[:], bc4(eGl[:, 0:4]), ALU.mult, r=[b_pA1, b_eGl], w=[b_o1])
                    pA2, b_pA2 = pF_r.get()
                    mm4(pA2, b_pA2, PT, b_PT, vn, b_vn)
                    o, b_o = o_r.get()
                    kb.tt("dve", o[:], pA2[:], o1[:], ALU.add, r=[b_pA2, b_o1], w=[b_o])
                    yield
                    pSn, b_pSn = pF_r.get()
                    mm4(pSn, b_pSn, kd, b_kd, vn, b_vn)
                    act4(Sf, Sf, eGl[:, 4:8], [b_Sf, b_eGl, b_Sb], [b_Sf])
                    kb.tt("dve", Sf[:], pSn[:], Sf[:], ALU.add, r=[b_pSn, b_Sf], w=[b_Sf])
                    kb.cp("act", Sb[:], Sf[:], r=[b_Sf], w=[b_Sb])
                    yield
                    sq, b_sq = sq_r.get()
                    kb.act(sq[:], o[:], AF.Square, r=[b_o], w=[b_sq])
                    ss, b_ss = ss_r.get()
                    kb.red("dve", ss[:], sq[:], ALU.add, r=[b_sq], w=[b_ss])
                    kb.ts("dve", ss[:], ss[:], 1.0 / 128, EPS, ALU.mult, ALU.add, r=[b_ss], w=[b_ss])
                    kb.act(ss[:], ss[:], AF.Sqrt, r=[b_ss], w=[b_ss])
                    kb.recip(ss[:], ss[:], r=[b_ss], w=[b_ss])
                    yield
                    yf, b_yf = yf_r.get()
                    kb.tt("dve", yf[:], o[:], bc4(ss[:, :]), ALU.mult, r=[b_o, b_ss], w=[b_yf])
                    yfv = yf[:, :, :].rearrange("p h d -> p (h d)")
                    kb.tt("dve", yfv, yfv, onw_t[:], ALU.mult, r=[b_yf, b_onw], w=[b_yf])
                    ybt, b_ybt = yb_r.get()
                    kb.tt("dve", ybt[:], yfv, z[:], ALU.mult, r=[b_yf, b_z], w=[b_ybt])
                    kb.dma(yb_s[n * 128:(n + 1) * 128, :], ybt[:], r=[b_ybt], w=[b_yb_s])

                def run_rr(gens):
                    results = [None] * len(gens)
                    active = list(range(len(gens)))
                    while active:
                        for gi in list(active):
                            try:
                                next(gens[gi])
                            except StopIteration as ex:
                                results[gi] = ex.value
                                active.remove(gi)
                    return results

                def scan_pair(sa, sb2):
                    yield from scan(sa)
                    yield from scan(sb2)

                WPAR, MAXAHEAD = 3, 5
                next_chunk, next_scan = 0, 0
                preps, done_st, scan_gen = [], {}, None
                while next_scan < NT:
                    while len(preps) < WPAR and next_chunk < NT and (next_chunk - next_scan) < MAXAHEAD:
                        preps.append((next_chunk, prep(next_chunk)))
                        next_chunk += 1
                    for (n_, g_) in list(preps):
                        try:
                            next(g_)
                        except StopIteration as ex:
                            done_st[n_] = ex.value
                            preps.remove((n_, g_))
                    if scan_gen is None and next_scan in done_st:
                        scan_gen = scan(done_st.pop(next_scan))
                    if scan_gen is not None:
                        try:
                            next(scan_gen)
                        except StopIteration:
                            scan_gen = None
                            next_scan += 1
                S.barrier()

            if li == 0 and stop_after == "D":
                return finish(nc, kb, top, out, b_out)

            def make_norm(ph):
                junk_r = kb.ring_sb(ph, 1, [128, D], BF16, "njunk")
                xn_r = kb.ring_sb(ph, 2, [128, D], BF16, "nxn")
                st_r = kb.ring_sb(ph, 4, [128, 2], F32, "nst")
                pT_r = kb.ring_ps(ph, 2, [128, 8, 128], BF16, "npT")

                def norm_tile(xt_ap, b_xt, ai, hv, b_hT):
                    junk, b_junk = junk_r.get()
                    st, b_st = st_r.get()
                    kb.act(junk[:], xt_ap, AF.Square, r=[b_xt], w=[b_junk, b_st], accum_out=st[:, 0:1])
                    kb.ts("dve", st[:, 1:2], st[:, 0:1], 1.0 / D, EPS, ALU.mult, ALU.add, r=[b_st], w=[b_st])
                    kb.act(st[:, 1:2], st[:, 1:2], AF.Sqrt, r=[b_st], w=[b_st])
                    kb.recip(st[:, 1:2], st[:, 1:2], r=[b_st], w=[b_st])
                    xn, b_xn = xn_r.get()
                    kb.act(xn[:], xt_ap, AF.Copy, r=[b_xt, b_st], w=[b_xn], scale=st[:, 1:2])
                    pT, b_pT = pT_r.get()
                    for k in range(8):
                        kb.tr(pT[:, k, :], xn[:, k * 128:(k + 1) * 128], identb[:], r=[b_xn, b_identb], w=[b_pT])
                    kb.tt("dve", hv, pT[:], AB[:, ai, :].unsqueeze(2).to_broadcast([128, 8, 128]), ALU.mult,
                          r=[b_pT, b_AB], w=[b_hT])
                    kb.tt("pool", hv, hv, AB[:, ai + 1, :].unsqueeze(2).to_broadcast([128, 8, 128]), ALU.add,
                          r=[b_hT, b_AB], w=[b_hT])
                return norm_tile

            for hf in ([None] if last else [0, 1]):
                with ExitStack() as phEF:
                    x1, _ = kb.sb(phEF, [128, 16, D], F32, "x1")
                    b_x1 = [Buf() for _ in range(16)]

                    with ExitStack() as ph:
                        Wua, b_Wua = kb.sb(ph, [128, 4, D], BF16, "Wua")
                        Wub, b_Wub = kb.sb(ph, [128, 4, D], BF16, "Wub")
                        Wg, b_Wg = kb.sb(ph, [128, 8, 2 * D], BF16, "Wg")
                        Wo, b_Wo = kb.sb(ph, [128, 8, D], BF16, "Wo")
                        with ExitStack() as ph2:
                            stg = kb.ring_sb(ph2, 2, [128, 8, 512], F32)
                            wuav = w_up_a.rearrange("(k p) n -> p k n", p=128)
                            wubv = w_up_b.rearrange("(k p) n -> p k n", p=128)
                            wov = w_out.rearrange("(k p) n -> p k n", p=128)
                            i = 0
                            for (dst, bd, src, nk, ncol, c0s) in ((Wua, b_Wua, wuav, 4, 2, 0), (Wub, b_Wub, wubv, 4, 2, 0),
                                                                  (Wg, b_Wg, wiv, 8, 4, 3592), (Wo, b_Wo, wov, 8, 2, 0)):
                                for g in range(ncol):
                                    st_t, b_st = stg.get()
                                    kb.dma(st_t[:, 0:nk, :], src[:, :, c0s + g * 512:c0s + (g + 1) * 512], w=[b_st])
                                    kb.cp("pool" if i % 2 else "dve", dst[:, :, g * 512:(g + 1) * 512], st_t[:, 0:nk, :],
                                          r=[b_st], w=[bd])
                                    i += 1
                            S.barrier()
                        norm_tile = make_norm(ph)
                        hoT_r = kb.ring_sb(ph, 1, [128, 8, 512], BF16, "hoT")
                        yaT_r = kb.ring_sb(ph, 1, [128, 4, 512], BF16, "yaT")
                        ybT_r = kb.ring_sb(ph, 1, [128, 4, 512], BF16, "ybT")
                        yl_r = kb.ring_sb(ph, 4, [128, 512], BF16, "yl")
                        yo_r = kb.ring_sb(ph, 2, [128, 512], BF16, "yo")
                        xl_r = kb.ring_sb(ph, 1, [128, D], F32, "xl")
                        pX_r = kb.ring_ps(ph, 1, [128, 8, 128], BF16, "epX")
                        p5_r = kb.ring_ps(ph, 5, [128, 512], F32, "ep5")
                        sg_r = kb.ring_sb(ph, 2, [128, 512], F32, "esg")
                        m12_r = kb.ring_sb(ph, 2, [128, 512], F32, "em12")
                        mT_r = kb.ring_sb(ph, 1, [128, 8, 512], BF16, "mT")
                        tmo_r = kb.ring_sb(ph, 2, [128, 512], F32, "tmo")
                        for c in range(4):
                            hoT, b_hoT = hoT_r.get()
                            yaT, b_yaT = yaT_r.get()
                            ybT, b_ybT = ybT_r.get()
                            for j in range(4):
                                ti = 4 * c + j
                                if hf is not None:
                                    r0 = hf * TO + ti * 128
                                    kb.dma(x1[:, ti, :], xsrc_full[r0:r0 + 128, :], r=[b_xfull_s], w=[b_x1[ti]])
                                elif li == 0:
                                    kb.dma(x1[:, ti, :], xo[ti * 128:(ti + 1) * 128, :], w=[b_x1[ti]])
                                else:
                                    xl, b_xl = xl_r.get()
                                    kb.dma(x1[:, ti, :], xfull_s[ti * 128:(ti + 1) * 128, :], r=[b_xfull_s], w=[b_x1[ti]])
                                    kb.dma(xl[:], xfull_s[TO + ti * 128:TO + (ti + 1) * 128, :], r=[b_xfull_s], w=[b_xl])
                                    kb.ts("dve", x1[:, ti, :], x1[:, ti, :], selt[:, 0:1], None, ALU.mult, r=[b_x1[ti], b_selt], w=[b_x1[ti]])
                                    kb.stt("dve", x1[:, ti, :], xl[:], selt[:, 1:2], x1[:, ti, :], ALU.mult, ALU.add,
                                           r=[b_xl, b_selt, b_x1[ti]], w=[b_x1[ti]])
                                norm_tile(x1[:, ti, :], b_x1[ti], 0, hoT[:, :, j * 128:(j + 1) * 128], b_hoT)
                                for (src, bsrc, dstT, bdst) in ((ya_s, b_ya_s, yaT, b_yaT), (yb_s, b_yb_s, ybT, b_ybT)):
                                    if hf is not None:
                                        yo, b_yo = yl_r.get()
                                        r0 = hf * TO + ti * 128
                                        kb.dma(yo[:], src[r0:r0 + 128, :], r=[bsrc], w=[b_yo])
                                    else:
                                        la, b_la = yl_r.get()
                                        lb, b_lb = yl_r.get()
                                        kb.dma(la[:], src[ti * 128:(ti + 1) * 128, :], r=[bsrc], w=[b_la])
                                        kb.dma(lb[:], src[TO + ti * 128:TO + (ti + 1) * 128, :], r=[bsrc], w=[b_lb])
                                        yo, b_yo = yo_r.get()
                                        kb.ts("dve", yo[:], la[:], selt[:, 0:1], None, ALU.mult, r=[b_la, b_selt], w=[b_yo])
                                        kb.stt("dve", yo[:], lb[:], selt[:, 1:2], yo[:], ALU.mult, ALU.add, r=[b_lb, b_selt, b_yo], w=[b_yo])
                                    pX, b_pX = pX_r.get()
                                    for kc in range(4):
                                        kb.tr(pX[:, kc, :], yo[:, kc * 128:(kc + 1) * 128], identb[:], r=[b_yo, b_identb], w=[b_pX])
                                    kb.cp("act", dstT[:, :, j * 128:(j + 1) * 128], pX[:, 0:4, :], r=[b_pX], w=[bdst])
                            mT, b_mT = mT_r.get()
                            for f in range(8):
                                fs = slice(f * 128, (f + 1) * 128)
                                pUa, b_pUa = p5_r.get()
                                for kc in range(4):
                                    kb.mm(pUa[:], Wua[:, kc, fs], yaT[:, kc, :], kc == 0, kc == 3, r=[b_Wua, b_yaT], w=[b_pUa])
                                pga, b_pga = p5_r.get()
                                for k in range(8):
                                    kb.mm(pga[:], Wg[:, k, fs], hoT[:, k, :], k == 0, k == 7, r=[b_Wg, b_hoT], w=[b_pga])
                                sa, b_sa = sg_r.get()
                                kb.act(sa[:], pga[:], AF.Sigmoid, r=[b_pga], w=[b_sa])
                                m1, b_m1 = m12_r.get()
                                kb.tt("dve", m1[:], pUa[:], sa[:], ALU.mult, r=[b_pUa, b_sa], w=[b_m1])
                                pUb, b_pUb = p5_r.get()
                                for kc in range(4):
                                    kb.mm(pUb[:], Wub[:, kc, fs], ybT[:, kc, :], kc == 0, kc == 3, r=[b_Wub, b_ybT], w=[b_pUb])
                                pgb, b_pgb = p5_r.get()
                                for k in range(8):
                                    kb.mm(pgb[:], Wg[:, k, D + f * 128:D + (f + 1) * 128], hoT[:, k, :], k == 0, k == 7,
                                          r=[b_Wg, b_hoT], w=[b_pgb])
                                sb_, b_sb_ = sg_r.get()
                                kb.act(sb_[:], pgb[:], AF.Sigmoid, r=[b_pgb], w=[b_sb_])
                                m2, b_m2 = m12_r.get()
                                kb.tt("dve", m2[:], pUb[:], sb_[:], ALU.mult, r=[b_pUb, b_sb_], w=[b_m2])
                                kb.tt("pool", mT[:, f, :], m1[:], m2[:], ALU.add, r=[b_m1, b_m2], w=[b_mT])
                            for j in range(4):
                                ti = 4 * c + j
                                for half in range(2):
                                    hs = slice(half * 512, (half + 1) * 512)
                                    pmo, b_pmo = p5_r.get()
                                    for f in range(8):
                                        kb.mm(pmo[:], mT[:, f, j * 128:(j + 1) * 128], Wo[:, f, hs], f == 0, f == 7,
                                              r=[b_mT, b_Wo], w=[b_pmo])
                                    tmo, b_tmo = tmo_r.get()
                                    kb.tt("dve", tmo[:], pmo[:], G1bc[:, hs], ALU.mult, r=[b_pmo, b_G1bc], w=[b_tmo])
                                    kb.tt("pool", x1[:, ti, hs], x1[:, ti, hs], tmo[:], ALU.add, r=[b_tmo, b_x1[ti]], w=[b_x1[ti]])
                        if dbg and li == 0:
                            for ti in range(16):
                                kb.dma(dbg_x1[ti * 128:(ti + 1) * 128, :], x1[:, ti, :], r=[b_x1[ti]], w=[Buf()])
                        S.barrier()

                    if li == 0 and stop_after == "E":
                        return finish(nc, kb, top, out, b_out)

                    with ExitStack() as ph:
                        h2T, _ = kb.sb(ph, [128, 8, TO], BF16, "h2T")
                        b_h2T = [Buf() for _ in range(4)]
                        comb, b_comb = kb.sb(ph, [128, 16, 32], F32, "comb")
                        Wr, b_Wr = kb.sb(ph, [128, 8, 36], BF16, "Wr")
                        Wrf, b_Wrf = kb.sb(ph, [128, 8, 36], F32, "Wrf")
                        brt, b_brt = kb.sb(ph, [128, 36], F32, "brt")
                        kb.dma(Wrf[:], w_r.rearrange("(k p) n -> p k n", p=128), w=[b_Wrf])
                        kb.cp("dve", Wr[:], Wrf[:], r=[b_Wrf], w=[b_Wr])
                        kb.dma(brt[:], b_r[:, :], w=[b_brt])
                        norm_tile = make_norm(ph)
                        p6_r = kb.ring_ps(ph, 6, [128, 512], F32, "fp6")
                        lg_r = kb.ring_sb(ph, 2, [128, 36], F32, "lg")
                        sm_r = kb.ring_sb(ph, 2, [128, 16], F32, "sm")
                        e4_r = kb.ring_sb(ph, 2, [128, 4], F32, "e4")
                        lem_r = kb.ring_sb(ph, 2, [128, 32], F32, "lem")
                        m8_r = kb.ring_sb(ph, 2, [128, 8], F32, "fm8")
                        c12_r = kb.ring_sb(ph, 4, [128, 32], F32, "c12")
                        def route(ti):
                            c = ti // 4
                            tsl = slice(ti * 128, (ti + 1) * 128)
                            norm_tile(x1[:, ti, :], b_x1[ti], 2, h2T[:, :, tsl], b_h2T[c])
                            pl, b_pl = p6_r.get()
                            for k in range(8):
                                kb.mm(pl[:, 0:36], h2T[:, k, tsl], Wr[:, k, :], k == 0, k == 7, r=[b_h2T[c], b_Wr], w=[b_pl])
                            lg, b_lg = lg_r.get()
                            kb.tt("dve", lg[:], pl[:, 0:36], brt[:], ALU.add, r=[b_pl, b_brt], w=[b_lg])
                            sm, b_sm = sm_r.get()
                            kb.red("dve", sm[:, 0:1], lg[:, 0:4], ALU.max, r=[b_lg], w=[b_sm])
                            kb.ts("dve", sm[:, 1:2], sm[:, 0:1], -1.0, None, ALU.mult, r=[b_sm], w=[b_sm])
                            e4, b_e4 = e4_r.get()
                            kb.act(e4[:], lg[:, 0:4], AF.Exp, r=[b_lg, b_sm], w=[b_e4, b_sm], bias=sm[:, 1:2], accum_out=sm[:, 2:3])
                            kb.recip(sm[:, 3:4], sm[:, 2:3], r=[b_sm], w=[b_sm])
                            kb.ts("dve", sm[:, 8:12], lg[:, 0:4], sm[:, 0:1], None, ALU.is_ge, r=[b_lg, b_sm], w=[b_sm])
                            kb.ts("dve", sm[:, 8:12], sm[:, 8:12], -1.0, 1e9, ALU.add, ALU.mult, r=[b_sm], w=[b_sm])
                            lem, b_lem = lem_r.get()
                            kb.tt("dve", lem[:, :].rearrange("p (g e) -> p g e", e=8), lg[:, 4:36].rearrange("p (g e) -> p g e", e=8),
                                  sm[:, 8:12].unsqueeze(2).to_broadcast([128, 4, 8]), ALU.add, r=[b_lg, b_sm], w=[b_lem])
                            m8, b_m8 = m8_r.get()
                            kb.max8(m8[:], lem[:], r=[b_lem], w=[b_m8])
                            kb.tt("dve", sm[:, 4:5], m8[:, 0:1], m8[:, 1:2], ALU.subtract, r=[b_m8], w=[b_sm])
                            kb.act(sm[:, 5:6], sm[:, 4:5], AF.Sigmoid, r=[b_sm], w=[b_sm])
                            kb.tt("dve", sm[:, 6:7], sm[:, 5:6], sm[:, 3:4], ALU.mult, r=[b_sm], w=[b_sm])
                            kb.tt("dve", sm[:, 7:8], sm[:, 3:4], sm[:, 6:7], ALU.subtract, r=[b_sm], w=[b_sm])
                            c1, b_c1 = c12_r.get()
                            c2, b_c2 = c12_r.get()
                            kb.ts("dve", c1[:], lem[:], m8[:, 0:1], sm[:, 6:7], ALU.is_equal, ALU.mult, r=[b_lem, b_m8, b_sm], w=[b_c1])
                            kb.ts("dve", c2[:], lem[:], m8[:, 1:2], sm[:, 7:8], ALU.is_equal, ALU.mult, r=[b_lem, b_m8, b_sm], w=[b_c2])
                            kb.tt("dve", comb[:, ti, :], c1[:], c2[:], ALU.add, r=[b_c1, b_c2], w=[b_comb])
                        stg_r = kb.ring_sb(ph, 3, [128, 2048], F32, "xstg")
                        Wge_r = kb.ring_sb(ph, 2, [128, 8, 256], BF16, "Wge")
                        Wue_r = kb.ring_sb(ph, 2, [128, 8, 256], BF16, "Wue")
                        Wde_r = kb.ring_sb(ph, 2, [128, 2, D], BF16, "Wde")
                        sg_r = kb.ring_sb(ph, 3, [128, 512], F32, "fsg")
                        aT_r = kb.ring_sb(ph, 6, [128, 512], BF16, "aT")
                        tmo_r = kb.ring_sb(ph, 4, [128, 512], F32, "ftmo")
                        for e in range(32):
                            Wge, b_Wge = Wge_r.get()
                            Wue, b_Wue = Wue_r.get()
                            Wde, b_Wde = Wde_r.get()
                            s1, b_s1 = stg_r.get()
                            kb.dma(s1[:, :].rearrange("p (k f) -> p k f", f=256), w_eg[e].rearrange("(k p) f -> p k f", p=128), w=[b_s1])
                            kb.cp("pool", Wge[:, :, :].rearrange("p k f -> p (k f)"), s1[:], r=[b_s1], w=[b_Wge])
                            s2, b_s2 = stg_r.get()
                            kb.dma(s2[:, :].rearrange("p (k f) -> p k f", f=256), w_eu[e].rearrange("(k p) f -> p k f", p=128), w=[b_s2])
                            kb.cp("act", Wue[:, :, :].rearrange("p k f -> p (k f)"), s2[:], r=[b_s2], w=[b_Wue])
                            s3, b_s3 = stg_r.get()
                            kb.dma(s3[:, :].rearrange("p (k n) -> p k n", n=D), w_ed[e].rearrange("(k p) n -> p k n", p=128), w=[b_s3])
                            kb.tt("dve", Wde[:, :, :], s3[:, :].rearrange("p (k n) -> p k n", n=D),
                                  G2bc[:, :].unsqueeze(1).to_broadcast([128, 2, D]), ALU.mult, r=[b_s3, b_G2bc], w=[b_Wde])
                            def gate_up(c):
                                cs = slice(c * 512, (c + 1) * 512)
                                aTs = []
                                for ft in range(2):
                                    fs = slice(ft * 128, (ft + 1) * 128)
                                    pGt, b_pGt = p6_r.get()
                                    for k in range(8):
                                        kb.mm(pGt[:], Wge[:, k, fs], h2T[:, k, cs], k == 0, k == 7, r=[b_Wge, b_h2T[c]], w=[b_pGt])
                                    pUt, b_pUt = p6_r.get()
                                    for k in range(8):
                                        kb.mm(pUt[:], Wue[:, k, fs], h2T[:, k, cs], k == 0, k == 7, r=[b_Wue, b_h2T[c]], w=[b_pUt])
                                    sg, b_sg = sg_r.get()
                                    kb.act(sg[:], pGt[:], AF.Silu, r=[b_pGt], w=[b_sg])
                                    aT, b_aT = aT_r.get()
                                    kb.tt("dve", aT[:], pUt[:], sg[:], ALU.mult, r=[b_pUt, b_sg], w=[b_aT])
                                    aTs.append((aT, b_aT))
                                return aTs

                            def down(c, aTs):
                                for j in range(4):
                                    ti = 4 * c + j
                                    for half in range(2):
                                        hs = slice(half * 512, (half + 1) * 512)
                                        pd, b_pd = p6_r.get()
                                        for ft in range(2):
                                            kb.mm(pd[:], aTs[ft][0][:, j * 128:(j + 1) * 128], Wde[:, ft, hs], ft == 0, ft == 1,
                                                  r=[aTs[ft][1], b_Wde], w=[b_pd])
                                        kb.stt("dve", x1[:, ti, hs], pd[:], comb[:, ti, e:e + 1], x1[:, ti, hs], ALU.mult, ALU.add,
                                               r=[b_pd, b_comb, b_x1[ti]], w=[b_x1[ti]])

                            if e == 0:
                                for ti in range(4):
                                    route(ti)
                            cur_a = gate_up(0)
                            for c in range(4):
                                if e == 0 and c + 1 < 4:
                                    for ti in range(4 * (c + 1), 4 * (c + 2)):
                                        route(ti)
                                nxt_a = gate_up(c + 1) if c + 1 < 4 else None
                                down(c, cur_a)
                                cur_a = nxt_a
                        if final and last:
                            fnw_t, b_fnw = kb.sb(ph, [128, D], F32, "fnw")
                            kb.dma(fnw_t[:], fnw[:, :], w=[b_fnw])
                            fst_r = kb.ring_sb(ph, 2, [128, 2], F32, "fst")
                            fj_r = kb.ring_sb(ph, 2, [128, D], F32, "fj")
                            for ti in range(16):
                                st, b_st = fst_r.get()
                                fj, b_fj = fj_r.get()
                                kb.act(fj[:], x1[:, ti, :], AF.Square, r=[b_x1[ti]], w=[b_fj, b_st], accum_out=st[:, 0:1])
                                kb.ts("dve", st[:, 1:2], st[:, 0:1], 1.0 / D, EPS, ALU.mult, ALU.add, r=[b_st], w=[b_st])
                                kb.act(st[:, 1:2], st[:, 1:2], AF.Sqrt, r=[b_st], w=[b_st])
                                kb.recip(st[:, 1:2], st[:, 1:2], r=[b_st], w=[b_st])
                                kb.act(fj[:], x1[:, ti, :], AF.Copy, r=[b_x1[ti], b_st], w=[b_fj], scale=st[:, 1:2])
                                kb.tt("dve", fj[:], fj[:], fnw_t[:], ALU.mult, r=[b_fj, b_fnw], w=[b_fj])
                                kb.dma(out[ti * 128:(ti + 1) * 128, :], fj[:], r=[b_fj], w=[b_out])
                        elif last:
                            for ti in range(16):
                                kb.dma(out[ti * 128:(ti + 1) * 128, :], x1[:, ti, :], r=[b_x1[ti]], w=[b_out])
                        else:
                            for ti in range(16):
                                r0 = hf * TO + ti * 128
                                kb.dma(xfull_s[r0:r0 + 128, :], x1[:, ti, :], r=[b_x1[ti]], w=[b_xfull_s])
                        S.barrier()
        return finish(nc, kb, top, out, b_out)


def finish(nc, kb, top, out, b_out):
    esem = {e: top.enter_context(nc.semaphore("es_" + e)) for e in ENGS}
    dsem = {}
    for e in ("sp", "pool"):
        for s in range(NDMA):
            dsem[(e, s)] = top.enter_context(nc.semaphore(f"ds_{e}_{s}"))
    with nc.Block() as block:
        kb.S.emit(block, esem, dsem)
    return nc


def make_consts(rel_bias):
    bf = ml_dtypes.bfloat16
    cst = {}
    cst["identb"] = np.eye(128, dtype=np.float32).astype(bf)
    cst["identf"] = np.eye(128, dtype=np.float32)
    oh = np.zeros((16, T), np.float32)
    for n in range(16):
        oh[n, n * 256:(n + 1) * 256] = 1.0
    cst["onehotK"] = oh.astype(bf)
    pn = np.zeros((128, 17, 8, 16), np.float32)
    for qb in range(17):
        pn[:, qb, :, qb:] = -1e30
    cst["padneg"] = pn.reshape(128, 17, 128)
    ki = np.arange(128)[:, None]
    col = np.arange(1024)[None, :]
    dist = col - 384 - ki
    bucket = t5_bucket_np(dist)
    rb = np.asarray(rel_bias, np.float32)
    cst["braw"] = np.ascontiguousarray(np.transpose(rb[bucket], (2, 0, 1)))
    cst["negm"] = np.where(dist >= 0, 0.0, -1e4).astype(np.float32)
    cst["b31"] = np.ascontiguousarray(np.broadcast_to(rb[31][None, :], (128, 8)))
    i = np.arange(128)[:, None]
    j = np.arange(128)[None, :]
    cst["mneg"] = np.where(j <= i, 0.0, -1e5).astype(np.float32)
    cst["strict"] = (j < i).astype(np.float32)
    cst["ut"] = (i <= j).astype(np.float32)
    bm = np.zeros((128, 5, 128), np.float32)
    bm[:, 0, :] = (i // 8 == j // 8)
    for lv, b in enumerate((8, 16, 32, 64)):
        bm[:, 1 + lv, :] = (i // (2 * b) == j // (2 * b)) & (i // b != j // b)
    cst["bmask"] = bm.astype(bf)
    return cst


def fm(v):
    return np.ascontiguousarray(np.asarray(v, np.float32).reshape(8, 128).T)


def layer_inputs(inp, l, cst):
    m = dict(cst)
    m["w_mod"] = np.ascontiguousarray(inp["w_mod"][l])
    m["b_mod"] = np.ascontiguousarray(inp["b_mod"][l][None, :])
    m["n1w"] = fm(inp["norm1_w"][l])
    m["n2w"] = fm(inp["norm2_w"][l])
    m["w_in"] = np.ascontiguousarray(inp["w_in"][l])
    cw = np.asarray(inp["conv_w"][l], np.float32)
    m["convT"] = np.ascontiguousarray(cw.reshape(4, 12, 128).transpose(2, 1, 0))
    m["alog"] = np.ascontiguousarray(np.broadcast_to(inp["a_log"][l][None, :], (128, 4)))
    m["dtb"] = np.ascontiguousarray(np.broadcast_to(inp["dt_bias"][l][None, :], (128, 4)))
    m["onw"] = np.ascontiguousarray(np.broadcast_to(np.tile(inp["onorm_w"][l], 4)[None, :], (128, 512)))
    m["w_up_a"] = np.ascontiguousarray(inp["w_up_a"][l])
    m["w_up_b"] = np.ascontiguousarray(inp["w_up_b"][l])
    m["w_out"] = np.ascontiguousarray(inp["w_out"][l])
    m["w_r"] = np.ascontiguousarray(np.concatenate([inp["w_rg"][l], inp["w_re"][l]], axis=1))
    br = np.concatenate([inp["b_rg"][l], inp["b_re"][l]])
    m["b_r"] = np.ascontiguousarray(np.broadcast_to(br[None, :], (128, 36)))
    m["w_eg"] = np.ascontiguousarray(inp["w_e_gate"][l].reshape(32, D, 256))
    m["w_eu"] = np.ascontiguousarray(inp["w_e_up"][l].reshape(32, D, 256))
    m["w_ed"] = np.ascontiguousarray(inp["w_e_down"][l].reshape(32, 256, D))
    m["fnw"] = np.ascontiguousarray(np.broadcast_to(inp["final_norm_w"][None, :], (128, D)))
    return m


def core_inputs(base, x_full, c, core):
    b, half = core // 2, core % 2
    m = dict(base)
    m["xf"] = np.ascontiguousarray(x_full[b])
    m["xo"] = np.ascontiguousarray(x_full[b, half * TO:(half + 1) * TO])
    m["cT"] = fm(c[b])
    s = np.zeros((128, 2), np.float32)
    s[:, half] = 1.0
    m["sel"] = s
    return m


_PROGS = {}


def _prog():
    if "p" not in _PROGS:
        _PROGS["p"] = build_program(final=True, nl=2)
    return _PROGS["p"]


def kernel(**inputs):
    inp = {k: np.asarray(v) for k, v in inputs.items()}
    cst = make_consts(inp["rel_bias"])
    x = np.asarray(inp["x"], np.float32)
    c = np.asarray(inp["c"], np.float32)
    base = dict(cst)
    shared = set(cst.keys()) | {"fnw"}
    for l in range(2):
        li = layer_inputs(inp, l, cst)
        for k, v in li.items():
            if k in shared:
                base[k] = v
            else:
                base[f"{k}_{l}"] = v
    in_maps = [core_inputs(base, x, c, core) for core in range(8)]
    res = run_bass_kernel_spmd(_prog(), in_maps, core_ids=list(range(8)))
    return np.stack([np.concatenate([res.results[2 * b]["out"], res.results[2 * b + 1]["out"]], axis=0)
                     for b in range(4)]).astype(np.float32)
```

```python
import math
from contextlib import ExitStack

import numpy as np
import ml_dtypes
import concourse.bass as bass
import concourse.mybir as mybir
from concourse.bass_utils import run_bass_kernel_spmd

F32 = mybir.dt.float32
BF16 = mybir.dt.bfloat16
AF = mybir.ActivationFunctionType
ALU = mybir.AluOpType
AX = mybir.AxisListType

D = 1024
T = 4096
TO = 2048
NT = 32
H_A = 8
H_B = 4
D_IN = 5640
EPS = 1e-6
BIG = 30000.0

ENGS = ("pe", "act", "dve", "pool", "sp")
NDMA = 12


class Buf:
    __slots__ = ("lw", "rs", "excl")

    def __init__(self, excl=False):
        self.lw = None
        self.rs = []
        self.excl = excl


class Op:
    __slots__ = ("eng", "fn", "deps", "dma", "needed", "sigval", "dsem", "dval", "prev_slot", "phase")

    def __init__(self, eng, fn, dma, phase):
        self.eng = eng
        self.fn = fn
        self.deps = []
        self.dma = dma
        self.needed = False
        self.sigval = None
        self.dsem = None
        self.dval = None
        self.prev_slot = None
        self.phase = phase


class Sched:
    def __init__(self):
        self.ops = {e: [] for e in ENGS}
        self.dma_slot = {e: 0 for e in ENGS}
        self.dma_last = {}
        self.dma_cnt = {}
        self.phase = 0
        self.last = {e: None for e in ENGS}

    def add(self, eng, fn, reads=(), writes=(), dma=False):
        op = Op(eng, fn, dma, self.phase)
        deps = []
        xr = [b for b in reads if b.excl]
        if xr:
            reads = [b for b in reads if not b.excl]
            writes = list(writes) + xr
        for b in reads:
            if b.lw is not None:
                deps.append(b.lw)
        for b in writes:
            if b.lw is not None:
                deps.append(b.lw)
            deps.extend(b.rs)
        seen = set()
        for d in deps:
            if id(d) in seen or d.phase < self.phase:
                continue
            seen.add(id(d))
            if d.eng == "pe" and eng == "pe" and not d.dma and not dma:
                continue
            op.deps.append(d)
            d.needed = True
        for b in reads:
            if not dma:
                b.rs = [o for o in b.rs if o.dma or o.eng != eng]
            b.rs.append(op)
        for b in writes:
            b.lw = op
            b.rs = []
        if dma:
            slot = self.dma_slot[eng]
            self.dma_slot[eng] = (slot + 1) % NDMA
            key = (eng, slot)
            op.prev_slot = self.dma_last.get(key)
            self.dma_last[key] = op
            self.dma_cnt[key] = self.dma_cnt.get(key, 0) + 16
            op.dsem = key
            op.dval = self.dma_cnt[key]
        else:
            self.last[eng] = op
        self.ops[eng].append(op)
        return op

    def barrier(self):
        deps = []
        for e in ENGS:
            if self.last[e] is not None:
                deps.append(self.last[e])
        deps.extend(self.dma_last.values())
        for e in ENGS:
            op = Op(e, None, False, self.phase)
            for d in deps:
                op.deps.append(d)
                d.needed = True
            self.ops[e].append(op)
        self.phase += 1

    def emit(self, block, esem, dsem):
        for e in ENGS:
            cnt = 0
            for op in self.ops[e]:
                if op.fn is not None and not op.dma and op.needed:
                    cnt += 1
                    op.sigval = cnt
        sched = self

        def run_engine(e, engobj):
            known = {}

            def wait(key, sem, val):
                if known.get(key, 0) >= val:
                    return
                engobj.wait_ge(sem, val)
                known[key] = val

            for op in sched.ops[e]:
                for d in op.deps:
                    if d.dma:
                        wait(d.dsem, dsem[d.dsem], d.dval)
                    else:
                        wait(d.eng, esem[d.eng], d.sigval)
                if op.fn is None:
                    continue
                if op.dma:
                    if op.prev_slot is not None:
                        p = op.prev_slot
                        wait(p.dsem, dsem[p.dsem], p.dval)
                    op.fn(engobj).then_inc(dsem[op.dsem], 16)
                else:
                    ins = op.fn(engobj)
                    if op.needed:
                        ins.then_inc(esem[e], 1)
            for (qe, slot), last in sched.dma_last.items():
                if qe == e:
                    wait(last.dsem, dsem[last.dsem], last.dval)

        block.tensor(lambda eng: run_engine("pe", eng))
        block.scalar(lambda eng: run_engine("act", eng))
        block.vector(lambda eng: run_engine("dve", eng))
        block.gpsimd(lambda eng: run_engine("pool", eng))
        block.sync(lambda eng: run_engine("sp", eng))


class Ring:
    def __init__(self, items):
        self.items = items
        self.i = 0

    def get(self):
        it = self.items[self.i % len(self.items)]
        self.i += 1
        return it


class K:
    def __init__(self, nc):
        self.nc = nc
        self.S = Sched()
        self.uid = 0

    def sb(self, st, shape, dt, name=None):
        self.uid += 1
        t = st.enter_context(self.nc.sbuf_tensor(f"{name or 't'}_{self.uid}", list(shape), dt))
        return t, Buf()

    def ps(self, st, shape, dt, name=None):
        self.uid += 1
        t = st.enter_context(self.nc.psum_tensor(f"{name or 'p'}_{self.uid}", list(shape), dt))
        return t, Buf(excl=True)

    def ring_sb(self, st, n, shape, dt, name=None):
        return Ring([self.sb(st, shape, dt, name) for _ in range(n)])

    def ring_ps(self, st, n, shape, dt, name=None):
        return Ring([self.ps(st, shape, dt, name) for _ in range(n)])

    def dma(self, out, in_, r=(), w=(), q="sp"):
        self.S.add(q, lambda e: e.dma_start(out=out, in_=in_), reads=r, writes=w, dma=True)

    def mm(self, out, lhsT, rhs, start, stop, r=(), w=()):
        self.S.add("pe", lambda e: e.matmul(out, lhsT=lhsT, rhs=rhs, start=start, stop=stop, skip_group_check=True), reads=r, writes=w)

    def tr(self, out, in_, ident, r=(), w=()):
        self.S.add("pe", lambda e: e.transpose(out=out, in_=in_, identity=ident), reads=r, writes=w)

    def act(self, out, in_, func, r=(), w=(), bias=None, scale=None, accum_out=None):
        kw = {}
        if bias is not None:
            kw["bias"] = bias
        if scale is not None:
            kw["scale"] = scale
        if accum_out is not None:
            kw["accum_out"] = accum_out
        self.S.add("act", lambda e: e.activation(out=out, in_=in_, func=func, **kw), reads=r, writes=w)

    def tt(self, eng, out, in0, in1, op, r=(), w=()):
        self.S.add(eng, lambda e: e.tensor_tensor(out=out, in0=in0, in1=in1, op=op), reads=r, writes=w)

    def ts(self, eng, out, in0, s1, s2, op0, op1=None, r=(), w=()):
        if op1 is None:
            self.S.add(eng, lambda e: e.tensor_scalar(out=out, in0=in0, scalar1=s1, scalar2=None, op0=op0),
                       reads=r, writes=w)
        else:
            self.S.add(eng, lambda e: e.tensor_scalar(out=out, in0=in0, scalar1=s1, scalar2=s2, op0=op0, op1=op1),
                       reads=r, writes=w)

    def stt(self, eng, out, in0, scalar, in1, op0, op1, r=(), w=()):
        self.S.add(eng, lambda e: e.scalar_tensor_tensor(out=out, in0=in0, scalar=scalar, in1=in1, op0=op0, op1=op1),
                   reads=r, writes=w)

    def cp(self, eng, out, in_, r=(), w=()):
        if eng == "act":
            self.S.add("act", lambda e: e.activation(out=out, in_=in_, func=AF.Copy), reads=r, writes=w)
        else:
            self.S.add(eng, lambda e: e.tensor_copy(out=out, in_=in_), reads=r, writes=w)

    def memset(self, eng, out, val, r=(), w=()):
        self.S.add(eng, lambda e: e.memset(out, val), reads=r, writes=w)

    def red(self, eng, out, in_, op, r=(), w=()):
        self.S.add(eng, lambda e: e.tensor_reduce(out=out, in_=in_, axis=AX.X, op=op), reads=r, writes=w)

    def recip(self, out, in_, r=(), w=()):
        self.S.add("dve", lambda e: e.reciprocal(out=out, in_=in_), reads=r, writes=w)

    def max8(self, out, in_, r=(), w=()):
        self.S.add("dve", lambda e: e.max(out=out, in_=in_), reads=r, writes=w)


def t5_bucket_np(dist):
    n = np.maximum(dist, 0)
    nf = np.maximum(n, 1).astype(np.float32)
    large = 16 + (np.log(nf / np.float32(16)) / np.float32(math.log(8.0)) * np.float32(16)).astype(np.int32)
    large = np.minimum(large, 31)
    return np.where(n < 16, n, large)


def build_program(final, dbg=False, stop_after=None, nl=1):
    nc = bass.Bass("TRN2", target_bir_lowering=False)
    kb = K(nc)
    S = kb.S

    def din(name, shape, dt=F32):
        return nc.dram_tensor(name, list(shape), dt, kind="ExternalInput").ap()

    def dscr(name, shape, dt):
        return nc.dram_tensor(name, list(shape), dt, kind=("ExternalOutput" if dbg else "Internal")).ap()

    xf = din("xf", [T, D])
    xo = din("xo", [TO, D])
    cT = din("cT", [128, 8])
    sel = din("sel", [128, 2])
    WL = []
    for li_ in range(nl):
        sfx = "" if nl == 1 else f"_{li_}"
        Wd_ = {}
        Wd_["w_mod"] = din("w_mod" + sfx, [D, 6 * D])
        Wd_["b_mod"] = din("b_mod" + sfx, [1, 6 * D])
        Wd_["n1w"] = din("n1w" + sfx, [128, 8])
        Wd_["n2w"] = din("n2w" + sfx, [128, 8])
        Wd_["w_in"] = din("w_in" + sfx, [D, D_IN])
        Wd_["convT"] = din("convT" + sfx, [128, 12, 4])
        Wd_["alog"] = din("alog" + sfx, [128, 4])
        Wd_["dtb"] = din("dtb" + sfx, [128, 4])
        Wd_["onw"] = din("onw" + sfx, [128, 512])
        Wd_["w_up_a"] = din("w_up_a" + sfx, [512, D])
        Wd_["w_up_b"] = din("w_up_b" + sfx, [512, D])
        Wd_["w_out"] = din("w_out" + sfx, [D, D])
        Wd_["w_r"] = din("w_r" + sfx, [D, 36])
        Wd_["b_r"] = din("b_r" + sfx, [128, 36])
        Wd_["w_eg"] = din("w_eg" + sfx, [32, D, 256])
        Wd_["w_eu"] = din("w_eu" + sfx, [32, D, 256])
        Wd_["w_ed"] = din("w_ed" + sfx, [32, 256, D])
        WL.append(Wd_)
    fnw = din("fnw", [128, D])
    identb_d = din("identb", [128, 128], BF16)
    identf_d = din("identf", [128, 128])
    onehotK = din("onehotK", [16, T], BF16)
    padneg_d = din("padneg", [128, 17, 128])
    braw = din("braw", [8, 128, 1024])
    negm = din("negm", [128, 1024])
    b31 = din("b31", [128, 8])
    mneg_d = din("mneg", [128, 128])
    strict_d = din("strict", [128, 128])
    ut_d = din("ut", [128, 128])
    bmask_d = din("bmask", [128, 5, 128], BF16)

    out = nc.dram_tensor("out", [TO, D], F32, kind="ExternalOutput").ap()

    qT_s = dscr("qT_s", [4, 128, T], BF16)
    kT_s = dscr("kT_s", [4, 128, T], BF16)
    MT_s = dscr("MT_s", [128, T], BF16)
    v_s = dscr("v_s", [NT, 128, 520], BF16)
    qTb_s = dscr("qTb_s", [4, 128, T], BF16)
    kTb_s = dscr("kTb_s", [4, 128, T], BF16)
    kb_s = dscr("kb_s", [NT, 128, 512], BF16)
    vb_s = dscr("vb_s", [NT, 128, 512], BF16)
    z_s = dscr("z_s", [NT, 128, 512], F32)
    ya_s = dscr("ya_s", [T, 512], BF16)
    xown_s = nc.dram_tensor("xown_s", [TO, D], F32, kind="Internal").ap()
    xfull_s = nc.dram_tensor("xfull_s", [T, D], F32, kind="Internal").ap()
    b_xown_s, b_xfull_s = Buf(), Buf()
    yb_s = dscr("yb_s", [T, 512], BF16)
    b_qT_s, b_kT_s, b_MT_s, b_v_s = Buf(), Buf(), Buf(), Buf()
    b_qTb_s, b_kTb_s, b_kb_s, b_vb_s, b_z_s, b_ya_s, b_yb_s = Buf(), Buf(), Buf(), Buf(), Buf(), Buf(), Buf()
    if dbg:
        dbg_mod = nc.dram_tensor("dbg_mod", [128, 48], F32, kind="ExternalOutput").ap()
        dbg_bg = nc.dram_tensor("dbg_bg", [128, NT, 8], F32, kind="ExternalOutput").ap()
        dbg_x1 = nc.dram_tensor("dbg_x1", [TO, D], F32, kind="ExternalOutput").ap()
    b_out = Buf()

    with ExitStack() as top:
        identb, b_identb = kb.sb(top, [128, 128], BF16, "identb")
        identf, b_identf = kb.sb(top, [128, 128], F32, "identf")
        onesb, b_onesb = kb.sb(top, [128, 128], BF16, "onesb")
        onesf, b_onesf = kb.sb(top, [128, 128], F32, "onesf")
        epsc, b_epsc = kb.sb(top, [128, 1], F32, "epsc")
        modT, b_modT = kb.sb(top, [128, 48], F32, "modT")
        AB, b_AB = kb.sb(top, [128, 4, 8], F32, "AB")
        G1bc, b_G1bc = kb.sb(top, [128, D], F32, "G1bc")
        G2bc, b_G2bc = kb.sb(top, [128, D], F32, "G2bc")
        BG, b_BG = kb.sb(top, [128, NT, 8], F32, "BG")
        selt, b_selt = kb.sb(top, [128, 2], F32, "selt")
        consts_r = [b_identb, b_identf, b_onesb, b_onesf, b_epsc]

        kb.dma(identb[:], identb_d[:, :], w=[b_identb])
        kb.dma(identf[:], identf_d[:, :], w=[b_identf])
        kb.dma(selt[:], sel[:, :], w=[b_selt])
        kb.memset("pool", onesb[:], 1.0, w=[b_onesb])
        kb.memset("pool", onesf[:], 1.0, w=[b_onesf])
        kb.memset("pool", epsc[:], EPS, w=[b_epsc])

        for li in range(nl):
            Wl = WL[li]
            w_mod = Wl["w_mod"]
            b_mod = Wl["b_mod"]
            n1w = Wl["n1w"]
            n2w = Wl["n2w"]
            w_in = Wl["w_in"]
            convT = Wl["convT"]
            alog = Wl["alog"]
            dtb = Wl["dtb"]
            onw = Wl["onw"]
            w_up_a = Wl["w_up_a"]
            w_up_b = Wl["w_up_b"]
            w_out = Wl["w_out"]
            w_r = Wl["w_r"]
            b_r = Wl["b_r"]
            w_eg = Wl["w_eg"]
            w_eu = Wl["w_eu"]
            w_ed = Wl["w_ed"]
            xsrc_full = xf if li == 0 else xfull_s
            xsrc_own = xo if li == 0 else xown_s
            last = (li == nl - 1)
            with ExitStack() as ph:
                ct_t, b_ct = kb.sb(ph, [128, 8], F32)
                cact, b_cact = kb.sb(ph, [128, 8], F32)
                CB, b_CB = kb.sb(ph, [128, 8, 128], F32)
                bm, b_bm = kb.sb(ph, [1, 6 * D], F32)
                modbc, b_modbc = kb.sb(ph, [128, 6 * D], F32)
                n1t, b_n1t = kb.sb(ph, [128, 8], F32)
                n2t, b_n2t = kb.sb(ph, [128, 8], F32)
                stg = kb.ring_sb(ph, 2, [128, 8, 512], F32)
                pmr = kb.ring_ps(ph, 2, [128, 512], F32)
                kb.dma(ct_t[:], cT[:, :], w=[b_ct])
                kb.dma(bm[:], b_mod[:, :], w=[b_bm])
                kb.dma(n1t[:], n1w[:, :], w=[b_n1t])
                kb.dma(n2t[:], n2w[:, :], w=[b_n2t])
                kb.act(cact[:], ct_t[:], AF.Silu, r=[b_ct], w=[b_cact])
                kb.cp("dve", CB[:], cact[:, :].unsqueeze(2).to_broadcast([128, 8, 128]), r=[b_cact], w=[b_CB])
                wmv = w_mod.rearrange("(k p) n -> p k n", p=128)
                for jg in range(12):
                    st_t, b_st = stg.get()
                    kb.dma(st_t[:], wmv[:, :, jg * 512:(jg + 1) * 512], w=[b_st])
                    pm, b_pm = pmr.get()
                    for k in range(8):
                        kb.mm(pm[:], CB[:, k, :], st_t[:, k, :], k == 0, False, r=[b_CB, b_st], w=[b_pm])
                    kb.mm(pm[:], onesf[0:1, :], bm[0:1, jg * 512:(jg + 1) * 512], False, True, r=[b_onesf, b_bm], w=[b_pm])
                    kb.cp("act" if jg % 2 else "dve", modbc[:, jg * 512:(jg + 1) * 512], pm[:], r=[b_pm], w=[b_modbc])
                with ExitStack() as ph2:
                    prod, b_prod = kb.sb(ph2, [128, 48, 128], F32)
                    kb.tt("dve", prod[:], modbc[:, :].rearrange("p (a b) -> p a b", b=128),
                          identf[:, :].unsqueeze(1).to_broadcast([128, 48, 128]), ALU.mult,
                          r=[b_modbc, b_identf], w=[b_prod])
                    kb.red("dve", modT[:], prod[:], ALU.add, r=[b_prod], w=[b_modT])
                kb.stt("dve", AB[:, 0, :], modT[:, 8:16], 1.0, n1t[:], ALU.add, ALU.mult, r=[b_modT, b_n1t], w=[b_AB])
                kb.cp("dve", AB[:, 1, :], modT[:, 0:8], r=[b_modT], w=[b_AB])
                kb.stt("dve", AB[:, 2, :], modT[:, 32:40], 1.0, n2t[:], ALU.add, ALU.mult, r=[b_modT, b_n2t], w=[b_AB])
                kb.cp("dve", AB[:, 3, :], modT[:, 24:32], r=[b_modT], w=[b_AB])
                kb.cp("act", G1bc[:], modbc[:, 2 * D:3 * D], r=[b_modbc], w=[b_G1bc])
                kb.cp("act", G2bc[:], modbc[:, 5 * D:6 * D], r=[b_modbc], w=[b_G2bc])
                if dbg and li == 0:
                    kb.dma(dbg_mod[:, :], modT[:], r=[b_modT], w=[Buf()])
                S.barrier()

            if li == 0 and stop_after == "M":
                return finish(nc, kb, top, out, b_out)

            with ExitStack() as ph:
                Wm, b_Wm = kb.sb(ph, [128, 8, 3600], BF16, "Wm")
                wiv = w_in.rearrange("(k p) n -> p k n", p=128)
                with ExitStack() as ph2:
                    stg = kb.ring_sb(ph2, 2, [128, 8, 512], F32)
                    for g in range(8):
                        c0 = g * 512
                        cw = min(512, 3600 - c0)
                        st_t, b_st = stg.get()
                        kb.dma(st_t[:, :, 0:cw], wiv[:, :, c0:c0 + cw], w=[b_st])
                        kb.cp("pool" if g % 2 else "dve", Wm[:, :, c0:c0 + cw], st_t[:, :, 0:cw], r=[b_st], w=[b_Wm])
                    S.barrier()
                padneg, b_padneg = kb.sb(ph, [128, 17, 128], F32, "padneg")
                kb.dma(padneg[:], padneg_d[:, :, :], w=[b_padneg])
                cvw, b_cvw = kb.sb(ph, [128, 12, 4], F32, "cvw")
                kb.dma(cvw[:], convT[:, :, :], w=[b_cvw])
                negA, b_negA = kb.sb(ph, [128, 4], F32, "negA")
                dtb_t, b_dtb = kb.sb(ph, [128, 4], F32, "dtb")
                kb.dma(negA[:], alog[:, :], w=[b_negA])
                kb.dma(dtb_t[:], dtb[:, :], w=[b_dtb])
                kb.act(negA[:], negA[:], AF.Exp, r=[b_negA], w=[b_negA])
                kb.ts("dve", negA[:], negA[:], -1.0, None, ALU.mult, r=[b_negA], w=[b_negA])
                KMbd, b_KMbd = kb.sb(ph, [128, 4, 32], BF16, "KMbd")
                kb.memset("pool", KMbd[:], 0.0, w=[b_KMbd])
                raw, b_raw = kb.sb(ph, [128, 12, 515], BF16, "raw")
                Dg, b_Dg = kb.sb(ph, [128, 12, 4, 128], BF16, "Dg")
                for ct_ in range(12):
                    for jj_ in range(4):
                        kb.ts("dve", Dg[:, ct_, jj_, :], identf[:], cvw[:, ct_, jj_:jj_ + 1], None, ALU.mult,
                              r=[b_identf, b_cvw], w=[b_Dg])
                b_rawc = [Buf() for _ in range(12)]
                kb.memset("pool", raw[:, :, 0:3], 0.0, w=b_rawc)

                xt_r = kb.ring_sb(ph, 3, [128, D], F32, "xt")
                junk_r = kb.ring_sb(ph, 2, [128, D], BF16, "junk")
                xn_r = kb.ring_sb(ph, 2, [128, D], BF16, "xn")
                st_r = kb.ring_sb(ph, 4, [128, 2], F32, "st")
                hT_r = kb.ring_sb(ph, 2, [128, 8, 512], BF16, "hT")
                pT_r = kb.ring_ps(ph, 2, [128, 8, 128], BF16, "pT")
                pm_r = kb.ring_ps(ph, 4, [128, 512], F32, "pm")
                pg_r = kb.ring_ps(ph, 1, [128, 512], F32, "pg")
                pX_r = kb.ring_ps(ph, 1, [128, 8, 128], BF16, "pX")
                qsb_r = kb.ring_sb(ph, 8, [128, 512], BF16, "qsb")
                ksb_r = kb.ring_sb(ph, 3, [128, 512], BF16, "ksb")
                km2_r = kb.ring_sb(ph, 2, [128, 2], F32, "km2")
                vsb_r = kb.ring_sb(ph, 3, [128, 8, 65], BF16, "vsb")
                for (vt, bvt) in vsb_r.items:
                    kb.memset("pool", vt[:, :, 64:65], 1.0, w=[bvt])
                zsb_r = kb.ring_sb(ph, 2, [128, 512], F32, "zsb")
                Gs_r = kb.ring_sb(ph, 2, [128, 128], F32, "Gs")
                m8_r = kb.ring_sb(ph, 2, [128, 8, 8], F32, "m8")
                Mbf_r = kb.ring_sb(ph, 5, [128, 128], BF16, "Mbf")
                MTsb_r = kb.ring_sb(ph, 2, [128, 512], BF16, "MTsb")
                t4_r = kb.ring_sb(ph, 4, [128, 8], F32, "t4")
                sc_r = kb.ring_sb(ph, 6, [128, 512], F32, "sc")
                sq_r = kb.ring_sb(ph, 4, [128, 512], BF16, "sq")
                rr_r = kb.ring_sb(ph, 3, [128, 512], F32, "rr")
                nrm_r = kb.ring_sb(ph, 5, [128, 512], BF16, "nrm")
                tok_r = kb.ring_sb(ph, 3, [128, 4, 128], BF16, "tok")

                def do_norm(c):
                    hT, b_hT = hT_r.get()
                    for j in range(4):
                        ti = 4 * c + j
                        xt, b_xt = xt_r.get()
                        kb.dma(xt[:], xsrc_full[ti * 128:(ti + 1) * 128, :], r=[b_xfull_s], w=[b_xt])
                        junk, b_junk = junk_r.get()
                        st, b_st = st_r.get()
                        kb.act(junk[:], xt[:], AF.Square, r=[b_xt], w=[b_junk, b_st], accum_out=st[:, 0:1])
                        kb.ts("dve", st[:, 1:2], st[:, 0:1], 1.0 / D, EPS, ALU.mult, ALU.add, r=[b_st], w=[b_st])
                        kb.act(st[:, 1:2], st[:, 1:2], AF.Sqrt, r=[b_st], w=[b_st])
                        kb.recip(st[:, 1:2], st[:, 1:2], r=[b_st], w=[b_st])
                        xn, b_xn = xn_r.get()
                        kb.act(xn[:], xt[:], AF.Copy, r=[b_xt, b_st], w=[b_xn], scale=st[:, 1:2])
                        pT, b_pT = pT_r.get()
                        for k in range(8):
                            kb.tr(pT[:, k, :], xn[:, k * 128:(k + 1) * 128], identb[:], r=[b_xn, b_identb], w=[b_pT])
                        hv = hT[:, :, j * 128:(j + 1) * 128]
                        kb.tt("dve", hv, pT[:], AB[:, 0, :].unsqueeze(2).to_broadcast([128, 8, 128]), ALU.mult,
                              r=[b_pT, b_AB], w=[b_hT])
                        kb.tt("pool", hv, hv, AB[:, 1, :].unsqueeze(2).to_broadcast([128, 8, 128]), ALU.add,
                              r=[b_hT, b_AB], w=[b_hT])
                    return hT, b_hT

                nxt_h = do_norm(0)
                for c in range(8):
                    hT, b_hT = nxt_h
                    if c + 1 < 8:
                        nxt_h = do_norm(c + 1)
                    cs = slice(c * 512, (c + 1) * 512)
                    import os
                    PARTS = os.environ.get("KPARTS", "kqsvg")
                    for p in range(4 if "k" in PARTS else 0):
                        pm, b_pm = pm_r.get()
                        for k in range(8):
                            kb.mm(pm[:], Wm[:, k, 512 + p * 128:512 + (p + 1) * 128], hT[:, k, :], k == 0, k == 7,
                                  r=[b_Wm, b_hT], w=[b_pm])
                        ksb, b_ksb = ksb_r.get()
                        kb.cp("act", ksb[:], pm[:], r=[b_pm], w=[b_ksb])
                        kb.dma(kT_s[p, :, cs], ksb[:], r=[b_ksb], w=[b_kT_s])
                        km2, b_km2 = km2_r.get()
                        kb.red("dve", km2[:], pm[:, :].rearrange("p (a b) -> p a b", b=256), ALU.add, r=[b_pm], w=[b_km2])
                        kb.cp("dve", KMbd[0:64, p, 2 * c:2 * c + 2], km2[0:64, :], r=[b_km2], w=[b_KMbd])
                        kb.cp("dve", KMbd[64:128, p, 16 + 2 * c:16 + 2 * c + 2], km2[64:128, :], r=[b_km2], w=[b_KMbd])
                    qs = []
                    for p in range(4 if "q" in PARTS else 0):
                        pm, b_pm = pm_r.get()
                        for k in range(8):
                            kb.mm(pm[:], Wm[:, k, p * 128:(p + 1) * 128], hT[:, k, :], k == 0, k == 7,
                                  r=[b_Wm, b_hT], w=[b_pm])
                        qsb, b_qsb = qsb_r.get()
                        kb.cp("act", qsb[:], pm[:], r=[b_pm], w=[b_qsb])
                        kb.dma(qT_s[p, :, cs], qsb[:], r=[b_qsb], w=[b_qT_s])
                        qs.append((qsb, b_qsb))
                    Mbfs = []
                    for j in range(4 if "s" in PARTS else 0):
                        ti = 4 * c + j
                        qb = ti // 2
                        Mbf, b_Mbf = Mbf_r.get()
                        if qb < 4:
                            kb.ts("dve", Mbf[:], padneg[:, qb + 1, :], -BIG, None, ALU.max, r=[b_padneg], w=[b_Mbf])
                        else:
                            pg, b_pg = pg_r.get()
                            for p in range(4):
                                kb.mm(pg[:, p * 32:(p + 1) * 32], qs[p][0][:, j * 128:(j + 1) * 128], KMbd[:, p, :], True, True,
                                      r=[qs[p][1], b_KMbd], w=[b_pg])
                            Gs, b_Gs = Gs_r.get()
                            kb.tt("dve", Gs[:], pg[:, 0:128], padneg[:, qb, :], ALU.add, r=[b_pg, b_padneg], w=[b_Gs])
                            m8, b_m8 = m8_r.get()
                            for h in range(8):
                                kb.max8(m8[:, h, :], Gs[:, h * 16:(h + 1) * 16], r=[b_Gs], w=[b_m8])
                            G3 = Gs[:, :].rearrange("p (h n) -> p h n", n=16)
                            kb.tt("dve", G3, G3, m8[:, :, 2:3].to_broadcast([128, 8, 16]), ALU.is_ge, r=[b_Gs, b_m8], w=[b_Gs])
                            M3 = Mbf[:, :].rearrange("p (h n) -> p h n", n=16)
                            kb.ts("dve", M3, G3, -1.0, BIG, ALU.add, ALU.mult, r=[b_Gs], w=[b_Mbf])
                            kb.memset("dve", M3[:, :, qb:qb + 1], 0.0, r=[], w=[b_Mbf])
                        Mbfs.append((Mbf, b_Mbf))
                    nj = 4 if "v" in PARTS else 0
                    for j in range(nj):
                        ti = 4 * c + j
                        hs = slice(j * 128, (j + 1) * 128)
                        pm, b_pm = pm_r.get()
                        for k in range(8):
                            kb.mm(pm[:], hT[:, k, hs], Wm[:, k, 1024:1536], k == 0, k == 7, r=[b_Wm, b_hT], w=[b_pm])
                        vsb, b_vsb = vsb_r.get()
                        kb.cp("dve", vsb[:, :, 0:64], pm[:, :].rearrange("p (h d) -> p h d", d=64), r=[b_pm], w=[b_vsb])
                        kb.dma(v_s[ti, :, :], vsb[:, :, :].rearrange("p h d -> p (h d)"), r=[b_vsb], w=[b_v_s])
                    t4s = []
                    for j in range(nj):
                        ti = 4 * c + j
                        hs = slice(j * 128, (j + 1) * 128)
                        pm, b_pm = pm_r.get()
                        for k in range(8):
                            kb.mm(pm[:, 0:8], hT[:, k, hs], Wm[:, k, 3584:3592], k == 0, k == 7, r=[b_Wm, b_hT], w=[b_pm])
                        t4, b_t4 = t4_r.get()
                        kb.cp("dve", t4[:, 0:4], pm[:, 0:4], r=[b_pm], w=[b_t4])
                        kb.tt("dve", t4[:, 4:8], pm[:, 4:8], dtb_t[:], ALU.add, r=[b_pm, b_dtb], w=[b_t4])
                        t4s.append((t4, b_t4))
                    for j in range(nj):
                        t4, b_t4 = t4s[j]
                        kb.act(BG[:, 4 * c + j, 0:4], t4[:, 0:4], AF.Sigmoid, r=[b_t4], w=[b_BG])
                    for j in range(nj):
                        ti = 4 * c + j
                        hs = slice(j * 128, (j + 1) * 128)
                        pm, b_pm = pm_r.get()
                        for k in range(8):
                            kb.mm(pm[:], hT[:, k, hs], Wm[:, k, 3072:3584], k == 0, k == 7, r=[b_Wm, b_hT], w=[b_pm])
                        zsb, b_zsb = zsb_r.get()
                        kb.act(zsb[:], pm[:], AF.Silu, r=[b_pm], w=[b_zsb])
                        kb.dma(z_s[ti, :, :], zsb[:], r=[b_zsb], w=[b_z_s])
                    for j in range(nj):
                        t4, b_t4 = t4s[j]
                        kb.act(t4[:, 4:8], t4[:, 4:8], AF.Exp, r=[b_t4], w=[b_t4])
                    for j in range(nj):
                        t4, b_t4 = t4s[j]
                        kb.act(t4[:, 4:8], t4[:, 4:8], AF.Ln, r=[b_t4], w=[b_t4], bias=1.0)
                        kb.tt("dve", BG[:, 4 * c + j, 4:8], t4[:, 4:8], negA[:], ALU.mult, r=[b_t4, b_negA], w=[b_BG])
                    if "s" in PARTS:
                        pX, b_pX = pX_r.get()
                        for j in range(4):
                            kb.tr(pX[:, j, :], Mbfs[j][0][:], identb[:], r=[Mbfs[j][1], b_identb], w=[b_pX])
                        MTsb, b_MTsb = MTsb_r.get()
                        kb.cp("act", MTsb[:, :].rearrange("p (a b) -> p a b", b=128), pX[:, 0:4, :], r=[b_pX], w=[b_MTsb])
                        kb.dma(MT_s[:, cs], MTsb[:], r=[b_MTsb], w=[b_MT_s])
                    for g3 in range(3 if "g" in PARTS else 0):
                        cts = list(range(4 * g3, 4 * g3 + 4))
                        scs = {}

                        def gproj(ct):
                            pm, b_pm = pm_r.get()
                            for k in range(8):
                                kb.mm(pm[:], Wm[:, k, 1536 + ct * 128:1536 + (ct + 1) * 128], hT[:, k, :], k == 0, k == 7,
                                      r=[b_Wm, b_hT], w=[b_pm])
                            kb.cp("act", raw[:, ct, 3:515], pm[:], r=[b_pm], w=[b_rawc[ct]])

                        gproj(cts[0])
                        for ii, ct in enumerate(cts):
                            if ii + 1 < 4:
                                gproj(cts[ii + 1])
                            brc = b_rawc[ct]
                            acc, b_acc = pm_r.get()
                            for jj in range(4):
                                kb.mm(acc[:], Dg[:, ct, jj, :], raw[:, ct, jj:jj + 512], jj == 0, jj == 3, r=[brc, b_Dg], w=[b_acc])
                            kb.cp("dve", raw[:, ct, 0:3], raw[:, ct, 512:515], r=[brc], w=[brc])
                            sc, b_sc = sc_r.get()
                            kb.act(sc[:], acc[:], AF.Silu, r=[b_acc], w=[b_sc])
                            scs[ct] = (sc, b_sc)
                        nrms = {}
                        if g3 < 2:
                            sqs, pns = {}, {}
                            for ct in cts:
                                sq, b_sq = sq_r.get()
                                kb.act(sq[:], scs[ct][0][:], AF.Square, r=[scs[ct][1]], w=[b_sq])
                                sqs[ct] = (sq, b_sq)
                            for ct in cts:
                                pn, b_pn = pm_r.get()
                                kb.mm(pn[:], onesb[:], sqs[ct][0][:], True, True, r=[b_onesb, sqs[ct][1]], w=[b_pn])
                                pns[ct] = (pn, b_pn)
                            for ct in cts:
                                sc, b_sc = scs[ct]
                                pn, b_pn = pns[ct]
                                rr, b_rr = rr_r.get()
                                kb.act(rr[:], pn[:], AF.Sqrt, r=[b_pn, b_epsc], w=[b_rr], bias=epsc[:, 0:1])
                                kb.recip(rr[:], rr[:], r=[b_rr], w=[b_rr])
                                nrm, b_nrm = nrm_r.get()
                                if ct < 4:
                                    kb.stt("dve", nrm[:], sc[:], 128.0 ** -0.5, rr[:], ALU.mult, ALU.mult, r=[b_sc, b_rr], w=[b_nrm])
                                    kb.dma(qTb_s[ct % 4, :, cs], nrm[:], r=[b_nrm], w=[b_qTb_s])
                                else:
                                    kb.tt("dve", nrm[:], sc[:], rr[:], ALU.mult, r=[b_sc, b_rr], w=[b_nrm])
                                    kb.dma(kTb_s[ct % 4, :, cs], nrm[:], r=[b_nrm], w=[b_kTb_s])
                                nrms[ct] = (nrm, b_nrm)
                        else:
                            for ct in cts:
                                nrm, b_nrm = nrm_r.get()
                                kb.cp("dve", nrm[:], scs[ct][0][:], r=[scs[ct][1]], w=[b_nrm])
                                nrms[ct] = (nrm, b_nrm)
                        if g3 >= 1:
                            for ct in cts:
                                nrm, b_nrm = nrms[ct]
                                head = ct % 4
                                pX, b_pX = pX_r.get()
                                for j in range(4):
                                    kb.tr(pX[:, j, :], nrm[:, j * 128:(j + 1) * 128], identb[:], r=[b_nrm, b_identb], w=[b_pX])
                                tok, b_tok = tok_r.get()
                                kb.cp("act", tok[:], pX[:, 0:4, :], r=[b_pX], w=[b_tok])
                                dst = kb_s if ct < 8 else vb_s
                                bdst = b_kb_s if ct < 8 else b_vb_s
                                kb.dma(dst[4 * c:4 * c + 4, :, head * 128:(head + 1) * 128].rearrange("j t d -> t j d"), tok[:],
                                       r=[b_tok], w=[bdst])
                if dbg and li == 0:
                    kb.dma(dbg_bg[:, :, :], BG[:], r=[b_BG], w=[Buf()])
                S.barrier()

            if li == 0 and stop_after == "A":
                return finish(nc, kb, top, out, b_out)

            with ExitStack() as ph:
                EB, b_EB = kb.sb(ph, [128, 8, 1024], BF16, "EB")
                nb31, b_nb31 = kb.sb(ph, [128, 8], F32, "nb31")
                negm_t, b_negm = kb.sb(ph, [128, 1024], F32, "negm")
                kb.dma(nb31[:], b31[:, :], w=[b_nb31])
                kb.ts("dve", nb31[:], nb31[:], -1.0, None, ALU.mult, r=[b_nb31], w=[b_nb31])
                kb.dma(negm_t[:], negm[:, :], w=[b_negm])
                with ExitStack() as ph2:
                    br_r = kb.ring_sb(ph2, 2, [128, 1024], F32, "braw")
                    for h in range(8):
                        brt, b_brt = br_r.get()
                        kb.dma(brt[:], braw[h, :, :], w=[b_brt])
                        kb.tt("dve", brt[:], brt[:], negm_t[:], ALU.add, r=[b_brt, b_negm], w=[b_brt])
                        kb.act(EB[:, h, :], brt[:], AF.Exp, r=[b_brt, b_nb31], w=[b_EB], bias=nb31[:, h:h + 1])
                    S.barrier()
                qa_r = kb.ring_sb(ph, 2, [128, T], BF16, "qaug")
                ka_r = kb.ring_sb(ph, 2, [128, T], BF16, "kaug")
                for (t_, b_) in qa_r.items + ka_r.items:
                    kb.memset("pool", t_[64:128, :], 0.0, w=[b_])
                vh_r = kb.ring_sb(ph, 2, [128, NT, 65], BF16, "vh")
                for (kt_, bk_) in ka_r.items:
                    kb.dma(kt_[64:80, :], onehotK[:, :], w=[bk_])
                pS_r = kb.ring_ps(ph, 4, [128, 512], F32, "pS")
                pO_r = kb.ring_ps(ph, 2, [128, 4, 128], F32, "pO")
                PT_r = kb.ring_sb(ph, 4, [128, 512], BF16, "PT")
                rec_r = kb.ring_sb(ph, 2, [128, 4, 1], F32, "rec")
                ya_r = kb.ring_sb(ph, 2, [128, 4, 64], BF16, "yat")
                for h in range(8):
                    p, hh = h // 2, h % 2
                    qa_t, b_qa = qa_r.get()
                    ka_t, b_ka = ka_r.get()
                    vh, b_vh = vh_r.get()
                    kb.dma(qa_t[0:64, :], qT_s[p, hh * 64:(hh + 1) * 64, :], r=[b_qT_s], w=[b_qa])
                    kb.dma(qa_t[64:80, :], MT_s[h * 16:(h + 1) * 16, :], r=[b_MT_s], w=[b_qa])
                    kb.dma(ka_t[0:64, :], kT_s[p, hh * 64:(hh + 1) * 64, :], r=[b_kT_s], w=[b_ka])
                    kb.dma(vh[:], v_s[:, :, h * 65:(h + 1) * 65].rearrange("n t d -> t n d"), r=[b_v_s], w=[b_vh])
                    for c in range(8):
                        cs = slice(c * 512, (c + 1) * 512)
                        pO, b_pO = pO_r.get()
                        nk = 4 * c + 4
                        def qk(kt_):
                            pS_, b_pS_ = pS_r.get()
                            kb.mm(pS_[:], ka_t[:, kt_ * 128:(kt_ + 1) * 128], qa_t[:, cs], True, True, r=[b_ka, b_qa], w=[b_pS_])
                            return pS_, b_pS_
                        nxt_qk = [qk(0), qk(1)]
                        for kt in range(nk):
                            pS, b_pS = nxt_qk.pop(0)
                            if kt + 2 < nk:
                                nxt_qk.append(qk(kt + 2))
                            PT, b_PT = PT_r.get()
                            kb.act(PT[:], pS[:], AF.Exp, r=[b_pS], w=[b_PT], scale=0.125)
                            if kt >= 4 * c - 1:
                                off = 512 * c - 128 * kt + 384
                                kb.tt("dve", PT[:], PT[:], EB[:, h, off:off + 512], ALU.mult, r=[b_PT, b_EB], w=[b_PT])
                            for j in range(4):
                                kb.mm(pO[:, j, 0:65], PT[:, j * 128:(j + 1) * 128], vh[:, kt, :],
                                      (kt == 0 and j == 0), (kt == nk - 1), r=[b_PT, b_vh], w=[b_pO])
                        rec, b_rec = rec_r.get()
                        kb.recip(rec[:], pO[:, :, 64:65], r=[b_pO], w=[b_rec])
                        yat, b_yat = ya_r.get()
                        kb.tt("dve", yat[:], pO[:, :, 0:64], rec[:, :, :].to_broadcast([128, 4, 64]), ALU.mult,
                              r=[b_pO, b_rec], w=[b_yat])
                        kb.dma(ya_s[cs, h * 64:(h + 1) * 64].rearrange("(j t) d -> t j d", t=128), yat[:],
                               r=[b_yat], w=[b_ya_s])
                S.barrier()

            if li == 0 and stop_after == "C":
                return finish(nc, kb, top, out, b_out)

            with ExitStack() as ph:
                mneg, b_mneg = kb.sb(ph, [128, 128], F32, "mneg")
                strict, b_strict = kb.sb(ph, [128, 128], F32, "strict")
                ut, b_ut = kb.sb(ph, [128, 128], F32, "ut")
                onw_t, b_onw = kb.sb(ph, [128, 512], F32, "onw")
                kb.dma(mneg[:], mneg_d[:, :], w=[b_mneg])
                kb.dma(strict[:], strict_d[:, :], w=[b_strict])
                kb.dma(ut[:], ut_d[:, :], w=[b_ut])
                kb.dma(onw_t[:], onw[:, :], w=[b_onw])
                Sf, b_Sf = kb.sb(ph, [128, 4, 128], F32, "Sf")
                Sb, b_Sb = kb.sb(ph, [128, 4, 128], BF16, "Sb")
                kb.memset("pool", Sf[:], 0.0, w=[b_Sf])
                kb.memset("pool", Sb[:], 0.0, w=[b_Sb])
                pF_r = kb.ring_ps(ph, 6, [128, 4, 128], F32, "pF")
                pB_r = kb.ring_ps(ph, 2, [128, 8, 128], BF16, "pB")
                R2 = lambda shape, dt, nm, n=2: kb.ring_sb(ph, n, shape, dt, nm)
                kT_r = R2([128, 4, 128], BF16, "gkT", 3)
                qT_r = R2([128, 4, 128], BF16, "gqT", 6)
                ktok_r = R2([128, 4, 128], BF16, "gktok", 3)
                vtok_r = R2([128, 4, 128], BF16, "gvtok", 3)
                z_r = R2([128, 512], F32, "gz", 6)
                gB_r = R2([128, 4, 128], F32, "gB", 3)
                gBn_r = R2([128, 4, 128], F32, "gBn", 3)
                Gcl_r = R2([128, 8], F32, "Gcl", 3)
                eGl_r = R2([128, 8], F32, "eGl", 6)
                f12_r = R2([128, 8], F32, "f12", 3)
                dec_r = R2([128, 4, 128], F32, "dec", 3)
                decS_r = R2([128, 4, 128], F32, "decS", 3)
                tmpL_r = R2([128, 4, 128], F32, "tmpL", 3)
                L_r = R2([128, 4, 128], BF16, "L", 3)
                P_r = R2([128, 4, 128], BF16, "P", 3)
                LP_r = R2([128, 8, 128], BF16, "LP", 6)
                gt_r = R2([128, 4, 128], BF16, "gt", 56)
                bmask, b_bmask = kb.sb(ph, [128, 5, 128], BF16, "bmask")
                kb.dma(bmask[:], bmask_d[:, :, :], w=[b_bmask])
                vb_r = R2([128, 4, 128], BF16, "vb", 3)
                kbg_r = R2([128, 4, 128], BF16, "kbg", 3)
                kd_r = R2([128, 4, 128], BF16, "kd", 6)
                u_r = R2([128, 4, 128], F32, "u", 6)
                wT_r = R2([128, 4, 128], BF16, "wT", 6)
                vn_r = R2([128, 4, 128], BF16, "vn")
                o1_r = R2([128, 4, 128], F32, "o1")
                o_r = R2([128, 4, 128], F32, "o")
                sq_r = R2([128, 4, 128], F32, "osq")
                ss_r = R2([128, 4], F32, "oss")
                yb_r = R2([128, 512], BF16, "ybt")
                yf_r = R2([128, 4, 128], F32, "yf")

                def bc4(ap):
                    return ap.unsqueeze(2).to_broadcast([128, 4, 128])

                def bcm(ap):
                    return ap.unsqueeze(1).to_broadcast([128, 4, 128])

                def act4(dst, src, col, r, w):
                    for h in range(4):
                        kb.act(dst[:, h, :], src[:, h, :], AF.Copy, r=r, w=w, scale=col[:, h:h + 1])

                def mm4(pt, b_pt, lhs, b_lhs, rhs, b_rhs, first=True):
                    for h in range(4):
                        kb.mm(pt[:, h, :], lhs[:, h, :], rhs[:, h, :], first and h == 0, True, r=[b_lhs, b_rhs], w=[b_pt])

                def prep(n):
                    ts_ = slice(n * 128, (n + 1) * 128)
                    kT, b_kT = kT_r.get()
                    qT, b_qT = qT_r.get()
                    ktok, b_ktok = ktok_r.get()
                    vtok, b_vtok = vtok_r.get()
                    z, b_z = z_r.get()
                    kb.dma(kT[:], kTb_s[:, :, ts_].rearrange("h d t -> d h t"), r=[b_kTb_s], w=[b_kT])
                    kb.dma(qT[:], qTb_s[:, :, ts_].rearrange("h d t -> d h t"), r=[b_qTb_s], w=[b_qT])
                    kb.dma(ktok[:, :, :].rearrange("p h d -> p (h d)"), kb_s[n, :, :], r=[b_kb_s], w=[b_ktok])
                    kb.dma(vtok[:, :, :].rearrange("p h d -> p (h d)"), vb_s[n, :, :], r=[b_vb_s], w=[b_vtok])
                    kb.dma(z[:], z_s[n, :, :], r=[b_z_s], w=[b_z])
                    beta = BG[:, n, 0:4]
                    g = BG[:, n, 4:8]
                    gB, b_gB = gB_r.get()
                    gBn, b_gBn = gBn_r.get()
                    kb.tt("pool", gB[:], bcm(onesf[:, :]), bc4(g), ALU.mult, r=[b_onesf, b_BG], w=[b_gB])
                    kb.ts("dve", gBn[:], gB[:], -1.0, None, ALU.mult, r=[b_gB], w=[b_gBn])
                    pG, b_pG = pF_r.get()
                    for h in range(4):
                        kb.mm(pG[:, h, :], ut[:], gB[:, h, :], h == 0, False, r=[b_ut, b_gB], w=[b_pG])
                        kb.mm(pG[:, h, :], gBn[:, h, :], ut[:], False, True, r=[b_ut, b_gBn], w=[b_pG])
                    pC, b_pC = pF_r.get()
                    pCv = pC[:, :, :].rearrange("p a b -> p (a b)")
                    kb.mm(pCv[:, 0:4], ut[:], g, True, True, r=[b_ut, b_BG], w=[b_pC])
                    kb.mm(pCv[:, 4:8], onesf[:], g, False, True, r=[b_onesf, b_BG], w=[b_pC])
                    Gcl, b_Gcl = Gcl_r.get()
                    kb.cp("dve", Gcl[:], pCv[:, 0:8], r=[b_pC], w=[b_Gcl])
                    eGl, b_eGl = eGl_r.get()
                    kb.act(eGl[:], Gcl[:], AF.Exp, r=[b_Gcl], w=[b_eGl])
                    f12, b_f12 = f12_r.get()
                    kb.tt("dve", f12[:, 0:4], beta, eGl[:, 0:4], ALU.mult, r=[b_BG, b_eGl], w=[b_f12])
                    kb.tt("dve", f12[:, 4:8], Gcl[:, 4:8], Gcl[:, 0:4], ALU.subtract, r=[b_Gcl], w=[b_f12])
                    kb.act(f12[:, 4:8], f12[:, 4:8], AF.Exp, r=[b_f12], w=[b_f12])
                    dec, b_dec = dec_r.get()
                    kb.tt("dve", dec[:], pG[:], bcm(mneg[:, :]), ALU.add, r=[b_pG, b_mneg], w=[b_dec])
                    kb.act(dec[:], dec[:], AF.Exp, r=[b_dec], w=[b_dec])
                    yield
                    decS, b_decS = decS_r.get()
                    kb.tt("dve", decS[:], dec[:], bcm(strict[:, :]), ALU.mult, r=[b_dec, b_strict], w=[b_decS])
                    pKK, b_pKK = pF_r.get()
                    mm4(pKK, b_pKK, kT, b_kT, kT, b_kT)
                    tmpL, b_tmpL = tmpL_r.get()
                    kb.tt("dve", tmpL[:], pKK[:], decS[:], ALU.mult, r=[b_pKK, b_decS], w=[b_tmpL])
                    L, b_L = L_r.get()
                    act4(L, tmpL, beta, [b_tmpL, b_BG], [b_L])
                    yield
                    pQK, b_pQK = pF_r.get()
                    mm4(pQK, b_pQK, qT, b_qT, kT, b_kT)
                    P, b_P = P_r.get()
                    kb.tt("dve", P[:], pQK[:], dec[:], ALU.mult, r=[b_pQK, b_dec], w=[b_P])
                    yield
                    pX, b_pX = pB_r.get()
                    for h in range(4):
                        kb.tr(pX[:, h, :], L[:, h, :], identb[:], r=[b_L, b_identb], w=[b_pX])
                    for h in range(4):
                        kb.tr(pX[:, 4 + h, :], P[:, h, :], identb[:], r=[b_P, b_identb], w=[b_pX])
                    LP, b_LP = LP_r.get()
                    kb.cp("act", LP[:], pX[:], r=[b_pX], w=[b_LP])
                    yield
                    LT = LP[:, 0:4, :]
                    PT = LP[:, 4:8, :]
                    cnt = [0]

                    def evac(dst, src_ps, b_src, b_dst, add=None, b_add=None, sub=False):
                        cnt[0] += 1
                        if add is None:
                            kb.cp("act" if cnt[0] % 2 else "dve", dst, src_ps, r=[b_src], w=[b_dst])
                        else:
                            kb.tt("dve", dst, add, src_ps, ALU.subtract if sub else ALU.add, r=[b_src, b_add], w=[b_dst])

                    def newt():
                        return gt_r.get()

                    L8, b_L8 = newt()
                    L8T, b_L8T = newt()
                    kb.tt("pool", L8[:], L[:], bcm(bmask[:, 0, :]), ALU.mult, r=[b_L, b_bmask], w=[b_L8])
                    kb.tt("pool", L8T[:], LT, bcm(bmask[:, 0, :]), ALU.mult, r=[b_LP, b_bmask], w=[b_L8T])
                    T0, b_T0 = newt()
                    T0T, b_T0T = newt()
                    kb.tt("pool", T0[:], bcm(identb[:, :]), L8[:], ALU.subtract, r=[b_identb, b_L8], w=[b_T0])
                    kb.tt("pool", T0T[:], bcm(identb[:, :]), L8T[:], ALU.subtract, r=[b_identb, b_L8T], w=[b_T0T])
                    def mk_E(lv):
                        E, b_E = newt()
                        ET, b_ET = newt()
                        kb.tt("pool", E[:], L[:], bcm(bmask[:, 1 + lv, :]), ALU.mult, r=[b_L, b_bmask], w=[b_E])
                        kb.tt("pool", ET[:], LT, bcm(bmask[:, 1 + lv, :]), ALU.mult, r=[b_LP, b_bmask], w=[b_ET])
                        return (E, b_E, ET, b_ET)
                    pM, b_pM = pF_r.get()
                    mm4(pM, b_pM, L8T, b_L8T, L8, b_L8)
                    M1, b_M1 = newt()
                    evac(M1[:], pM[:], b_pM, b_M1)
                    yield
                    pM, b_pM = pF_r.get()
                    mm4(pM, b_pM, L8, b_L8, L8T, b_L8T)
                    M1T, b_M1T = newt()
                    evac(M1T[:], pM[:], b_pM, b_M1T)
                    yield
                    pM, b_pM = pF_r.get()
                    mm4(pM, b_pM, T0T, b_T0T, M1, b_M1)
                    T1, b_T1 = newt()
                    evac(T1[:], pM[:], b_pM, b_T1, add=T0[:], b_add=b_T0)
                    yield
                    pM, b_pM = pF_r.get()
                    mm4(pM, b_pM, M1, b_M1, T0T, b_T0T)
                    T1T, b_T1T = newt()
                    evac(T1T[:], pM[:], b_pM, b_T1T, add=T0T[:], b_add=b_T0T)
                    yield
                    pM, b_pM = pF_r.get()
                    mm4(pM, b_pM, M1T, b_M1T, M1, b_M1)
                    M2, b_M2 = newt()
                    evac(M2[:], pM[:], b_pM, b_M2)
                    yield
                    pM, b_pM = pF_r.get()
                    mm4(pM, b_pM, T1T, b_T1T, M2, b_M2)
                    Tb, b_Tb = newt()
                    evac(Tb[:], pM[:], b_pM, b_Tb, add=T1[:], b_add=b_T1)
                    yield
                    pM, b_pM = pF_r.get()
                    mm4(pM, b_pM, M2, b_M2, T1T, b_T1T)
                    TbT, b_TbT = newt()
                    evac(TbT[:], pM[:], b_pM, b_TbT, add=T1T[:], b_add=b_T1T)
                    yield
                    nxtE = mk_E(0)
                    for lv in range(4):
                        E, b_E, ET, b_ET = nxtE
                        if lv < 3:
                            nxtE = mk_E(lv + 1)
                        pM, b_pM = pF_r.get()
                        mm4(pM, b_pM, E, b_E, TbT, b_TbT)
                        W1, b_W1 = newt()
                        evac(W1[:], pM[:], b_pM, b_W1)
                        yield
                        if lv < 3:
                            pV, b_pV = pF_r.get()
                            mm4(pV, b_pV, ET, b_ET, Tb, b_Tb)
                            V1, b_V1 = newt()
                            evac(V1[:], pV[:], b_pV, b_V1)
                            yield
                        pM, b_pM = pF_r.get()
                        mm4(pM, b_pM, Tb, b_Tb, W1, b_W1)
                        TnT, b_TnT = newt()
                        evac(TnT[:], pM[:], b_pM, b_TnT, add=TbT[:], b_add=b_TbT, sub=True)
                        yield
                        if lv < 3:
                            pV, b_pV = pF_r.get()
                            mm4(pV, b_pV, TbT, b_TbT, V1, b_V1)
                            Tn, b_Tn = newt()
                            evac(Tn[:], pV[:], b_pV, b_Tn, add=Tb[:], b_add=b_Tb, sub=True)
                            yield
                            Tb, b_Tb = Tn, b_Tn
                        TbT, b_TbT = TnT, b_TnT
                    Tt, b_Tt = TbT, b_TbT
                    vb, b_vb = vb_r.get()
                    kbg, b_kbg = kbg_r.get()
                    kd, b_kd = kd_r.get()
                    act4(vb, vtok, beta, [b_vtok, b_BG], [b_vb])
                    act4(kbg, ktok, f12[:, 0:4], [b_ktok, b_f12], [b_kbg])
                    act4(kd, ktok, f12[:, 4:8], [b_ktok, b_f12], [b_kd])
                    pu, b_pu = pF_r.get()
                    mm4(pu, b_pu, Tt, b_Tt, vb, b_vb)
                    u, b_u = u_r.get()
                    kb.cp("act", u[:], pu[:], r=[b_pu], w=[b_u])
                    pw, b_pw = pF_r.get()
                    mm4(pw, b_pw, kbg, b_kbg, Tt, b_Tt)
                    wT, b_wT = wT_r.get()
                    kb.cp("dve", wT[:], pw[:], r=[b_pw], w=[b_wT])
                    return dict(n=n, qT=(qT, b_qT), PT=(PT, b_LP), u=(u, b_u), wT=(wT, b_wT), kd=(kd, b_kd),
                                eGl=(eGl, b_eGl), z=(z, b_z))

                def scan(st_):
                    n = st_["n"]
                    qT, b_qT = st_["qT"]
                    PT, b_PT = st_["PT"]
                    u, b_u = st_["u"]
                    wT, b_wT = st_["wT"]
                    kd, b_kd = st_["kd"]
                    eGl, b_eGl = st_["eGl"]
                    z, b_z = st_["z"]
                    pwS, b_pwS = pF_r.get()
                    mm4(pwS, b_pwS, wT, b_wT, Sb, b_Sb)
                    vn, b_vn = vn_r.get()
                    kb.tt("dve", vn[:], u[:], pwS[:], ALU.subtract, r=[b_u, b_pwS], w=[b_vn])
                    yield
                    pA1, b_pA1 = pF_r.get()
                    mm4(pA1, b_pA1, qT, b_qT, Sb, b_Sb)
                    o1, b_o1 = o1_r.get()
                    kb.tt("dve", o1[:], pA1[:], bc4(eGl[:, 0:4]), ALU.mult, r=[b_pA1, b_eGl], w=[b_o1])
                    pA2, b_pA2 = pF_r.get()
                    mm4(pA2, b_pA2, PT, b_PT, vn, b_vn)
                    o, b_o = o_r.get()
                    kb.tt("dve", o[:], pA2[:], o1[:], ALU.add, r=[b_pA2, b_o1], w=[b_o])
                    yield
                    pSn, b_pSn = pF_r.get()
                    mm4(pSn, b_pSn, kd, b_kd, vn, b_vn)
                    act4(Sf, Sf, eGl[:, 4:8], [b_Sf, b_eGl, b_Sb], [b_Sf])
                    kb.tt("dve", Sf[:], pSn[:], Sf[:], ALU.add, r=[b_pSn, b_Sf], w=[b_Sf])
                    kb.cp("act", Sb[:], Sf[:], r=[b_Sf], w=[b_Sb])
                    yield
                    sq, b_sq = sq_r.get()
                    kb.act(sq[:], o[:], AF.Square, r=[b_o], w=[b_sq])
                    ss, b_ss = ss_r.get()
                    kb.red("dve", ss[:], sq[:], ALU.add, r=[b_sq], w=[b_ss])
                    kb.ts("dve", ss[:], ss[:], 1.0 / 128, EPS, ALU.mult, ALU.add, r=[b_ss], w=[b_ss])
                    kb.act(ss[:], ss[:], AF.Sqrt, r=[b_ss], w=[b_ss])
                    kb.recip(ss[:], ss[:], r=[b_ss], w=[b_ss])
                    yield
                    yf, b_yf = yf_r.get()
                    kb.tt("dve", yf[:], o[:], bc4(ss[:, :]), ALU.mult, r=[b_o, b_ss], w=[b_yf])
                    yfv = yf[:, :, :].rearrange("p h d -> p (h d)")
                    kb.tt("dve", yfv, yfv, onw_t[:], ALU.mult, r=[b_yf, b_onw], w=[b_yf])
                    ybt, b_ybt = yb_r.get()
                    kb.tt("dve", ybt[:], yfv, z[:], ALU.mult, r=[b_yf, b_z], w=[b_ybt])
                    kb.dma(yb_s[n * 128:(n + 1) * 128, :], ybt[:], r=[b_ybt], w=[b_yb_s])

                def run_rr(gens):
                    results = [None] * len(gens)
                    active = list(range(len(gens)))
                    while active:
                        for gi in list(active):
                            try:
                                next(gens[gi])
                            except StopIteration as ex:
                                results[gi] = ex.value
                                active.remove(gi)
                    return results

                def scan_pair(sa, sb2):
                    yield from scan(sa)
                    yield from scan(sb2)

                WPAR, MAXAHEAD = 3, 5
                next_chunk, next_scan = 0, 0
                preps, done_st, scan_gen = [], {}, None
                while next_scan < NT:
                    while len(preps) < WPAR and next_chunk < NT and (next_chunk - next_scan) < MAXAHEAD:
                        preps.append((next_chunk, prep(next_chunk)))
                        next_chunk += 1
                    for (n_, g_) in list(preps):
                        try:
                            next(g_)
                        except StopIteration as ex:
                            done_st[n_] = ex.value
                            preps.remove((n_, g_))
                    if scan_gen is None and next_scan in done_st:
                        scan_gen = scan(done_st.pop(next_scan))
                    if scan_gen is not None:
                        try:
                            next(scan_gen)
                        except StopIteration:
                            scan_gen = None
                            next_scan += 1
                S.barrier()

            if li == 0 and stop_after == "D":
                return finish(nc, kb, top, out, b_out)

            def make_norm(ph):
                junk_r = kb.ring_sb(ph, 1, [128, D], BF16, "njunk")
                xn_r = kb.ring_sb(ph, 2, [128, D], BF16, "nxn")
                st_r = kb.ring_sb(ph, 4, [128, 2], F32, "nst")
                pT_r = kb.ring_ps(ph, 2, [128, 8, 128], BF16, "npT")

                def norm_tile(xt_ap, b_xt, ai, hv, b_hT):
                    junk, b_junk = junk_r.get()
                    st, b_st = st_r.get()
                    kb.act(junk[:], xt_ap, AF.Square, r=[b_xt], w=[b_junk, b_st], accum_out=st[:, 0:1])
                    kb.ts("dve", st[:, 1:2], st[:, 0:1], 1.0 / D, EPS, ALU.mult, ALU.add, r=[b_st], w=[b_st])
                    kb.act(st[:, 1:2], st[:, 1:2], AF.Sqrt, r=[b_st], w=[b_st])
                    kb.recip(st[:, 1:2], st[:, 1:2], r=[b_st], w=[b_st])
                    xn, b_xn = xn_r.get()
                    kb.act(xn[:], xt_ap, AF.Copy, r=[b_xt, b_st], w=[b_xn], scale=st[:, 1:2])
                    pT, b_pT = pT_r.get()
                    for k in range(8):
                        kb.tr(pT[:, k, :], xn[:, k * 128:(k + 1) * 128], identb[:], r=[b_xn, b_identb], w=[b_pT])
                    kb.tt("dve", hv, pT[:], AB[:, ai, :].unsqueeze(2).to_broadcast([128, 8, 128]), ALU.mult,
                          r=[b_pT, b_AB], w=[b_hT])
                    kb.tt("pool", hv, hv, AB[:, ai + 1, :].unsqueeze(2).to_broadcast([128, 8, 128]), ALU.add,
                          r=[b_hT, b_AB], w=[b_hT])
                return norm_tile

            for hf in ([None] if last else [0, 1]):
                with ExitStack() as phEF:
                    x1, _ = kb.sb(phEF, [128, 16, D], F32, "x1")
                    b_x1 = [Buf() for _ in range(16)]

                    with ExitStack() as ph:
                        Wua, b_Wua = kb.sb(ph, [128, 4, D], BF16, "Wua")
                        Wub, b_Wub = kb.sb(ph, [128, 4, D], BF16, "Wub")
                        Wg, b_Wg = kb.sb(ph, [128, 8, 2 * D], BF16, "Wg")
                        Wo, b_Wo = kb.sb(ph, [128, 8, D], BF16, "Wo")
                        with ExitStack() as ph2:
                            stg = kb.ring_sb(ph2, 2, [128, 8, 512], F32)
                            wuav = w_up_a.rearrange("(k p) n -> p k n", p=128)
                            wubv = w_up_b.rearrange("(k p) n -> p k n", p=128)
                            wov = w_out.rearrange("(k p) n -> p k n", p=128)
                            i = 0
                            for (dst, bd, src, nk, ncol, c0s) in ((Wua, b_Wua, wuav, 4, 2, 0), (Wub, b_Wub, wubv, 4, 2, 0),
                                                                  (Wg, b_Wg, wiv, 8, 4, 3592), (Wo, b_Wo, wov, 8, 2, 0)):
                                for g in range(ncol):
                                    st_t, b_st = stg.get()
                                    kb.dma(st_t[:, 0:nk, :], src[:, :, c0s + g * 512:c0s + (g + 1) * 512], w=[b_st])
                                    kb.cp("pool" if i % 2 else "dve", dst[:, :, g * 512:(g + 1) * 512], st_t[:, 0:nk, :],
                                          r=[b_st], w=[bd])
                                    i += 1
                            S.barrier()
                        norm_tile = make_norm(ph)
                        hoT_r = kb.ring_sb(ph, 1, [128, 8, 512], BF16, "hoT")
                        yaT_r = kb.ring_sb(ph, 1, [128, 4, 512], BF16, "yaT")
                        ybT_r = kb.ring_sb(ph, 1, [128, 4, 512], BF16, "ybT")
                        yl_r = kb.ring_sb(ph, 4, [128, 512], BF16, "yl")
                        yo_r = kb.ring_sb(ph, 2, [128, 512], BF16, "yo")
                        xl_r = kb.ring_sb(ph, 1, [128, D], F32, "xl")
                        pX_r = kb.ring_ps(ph, 1, [128, 8, 128], BF16, "epX")
                        p5_r = kb.ring_ps(ph, 5, [128, 512], F32, "ep5")
                        sg_r = kb.ring_sb(ph, 2, [128, 512], F32, "esg")
                        m12_r = kb.ring_sb(ph, 2, [128, 512], F32, "em12")
                        mT_r = kb.ring_sb(ph, 1, [128, 8, 512], BF16, "mT")
                        tmo_r = kb.ring_sb(ph, 2, [128, 512], F32, "tmo")
                        for c in range(4):
                            hoT, b_hoT = hoT_r.get()
                            yaT, b_yaT = yaT_r.get()
                            ybT, b_ybT = ybT_r.get()
                            for j in range(4):
                                ti = 4 * c + j
                                if hf is not None:
                                    r0 = hf * TO + ti * 128
                                    kb.dma(x1[:, ti, :], xsrc_full[r0:r0 + 128, :], r=[b_xfull_s], w=[b_x1[ti]])
                                elif li == 0:
                                    kb.dma(x1[:, ti, :], xo[ti * 128:(ti + 1) * 128, :], w=[b_x1[ti]])
                                else:
                                    xl, b_xl = xl_r.get()
                                    kb.dma(x1[:, ti, :], xfull_s[ti * 128:(ti + 1) * 128, :], r=[b_xfull_s], w=[b_x1[ti]])
                                    kb.dma(xl[:], xfull_s[TO + ti * 128:TO + (ti + 1) * 128, :], r=[b_xfull_s], w=[b_xl])
                                    kb.ts("dve", x1[:, ti, :], x1[:, ti, :], selt[:, 0:1], None, ALU.mult, r=[b_x1[ti], b_selt], w=[b_x1[ti]])
                                    kb.stt("dve", x1[:, ti, :], xl[:], selt[:, 1:2], x1[:, ti, :], ALU.mult, ALU.add,
                                           r=[b_xl, b_selt, b_x1[ti]], w=[b_x1[ti]])
                                norm_tile(x1[:, ti, :], b_x1[ti], 0, hoT[:, :, j * 128:(j + 1) * 128], b_hoT)
                                for (src, bsrc, dstT, bdst) in ((ya_s, b_ya_s, yaT, b_yaT), (yb_s, b_yb_s, ybT, b_ybT)):
                                    if hf is not None:
                                        yo, b_yo = yl_r.get()
                                        r0 = hf * TO + ti * 128
                                        kb.dma(yo[:], src[r0:r0 + 128, :], r=[bsrc], w=[b_yo])
                                    else:
                                        la, b_la = yl_r.get()
                                        lb, b_lb = yl_r.get()
                                        kb.dma(la[:], src[ti * 128:(ti + 1) * 128, :], r=[bsrc], w=[b_la])
                                        kb.dma(lb[:], src[TO + ti * 128:TO + (ti + 1) * 128, :], r=[bsrc], w=[b_lb])
                                        yo, b_yo = yo_r.get()
                                        kb.ts("dve", yo[:], la[:], selt[:, 0:1], None, ALU.mult, r=[b_la, b_selt], w=[b_yo])
                                        kb.stt("dve", yo[:], lb[:], selt[:, 1:2], yo[:], ALU.mult, ALU.add, r=[b_lb, b_selt, b_yo], w=[b_yo])
                                    pX, b_pX = pX_r.get()
                                    for kc in range(4):
                                        kb.tr(pX[:, kc, :], yo[:, kc * 128:(kc + 1) * 128], identb[:], r=[b_yo, b_identb], w=[b_pX])
                                    kb.cp("act", dstT[:, :, j * 128:(j + 1) * 128], pX[:, 0:4, :], r=[b_pX], w=[bdst])
                            mT, b_mT = mT_r.get()
                            for f in range(8):
                                fs = slice(f * 128, (f + 1) * 128)
                                pUa, b_pUa = p5_r.get()
                                for kc in range(4):
                                    kb.mm(pUa[:], Wua[:, kc, fs], yaT[:, kc, :], kc == 0, kc == 3, r=[b_Wua, b_yaT], w=[b_pUa])
                                pga, b_pga = p5_r.get()
                                for k in range(8):
                                    kb.mm(pga[:], Wg[:, k, fs], hoT[:, k, :], k == 0, k == 7, r=[b_Wg, b_hoT], w=[b_pga])
                                sa, b_sa = sg_r.get()
                                kb.act(sa[:], pga[:], AF.Sigmoid, r=[b_pga], w=[b_sa])
                                m1, b_m1 = m12_r.get()
                                kb.tt("dve", m1[:], pUa[:], sa[:], ALU.mult, r=[b_pUa, b_sa], w=[b_m1])
                                pUb, b_pUb = p5_r.get()
                                for kc in range(4):
                                    kb.mm(pUb[:], Wub[:, kc, fs], ybT[:, kc, :], kc == 0, kc == 3, r=[b_Wub, b_ybT], w=[b_pUb])
                                pgb, b_pgb = p5_r.get()
                                for k in range(8):
                                    kb.mm(pgb[:], Wg[:, k, D + f * 128:D + (f + 1) * 128], hoT[:, k, :], k == 0, k == 7,
                                          r=[b_Wg, b_hoT], w=[b_pgb])
                                sb_, b_sb_ = sg_r.get()
                                kb.act(sb_[:], pgb[:], AF.Sigmoid, r=[b_pgb], w=[b_sb_])
                                m2, b_m2 = m12_r.get()
                                kb.tt("dve", m2[:], pUb[:], sb_[:], ALU.mult, r=[b_pUb, b_sb_], w=[b_m2])
                                kb.tt("pool", mT[:, f, :], m1[:], m2[:], ALU.add, r=[b_m1, b_m2], w=[b_mT])
                            for j in range(4):
                                ti = 4 * c + j
                                for half in range(2):
                                    hs = slice(half * 512, (half + 1) * 512)
                                    pmo, b_pmo = p5_r.get()
                                    for f in range(8):
                                        kb.mm(pmo[:], mT[:, f, j * 128:(j + 1) * 128], Wo[:, f, hs], f == 0, f == 7,
                                              r=[b_mT, b_Wo], w=[b_pmo])
                                    tmo, b_tmo = tmo_r.get()
                                    kb.tt("dve", tmo[:], pmo[:], G1bc[:, hs], ALU.mult, r=[b_pmo, b_G1bc], w=[b_tmo])
                                    kb.tt("pool", x1[:, ti, hs], x1[:, ti, hs], tmo[:], ALU.add, r=[b_tmo, b_x1[ti]], w=[b_x1[ti]])
                        if dbg and li == 0:
                            for ti in range(16):
                                kb.dma(dbg_x1[ti * 128:(ti + 1) * 128, :], x1[:, ti, :], r=[b_x1[ti]], w=[Buf()])
                        S.barrier()

                    if li == 0 and stop_after == "E":
                        return finish(nc, kb, top, out, b_out)

                    with ExitStack() as ph:
                        h2T, _ = kb.sb(ph, [128, 8, TO], BF16, "h2T")
                        b_h2T = [Buf() for _ in range(4)]
                        comb, b_comb = kb.sb(ph, [128, 16, 32], F32, "comb")
                        Wr, b_Wr = kb.sb(ph, [128, 8, 36], BF16, "Wr")
                        Wrf, b_Wrf = kb.sb(ph, [128, 8, 36], F32, "Wrf")
                        brt, b_brt = kb.sb(ph, [128, 36], F32, "brt")
                        kb.dma(Wrf[:], w_r.rearrange("(k p) n -> p k n", p=128), w=[b_Wrf])
                        kb.cp("dve", Wr[:], Wrf[:], r=[b_Wrf], w=[b_Wr])
                        kb.dma(brt[:], b_r[:, :], w=[b_brt])
                        norm_tile = make_norm(ph)
                        p6_r = kb.ring_ps(ph, 6, [128, 512], F32, "fp6")
                        lg_r = kb.ring_sb(ph, 2, [128, 36], F32, "lg")
                        sm_r = kb.ring_sb(ph, 2, [128, 16], F32, "sm")
                        e4_r = kb.ring_sb(ph, 2, [128, 4], F32, "e4")
                        lem_r = kb.ring_sb(ph, 2, [128, 32], F32, "lem")
                        m8_r = kb.ring_sb(ph, 2, [128, 8], F32, "fm8")
                        c12_r = kb.ring_sb(ph, 4, [128, 32], F32, "c12")
                        for ti in range(16):
                            c = ti // 4
                            tsl = slice(ti * 128, (ti + 1) * 128)
                            norm_tile(x1[:, ti, :], b_x1[ti], 2, h2T[:, :, tsl], b_h2T[c])
                            pl, b_pl = p6_r.get()
                            for k in range(8):
                                kb.mm(pl[:, 0:36], h2T[:, k, tsl], Wr[:, k, :], k == 0, k == 7, r=[b_h2T[c], b_Wr], w=[b_pl])
                            lg, b_lg = lg_r.get()
                            kb.tt("dve", lg[:], pl[:, 0:36], brt[:], ALU.add, r=[b_pl, b_brt], w=[b_lg])
                            sm, b_sm = sm_r.get()
                            kb.red("dve", sm[:, 0:1], lg[:, 0:4], ALU.max, r=[b_lg], w=[b_sm])
                            kb.ts("dve", sm[:, 1:2], sm[:, 0:1], -1.0, None, ALU.mult, r=[b_sm], w=[b_sm])
                            e4, b_e4 = e4_r.get()
                            kb.act(e4[:], lg[:, 0:4], AF.Exp, r=[b_lg, b_sm], w=[b_e4, b_sm], bias=sm[:, 1:2], accum_out=sm[:, 2:3])
                            kb.recip(sm[:, 3:4], sm[:, 2:3], r=[b_sm], w=[b_sm])
                            kb.ts("dve", sm[:, 8:12], lg[:, 0:4], sm[:, 0:1], None, ALU.is_ge, r=[b_lg, b_sm], w=[b_sm])
                            kb.ts("dve", sm[:, 8:12], sm[:, 8:12], -1.0, 1e9, ALU.add, ALU.mult, r=[b_sm], w=[b_sm])
                            lem, b_lem = lem_r.get()
                            kb.tt("dve", lem[:, :].rearrange("p (g e) -> p g e", e=8), lg[:, 4:36].rearrange("p (g e) -> p g e", e=8),
                                  sm[:, 8:12].unsqueeze(2).to_broadcast([128, 4, 8]), ALU.add, r=[b_lg, b_sm], w=[b_lem])
                            m8, b_m8 = m8_r.get()
                            kb.max8(m8[:], lem[:], r=[b_lem], w=[b_m8])
                            kb.tt("dve", sm[:, 4:5], m8[:, 0:1], m8[:, 1:2], ALU.subtract, r=[b_m8], w=[b_sm])
                            kb.act(sm[:, 5:6], sm[:, 4:5], AF.Sigmoid, r=[b_sm], w=[b_sm])
                            kb.tt("dve", sm[:, 6:7], sm[:, 5:6], sm[:, 3:4], ALU.mult, r=[b_sm], w=[b_sm])
                            kb.tt("dve", sm[:, 7:8], sm[:, 3:4], sm[:, 6:7], ALU.subtract, r=[b_sm], w=[b_sm])
                            c1, b_c1 = c12_r.get()
                            c2, b_c2 = c12_r.get()
                            kb.ts("dve", c1[:], lem[:], m8[:, 0:1], sm[:, 6:7], ALU.is_equal, ALU.mult, r=[b_lem, b_m8, b_sm], w=[b_c1])
                            kb.ts("dve", c2[:], lem[:], m8[:, 1:2], sm[:, 7:8], ALU.is_equal, ALU.mult, r=[b_lem, b_m8, b_sm], w=[b_c2])
                            kb.tt("dve", comb[:, ti, :], c1[:], c2[:], ALU.add, r=[b_c1, b_c2], w=[b_comb])
                        stg_r = kb.ring_sb(ph, 3, [128, 2048], F32, "xstg")
                        Wge_r = kb.ring_sb(ph, 2, [128, 8, 256], BF16, "Wge")
                        Wue_r = kb.ring_sb(ph, 2, [128, 8, 256], BF16, "Wue")
                        Wde_r = kb.ring_sb(ph, 2, [128, 2, D], BF16, "Wde")
                        sg_r = kb.ring_sb(ph, 3, [128, 512], F32, "fsg")
                        aT_r = kb.ring_sb(ph, 6, [128, 512], BF16, "aT")
                        tmo_r = kb.ring_sb(ph, 4, [128, 512], F32, "ftmo")
                        for e in range(32):
                            Wge, b_Wge = Wge_r.get()
                            Wue, b_Wue = Wue_r.get()
                            Wde, b_Wde = Wde_r.get()
                            s1, b_s1 = stg_r.get()
                            kb.dma(s1[:, :].rearrange("p (k f) -> p k f", f=256), w_eg[e].rearrange("(k p) f -> p k f", p=128), w=[b_s1])
                            kb.cp("pool", Wge[:, :, :].rearrange("p k f -> p (k f)"), s1[:], r=[b_s1], w=[b_Wge])
                            s2, b_s2 = stg_r.get()
                            kb.dma(s2[:, :].rearrange("p (k f) -> p k f", f=256), w_eu[e].rearrange("(k p) f -> p k f", p=128), w=[b_s2])
                            kb.cp("act", Wue[:, :, :].rearrange("p k f -> p (k f)"), s2[:], r=[b_s2], w=[b_Wue])
                            s3, b_s3 = stg_r.get()
                            kb.dma(s3[:, :].rearrange("p (k n) -> p k n", n=D), w_ed[e].rearrange("(k p) n -> p k n", p=128), w=[b_s3])
                            kb.tt("dve", Wde[:, :, :], s3[:, :].rearrange("p (k n) -> p k n", n=D),
                                  G2bc[:, :].unsqueeze(1).to_broadcast([128, 2, D]), ALU.mult, r=[b_s3, b_G2bc], w=[b_Wde])
                            def gate_up(c):
                                cs = slice(c * 512, (c + 1) * 512)
                                aTs = []
                                for ft in range(2):
                                    fs = slice(ft * 128, (ft + 1) * 128)
                                    pGt, b_pGt = p6_r.get()
                                    for k in range(8):
                                        kb.mm(pGt[:], Wge[:, k, fs], h2T[:, k, cs], k == 0, k == 7, r=[b_Wge, b_h2T[c]], w=[b_pGt])
                                    pUt, b_pUt = p6_r.get()
                                    for k in range(8):
                                        kb.mm(pUt[:], Wue[:, k, fs], h2T[:, k, cs], k == 0, k == 7, r=[b_Wue, b_h2T[c]], w=[b_pUt])
                                    sg, b_sg = sg_r.get()
                                    kb.act(sg[:], pGt[:], AF.Silu, r=[b_pGt], w=[b_sg])
                                    aT, b_aT = aT_r.get()
                                    kb.tt("dve", aT[:], pUt[:], sg[:], ALU.mult, r=[b_pUt, b_sg], w=[b_aT])
                                    aTs.append((aT, b_aT))
                                return aTs

                            def down(c, aTs):
                                for j in range(4):
                                    ti = 4 * c + j
                                    for half in range(2):
                                        hs = slice(half * 512, (half + 1) * 512)
                                        pd, b_pd = p6_r.get()
                                        for ft in range(2):
                                            kb.mm(pd[:], aTs[ft][0][:, j * 128:(j + 1) * 128], Wde[:, ft, hs], ft == 0, ft == 1,
                                                  r=[aTs[ft][1], b_Wde], w=[b_pd])
                                        kb.stt("dve", x1[:, ti, hs], pd[:], comb[:, ti, e:e + 1], x1[:, ti, hs], ALU.mult, ALU.add,
                                               r=[b_pd, b_comb, b_x1[ti]], w=[b_x1[ti]])

                            cur_a = gate_up(0)
                            for c in range(4):
                                nxt_a = gate_up(c + 1) if c + 1 < 4 else None
                                down(c, cur_a)
                                cur_a = nxt_a
                        if final and last:
                            fnw_t, b_fnw = kb.sb(ph, [128, D], F32, "fnw")
                            kb.dma(fnw_t[:], fnw[:, :], w=[b_fnw])
                            fst_r = kb.ring_sb(ph, 2, [128, 2], F32, "fst")
                            fj_r = kb.ring_sb(ph, 2, [128, D], F32, "fj")
                            for ti in range(16):
                                st, b_st = fst_r.get()
                                fj, b_fj = fj_r.get()
                                kb.act(fj[:], x1[:, ti, :], AF.Square, r=[b_x1[ti]], w=[b_fj, b_st], accum_out=st[:, 0:1])
                                kb.ts("dve", st[:, 1:2], st[:, 0:1], 1.0 / D, EPS, ALU.mult, ALU.add, r=[b_st], w=[b_st])
                                kb.act(st[:, 1:2], st[:, 1:2], AF.Sqrt, r=[b_st], w=[b_st])
                                kb.recip(st[:, 1:2], st[:, 1:2], r=[b_st], w=[b_st])
                                kb.act(fj[:], x1[:, ti, :], AF.Copy, r=[b_x1[ti], b_st], w=[b_fj], scale=st[:, 1:2])
                                kb.tt("dve", fj[:], fj[:], fnw_t[:], ALU.mult, r=[b_fj, b_fnw], w=[b_fj])
                                kb.dma(out[ti * 128:(ti + 1) * 128, :], fj[:], r=[b_fj], w=[b_out])
                        elif last:
                            for ti in range(16):
                                kb.dma(out[ti * 128:(ti + 1) * 128, :], x1[:, ti, :], r=[b_x1[ti]], w=[b_out])
                        else:
                            for ti in range(16):
                                r0 = hf * TO + ti * 128
                                kb.dma(xfull_s[r0:r0 + 128, :], x1[:, ti, :], r=[b_x1[ti]], w=[b_xfull_s])
                        S.barrier()
        return finish(nc, kb, top, out, b_out)


def finish(nc, kb, top, out, b_out):
    esem = {e: top.enter_context(nc.semaphore("es_" + e)) for e in ENGS}
    dsem = {}
    for e in ("sp", "pool"):
        for s in range(NDMA):
            dsem[(e, s)] = top.enter_context(nc.semaphore(f"ds_{e}_{s}"))
    with nc.Block() as block:
        kb.S.emit(block, esem, dsem)
    return nc


def make_consts(rel_bias):
    bf = ml_dtypes.bfloat16
    cst = {}
    cst["identb"] = np.eye(128, dtype=np.float32).astype(bf)
    cst["identf"] = np.eye(128, dtype=np.float32)
    oh = np.zeros((16, T), np.float32)
    for n in range(16):
        oh[n, n * 256:(n + 1) * 256] = 1.0
    cst["onehotK"] = oh.astype(bf)
    pn = np.zeros((128, 17, 8, 16), np.float32)
    for qb in range(17):
        pn[:, qb, :, qb:] = -1e30
    cst["padneg"] = pn.reshape(128, 17, 128)
    ki = np.arange(128)[:, None]
    col = np.arange(1024)[None, :]
    dist = col - 384 - ki
    bucket = t5_bucket_np(dist)
    rb = np.asarray(rel_bias, np.float32)
    cst["braw"] = np.ascontiguousarray(np.transpose(rb[bucket], (2, 0, 1)))
    cst["negm"] = np.where(dist >= 0, 0.0, -1e4).astype(np.float32)
    cst["b31"] = np.ascontiguousarray(np.broadcast_to(rb[31][None, :], (128, 8)))
    i = np.arange(128)[:, None]
    j = np.arange(128)[None, :]
    cst["mneg"] = np.where(j <= i, 0.0, -1e5).astype(np.float32)
    cst["strict"] = (j < i).astype(np.float32)
    cst["ut"] = (i <= j).astype(np.float32)
    bm = np.zeros((128, 5, 128), np.float32)
    bm[:, 0, :] = (i // 8 == j // 8)
    for lv, b in enumerate((8, 16, 32, 64)):
        bm[:, 1 + lv, :] = (i // (2 * b) == j // (2 * b)) & (i // b != j // b)
    cst["bmask"] = bm.astype(bf)
    return cst


def fm(v):
    return np.ascontiguousarray(np.asarray(v, np.float32).reshape(8, 128).T)


def layer_inputs(inp, l, cst):
    m = dict(cst)
    m["w_mod"] = np.ascontiguousarray(inp["w_mod"][l])
    m["b_mod"] = np.ascontiguousarray(inp["b_mod"][l][None, :])
    m["n1w"] = fm(inp["norm1_w"][l])
    m["n2w"] = fm(inp["norm2_w"][l])
    m["w_in"] = np.ascontiguousarray(inp["w_in"][l])
    cw = np.asarray(inp["conv_w"][l], np.float32)
    m["convT"] = np.ascontiguousarray(cw.reshape(4, 12, 128).transpose(2, 1, 0))
    m["alog"] = np.ascontiguousarray(np.broadcast_to(inp["a_log"][l][None, :], (128, 4)))
    m["dtb"] = np.ascontiguousarray(np.broadcast_to(inp["dt_bias"][l][None, :], (128, 4)))
    m["onw"] = np.ascontiguousarray(np.broadcast_to(np.tile(inp["onorm_w"][l], 4)[None, :], (128, 512)))
    m["w_up_a"] = np.ascontiguousarray(inp["w_up_a"][l])
    m["w_up_b"] = np.ascontiguousarray(inp["w_up_b"][l])
    m["w_out"] = np.ascontiguousarray(inp["w_out"][l])
    m["w_r"] = np.ascontiguousarray(np.concatenate([inp["w_rg"][l], inp["w_re"][l]], axis=1))
    br = np.concatenate([inp["b_rg"][l], inp["b_re"][l]])
    m["b_r"] = np.ascontiguousarray(np.broadcast_to(br[None, :], (128, 36)))
    m["w_eg"] = np.ascontiguousarray(inp["w_e_gate"][l].reshape(32, D, 256))
    m["w_eu"] = np.ascontiguousarray(inp["w_e_up"][l].reshape(32, D, 256))
    m["w_ed"] = np.ascontiguousarray(inp["w_e_down"][l].reshape(32, 256, D))
    m["fnw"] = np.ascontiguousarray(np.broadcast_to(inp["final_norm_w"][None, :], (128, D)))
    return m


def core_inputs(base, x_full, c, core):
    b, half = core // 2, core % 2
    m = dict(base)
    m["xf"] = np.ascontiguousarray(x_full[b])
    m["xo"] = np.ascontiguousarray(x_full[b, half * TO:(half + 1) * TO])
    m["cT"] = fm(c[b])
    s = np.zeros((128, 2), np.float32)
    s[:, half] = 1.0
    m["sel"] = s
    return m


_PROGS = {}


def _prog():
    if "p" not in _PROGS:
        _PROGS["p"] = build_program(final=True, nl=2)
    return _PROGS["p"]


def kernel(**inputs):
    inp = {k: np.asarray(v) for k, v in inputs.items()}
    cst = make_consts(inp["rel_bias"])
    x = np.asarray(inp["x"], np.float32)
    c = np.asarray(inp["c"], np.float32)
    base = dict(cst)
    shared = set(cst.keys()) | {"fnw"}
    for l in range(2):
        li = layer_inputs(inp, l, cst)
        for k, v in li.items():
            if k in shared:
                base[k] = v
            else:
                base[f"{k}_{l}"] = v
    in_maps = [core_inputs(base, x, c, core) for core in range(8)]
    res = run_bass_kernel_spmd(_prog(), in_maps, core_ids=list(range(8)))
    return np.stack([np.concatenate([res.results[2 * b]["out"], res.results[2 * b + 1]["out"]], axis=0)
                     for b in range(4)]).astype(np.float32)
```
